# Optimizing a Trainium2 kernel written in Bass

```python
import math
import jax, jax.numpy as jnp
from jax import lax
import numpy as np

D_MODEL = 1024
BATCH = 8
SEQ = 4096
DEPTH = 1

HEAD_DIM = 64
SB_HEADS = 8
FOX_HEADS = 8
SB_WIDTH = SB_HEADS * HEAD_DIM
FOX_WIDTH = FOX_HEADS * HEAD_DIM
MIX_WIDTH = SB_WIDTH + FOX_WIDTH
IN_COLS = 3 * SB_WIDTH + 3 * FOX_WIDTH + FOX_HEADS
Q_BLOCK = 128
PEER_HEADS = 8
N_KEYS = 128
N_EXPERTS = N_KEYS * N_KEYS
PEER_TOPK = 16
PEER_DKEY = 256
PEER_HALF = PEER_DKEY // 2
PEER_CHUNK = 128
N_MOD = 6
EPS = 1e-6

kernel_name = "hybrid_sb_fox_peer_adaln_block"


def rmsnorm(x, g):
    xf = x.astype(jnp.float32)
    y = xf * lax.rsqrt(jnp.mean(xf * xf, axis=-1, keepdims=True) + EPS)
    return (y * g.astype(jnp.float32)).astype(x.dtype)


def modulate(h, shift, scale):
    return h * (1.0 + scale[:, None, :]) + shift[:, None, :]


def split_heads(t, n_heads):
    b, s, _ = t.shape
    return t.reshape(b, s, n_heads, HEAD_DIM).transpose(0, 2, 1, 3)


def headwise_rmsnorm(o, g):
    of = o.astype(jnp.float32)
    y = of * lax.rsqrt(jnp.mean(of * of, axis=-1, keepdims=True) + EPS)
    return (y * g.astype(jnp.float32)[None, :, None, :]).astype(o.dtype)


def stick_breaking_attention(q, k, v):
    s_len = q.shape[2]
    inv = 1.0 / math.sqrt(HEAD_DIM)
    outs = []
    for i0 in range(0, s_len, Q_BLOCK):
        end = i0 + Q_BLOCK
        qb = q[:, :, i0:end]
        kb = k[:, :, :end]
        vb = v[:, :, :end]
        z = jnp.einsum('bhqd,bhkd->bhqk', qb, kb).astype(jnp.float32) * inv
        qpos = i0 + jnp.arange(Q_BLOCK)
        kpos = jnp.arange(end)
        strict = kpos[None, :] < qpos[:, None]
        log1m = jnp.where(strict, -jax.nn.softplus(z), 0.0)
        after = lax.cumsum(log1m, axis=3, reverse=True) - log1m
        a = jnp.where(strict, jnp.exp(jax.nn.log_sigmoid(z) + after), 0.0)
        outs.append(jnp.einsum('bhqk,bhkd->bhqd', a.astype(vb.dtype), vb))
    return jnp.concatenate(outs, axis=2)


def forgetting_attention(q, k, v, log_f):
    s_len = q.shape[2]
    inv = 1.0 / math.sqrt(HEAD_DIM)
    cum = lax.cumsum(log_f, axis=2)
    outs = []
    for i0 in range(0, s_len, Q_BLOCK):
        end = i0 + Q_BLOCK
        qb = q[:, :, i0:end]
        kb = k[:, :, :end]
        vb = v[:, :, :end]
        z = jnp.einsum('bhqd,bhkd->bhqk', qb, kb).astype(jnp.float32) * inv
        z = z + cum[:, :, i0:end, None] - cum[:, :, None, :end]
        qpos = i0 + jnp.arange(Q_BLOCK)
        kpos = jnp.arange(end)
        causal = kpos[None, :] <= qpos[:, None]
        p = jax.nn.softmax(jnp.where(causal, z, -jnp.inf), axis=-1)
        outs.append(jnp.einsum('bhqk,bhkd->bhqd', p.astype(vb.dtype), vb))
    return jnp.concatenate(outs, axis=2)


def peer_ffn(h, w_query, sub_keys, expert_u, expert_v):
    b, s, d = h.shape
    t = b * s
    hf = h.reshape(t, d)
    q = (hf @ w_query).reshape(t, PEER_HEADS, 2, PEER_HALF)
    scores = jnp.einsum('thpc,hpnc->thpn', q, sub_keys).astype(jnp.float32)
    top_s, top_i = lax.top_k(scores, PEER_TOPK)
    cand = top_s[:, :, 0, :, None] + top_s[:, :, 1, None, :]
    best_s, best_pos = lax.top_k(cand.reshape(t, PEER_HEADS, PEER_TOPK * PEER_TOPK), PEER_TOPK)
    i1 = jnp.take_along_axis(top_i[:, :, 0], best_pos // PEER_TOPK, axis=-1)
    i2 = jnp.take_along_axis(top_i[:, :, 1], best_pos % PEER_TOPK, axis=-1)
    expert_idx = i1 * N_KEYS + i2
    gates = jax.nn.softmax(best_s, axis=-1)
    n_chunks = t // PEER_CHUNK

    def chunk_fn(args):
        xc, idx, g = args
        u = expert_u[idx]
        act = jax.nn.gelu(jnp.einsum('cd,chkd->chk', xc, u), approximate=False)
        vv = expert_v[idx]
        return jnp.einsum('chk,chkd->cd', (g * act).astype(vv.dtype), vv)

    out = lax.map(chunk_fn, (hf.reshape(n_chunks, PEER_CHUNK, d),
                             expert_idx.reshape(n_chunks, PEER_CHUNK, PEER_HEADS, PEER_TOPK),
                             gates.reshape(n_chunks, PEER_CHUNK, PEER_HEADS, PEER_TOPK)))
    return out.reshape(b, s, d).astype(h.dtype)


def setup_inputs(seed: int = 0) -> dict:
    key = jax.random.key(seed)
    ks = jax.random.split(key, 20)
    f32 = jnp.float32
    D = D_MODEL
    nrm = lambda k, shape, scale: jax.random.normal(k, shape, f32) * scale
    return {
        "x": nrm(ks[0], (BATCH, SEQ, D), 1.0),
        "c": nrm(ks[1], (BATCH, D), 1.0),
        "w_ada": nrm(ks[2], (DEPTH, D, N_MOD * D), D ** -0.5),
        "b_ada": nrm(ks[3], (DEPTH, N_MOD * D), 0.02),
        "norm_mix_g": 1.0 + nrm(ks[4], (DEPTH, D), 0.02),
        "w_in": nrm(ks[5], (DEPTH, D, IN_COLS), D ** -0.5),
        "b_fgate": 3.0 + nrm(ks[6], (DEPTH, FOX_HEADS), 0.5),
        "gn_sb_g": 1.0 + nrm(ks[7], (DEPTH, SB_HEADS, HEAD_DIM), 0.02),
        "gn_fox_g": 1.0 + nrm(ks[8], (DEPTH, FOX_HEADS, HEAD_DIM), 0.02),
        "w_out": nrm(ks[9], (DEPTH, MIX_WIDTH, D), MIX_WIDTH ** -0.5),
        "norm_ffn_g": 1.0 + nrm(ks[10], (DEPTH, D), 0.02),
        "w_query": nrm(ks[11], (DEPTH, D, PEER_HEADS * PEER_DKEY), D ** -0.5),
        "sub_keys": nrm(ks[12], (DEPTH, PEER_HEADS, 2, N_KEYS, PEER_HALF), PEER_HALF ** -0.5),
        "expert_u": nrm(ks[13], (DEPTH, N_EXPERTS, D), D ** -0.5),
        "expert_v": nrm(ks[14], (DEPTH, N_EXPERTS, D), PEER_HEADS ** -0.5),
        "w_ada_final": nrm(ks[15], (D, 2 * D), D ** -0.5),
        "b_ada_final": nrm(ks[16], (2 * D,), 0.02),
        "norm_final_g": 1.0 + nrm(ks[17], (D,), 0.02),
    }


def reference(x, c, w_ada, b_ada, norm_mix_g, w_in, b_fgate, gn_sb_g, gn_fox_g, w_out,
              norm_ffn_g, w_query, sub_keys, expert_u, expert_v, w_ada_final, b_ada_final,
              norm_final_g):
    c_act = jax.nn.silu(c)
    for l in range(DEPTH):
        mod = c_act @ w_ada[l] + b_ada[l]
        sh1, sc1, g1, sh2, sc2, g2 = jnp.split(mod, N_MOD, axis=-1)

        h = modulate(rmsnorm(x, norm_mix_g[l]), sh1, sc1)
        proj = h @ w_in[l]
        o = 0
        q_sb = proj[..., o:o + SB_WIDTH]; o += SB_WIDTH
        k_sb = proj[..., o:o + SB_WIDTH]; o += SB_WIDTH
        v_sb = proj[..., o:o + SB_WIDTH]; o += SB_WIDTH
        q_fx = proj[..., o:o + FOX_WIDTH]; o += FOX_WIDTH
        k_fx = proj[..., o:o + FOX_WIDTH]; o += FOX_WIDTH
        v_fx = proj[..., o:o + FOX_WIDTH]; o += FOX_WIDTH
        f_logit = proj[..., o:o + FOX_HEADS]

        o_sb = stick_breaking_attention(split_heads(q_sb, SB_HEADS), split_heads(k_sb, SB_HEADS),
                                        split_heads(v_sb, SB_HEADS))
        log_f = jax.nn.log_sigmoid((f_logit + b_fgate[l]).astype(jnp.float32)).transpose(0, 2, 1)
        o_fx = forgetting_attention(split_heads(q_fx, FOX_HEADS), split_heads(k_fx, FOX_HEADS),
                                    split_heads(v_fx, FOX_HEADS), log_f)
        o_sb = headwise_rmsnorm(o_sb, gn_sb_g[l])
        o_fx = headwise_rmsnorm(o_fx, gn_fox_g[l])
        b, _, s, _ = o_sb.shape
        merged = jnp.concatenate([o_sb.transpose(0, 2, 1, 3).reshape(b, s, SB_WIDTH),
                                  o_fx.transpose(0, 2, 1, 3).reshape(b, s, FOX_WIDTH)], axis=-1)
        x = x + g1[:, None, :] * (merged @ w_out[l])

        h2 = modulate(rmsnorm(x, norm_ffn_g[l]), sh2, sc2)
        x = x + g2[:, None, :] * peer_ffn(h2, w_query[l], sub_keys[l], expert_u[l], expert_v[l])

    modf = c_act @ w_ada_final + b_ada_final
    shf, scf = jnp.split(modf, 2, axis=-1)
    return modulate(rmsnorm(x, norm_final_g), shf, scf)
```

```python
import contextlib
import math

import numpy as np
import concourse.bass as bass
import concourse.mybir as mybir
from concourse.bass_utils import run_bass_kernel_spmd

F32 = mybir.dt.float32
BF16 = mybir.dt.bfloat16
I32 = mybir.dt.int32
U32 = mybir.dt.uint32
AF = mybir.ActivationFunctionType
ALU = mybir.AluOpType
AX = mybir.AxisListType

D = 1024
EPS = 1e-6
N_CORES = 8
SEQ = 4096
NEG = -1.0e30


class _Op:
    __slots__ = ("eng", "fn", "deps", "dma", "semkey", "signal", "semval", "waits")

    def __init__(self, eng, fn, deps, dma, semkey):
        self.eng = eng
        self.fn = fn
        self.deps = deps
        self.dma = dma
        self.semkey = semkey
        self.signal = False
        self.semval = 0
        self.waits = ()


class Prog:
    ENGS = ("pe", "act", "dve", "pool", "sp")

    def __init__(self, nc):
        self.nc = nc
        self.ops = []
        self.last_w = {}
        self.readers = {}
        self.pending = {e: set() for e in self.ENGS}
        self.last_eng = {}
        self.last_dma = {}

    def op(self, eng, fn, r=(), w=(), dma=False, key=None):
        idx = len(self.ops)
        deps = set()
        for res in r:
            lw = self.last_w.get(res)
            if lw is not None:
                deps.add(lw)
        for res in w:
            lw = self.last_w.get(res)
            if lw is not None:
                deps.add(lw)
            deps.update(self.readers.get(res, ()))
        if self.pending[eng]:
            deps |= self.pending[eng]
            self.pending[eng] = set()
        for res in r:
            self.readers.setdefault(res, []).append(idx)
        for res in w:
            self.last_w[res] = idx
            self.readers[res] = []
        semkey = None
        if dma:
            semkey = key if key is not None else (w[0] if w else ("dma", idx))
            self.last_dma[semkey] = idx
        else:
            self.last_eng[eng] = idx
        self.ops.append(_Op(eng, fn, deps, dma, semkey))
        return idx

    def barrier(self):
        deps = set(self.last_eng.values()) | set(self.last_dma.values())
        for e in self.ENGS:
            self.pending[e] |= deps
        self.last_w.clear()
        self.readers.clear()

    def emit(self, final_wait_keys=()):
        nc = self.nc
        ops = self.ops
        cnt = {e: 0 for e in self.ENGS}
        for o in ops:
            best = {}
            keep = set()
            for d in o.deps:
                p = ops[d]
                if p.dma:
                    keep.add(d)
                else:
                    if p.eng == "pe" and o.eng == "pe" and not o.dma:
                        continue
                    if p.eng not in best or best[p.eng] < d:
                        best[p.eng] = d
            keep.update(best.values())
            o.deps = keep
            for d in keep:
                ops[d].signal = True
        dma_cnt = {}
        dma_keys = []
        for o in ops:
            waits = {}
            for d in o.deps:
                p = ops[d]
                if p.dma:
                    waits[("d", p.semkey)] = dma_cnt[p.semkey]
                else:
                    k = ("e", p.eng)
                    if waits.get(k, 0) < p.semval:
                        waits[k] = p.semval
            o.waits = waits
            if o.dma:
                if o.semkey not in dma_cnt:
                    dma_cnt[o.semkey] = 0
                    dma_keys.append(o.semkey)
                dma_cnt[o.semkey] += 16
                o.semval = dma_cnt[o.semkey]
                o.signal = True
            elif o.signal:
                cnt[o.eng] += 1
                o.semval = cnt[o.eng]
        with contextlib.ExitStack() as st:
            esem = {e: st.enter_context(nc.semaphore("s_" + e)) for e in self.ENGS}
            dsem = {}
            for k in dma_keys:
                dsem[k] = st.enter_context(nc.semaphore("d%d" % len(dsem)))
            block = st.enter_context(nc.Block())

            def run(engname, e):
                waited = {}
                for o in ops:
                    if o.eng != engname:
                        continue
                    for k, v in o.waits.items():
                        if waited.get(k, 0) >= v:
                            continue
                        s = dsem[k[1]] if k[0] == "d" else esem[k[1]]
                        e.wait_ge(s, v)
                        waited[k] = v
                    inst = o.fn(e)
                    if o.signal:
                        if o.dma:
                            inst.then_inc(dsem[o.semkey], 16)
                        else:
                            inst.then_inc(esem[o.eng], 1)
                if engname == "sp":
                    for k in final_wait_keys:
                        e.wait_ge(dsem[k], dma_cnt[k])

            @block.tensor
            def _(e):
                run("pe", e)

            @block.scalar
            def _(e):
                run("act", e)

            @block.vector
            def _(e):
                run("dve", e)

            @block.gpsimd
            def _(e):
                run("pool", e)

            @block.sync
            def _(e):
                run("sp", e)
        return len(dsem), dict(cnt)


def I(name, *args, **kwargs):
    return lambda e: getattr(e, name)(*args, **kwargs)


class Arena:
    def __init__(self, ap, nwords):
        self.ap = ap
        self.n = nwords
        self.off = 0
        self.marks = []

    def f32(self, n):
        assert self.off + n <= self.n, ("arena overflow", self.off, n, self.n)
        a = self.ap[:, self.off:self.off + n]
        self.off += n
        return a

    def b16(self, n):
        assert n % 2 == 0
        return self.f32(n // 2).bitcast(BF16)

    def i32(self, n):
        return self.f32(n).bitcast(I32)

    def u32(self, n):
        return self.f32(n).bitcast(U32)

    def mark(self):
        self.marks.append(self.off)

    def release(self):
        self.off = self.marks.pop()


def build(S=SEQ, dbg=False, n_tok_tiles_c=None):
    NT = S // 128
    NCH = S // 512
    nc = bass.Bass("TRN2", target_bir_lowering=False)

    def din(name, shape, dt=F32):
        return nc.dram_tensor(name, list(shape), dt, kind="ExternalInput").ap()

    x = din("x", [S, D])
    cT = din("cT", [128, 8])
    w_ada = din("w_ada", [D, 6 * D])
    b_ada = din("b_ada", [1, 6 * D])
    norm_mix_g = din("norm_mix_g", [1, D])
    w_in = din("w_in", [D, 3080])
    b_fgate = din("b_fgate", [8, 1])
    gn = din("gn", [128, 8])
    w_out = din("w_out", [D, D])
    norm_ffn_g = din("norm_ffn_g", [1, D])
    w_query = din("w_query", [D, 2048])
    sub_keys = din("sub_keys", [16, 128, 128])
    expert_uv = din("expert_uv", [16384, 2 * D])
    w_ada_final = din("w_ada_final", [D, 2 * D])
    b_ada_final = din("b_ada_final", [1, 2 * D])
    norm_final_g = din("norm_final_g", [1, D])
    out = nc.dram_tensor("out", [S, D], F32, kind="ExternalOutput").ap()

    def dscr(name, shape, dt=BF16):
        return nc.dram_tensor(name, list(shape), dt, kind=("ExternalOutput" if dbg else "Internal")).ap()

    dbg_t = {}
    if dbg:
        for nm, shp, dt in (("d_mod", [128, 8 * D], F32), ("d_x1", [128, D], F32), ("d_h2", [128, D], BF16),
                            ("d_eidx", [128, 128], I32), ("d_gates", [128, 128], F32), ("d_araw", [128, 128], F32),
                            ("d_sc", [128, 2048], F32), ("d_peer", [128, D], F32), ("d_m16", [128, 256], F32)):
            dbg_t[nm] = nc.dram_tensor(nm, shp, dt, kind="ExternalOutput").ap()

    qT_d = dscr("qT_d", [8, 128, S])
    kT_d = dscr("kT_d", [8, 128, S])
    v_d = dscr("v_d", [8, 128, NT, 128])
    aug_d = dscr("aug_d", [8, 2, 6, S])
    md_d = dscr("md_d", [8, 128, S])
    uvb = nc.dram_tensor("uvb", [16384, 2 * D], BF16, kind="Internal").ap()

    P = Prog(nc)
    ARENA_WORDS = 53000
    with contextlib.ExitStack() as st:
        arena_t = st.enter_context(nc.sbuf_tensor("arena", [128, ARENA_WORDS], F32))
        A = Arena(arena_t, ARENA_WORDS)
        ps = [st.enter_context(nc.psum_tensor("ps%d" % i, [128, 512], F32))[:, :] for i in range(8)]
        psr = ["ps%d" % i for i in range(8)]

        ones_f = A.f32(128)
        L_f = A.f32(128)
        bd_f = A.f32(128)
        mstrict_f = A.f32(128)
        ident_b = A.b16(128)
        ones_b = A.b16(128)
        mincl_b = A.b16(128)
        iota16 = A.f32(16)
        gn_sb = A.f32(8)
        nbfg = A.f32(1)
        MOD = A.f32(6 * D)
        MODF = A.f32(2 * D)
        B1, A1, G1 = MOD[:, 0:D], MOD[:, D:2 * D], MOD[:, 2 * D:3 * D]
        B2, A2, G2 = MOD[:, 3 * D:4 * D], MOD[:, 4 * D:5 * D], MOD[:, 5 * D:6 * D]
        BFr, AFr = MODF[:, 0:D], MODF[:, D:2 * D]
        WQ_OFF = ARENA_WORDS - 8192
        wq_b = arena_t[:, WQ_OFF:ARENA_WORDS].bitcast(BF16).rearrange("p (k n) -> p k n", k=8)
        cv_off = A.off
        cvb = [A.b16(2 * 2 * D).rearrange("p (r n) -> p r n", r=2) for _ in range(2)]
        uv_v = expert_uv.rearrange("(p r) n -> p r n", p=128)
        uvb_v = uvb.rearrange("(p r) n -> p r n", p=128)
        conv_state = {"i": 0}

        def conv_step(n):
            for _ in range(n):
                i = conv_state["i"]
                if i > 0 and i <= 64:
                    j = i - 1
                    P.op("pool", I("dma_start", out=uvb_v[:, 2 * j:2 * j + 2, :], in_=cvb[j % 2]),
                         r=[("cvb", j % 2)], w=[("uvb", j)], dma=True, key=("cvs", j % 2))
                if i < 64:
                    P.op("pool", I("dma_start", out=cvb[i % 2], in_=uv_v[:, 2 * i:2 * i + 2, :]),
                         w=[("cvb", i % 2)], dma=True, key=("cvl", i % 2))
                conv_state["i"] = i + 1

        P.op("pool", I("memset", ones_f, 1.0), w=["ones_f"])
        P.op("pool", I("memset", ones_b, 1.0), w=["ones_b"])
        P.op("pool", I("affine_select", out=L_f, in_=ones_f, pattern=[[-1, 128]], compare_op=ALU.is_ge,
                                               fill=0.0, base=0, channel_multiplier=1), r=["ones_f"], w=["L_f"])
        P.op("pool", I("affine_select", out=mstrict_f, in_=ones_f, pattern=[[1, 128]], compare_op=ALU.is_gt,
                                               fill=0.0, base=0, channel_multiplier=-1), r=["ones_f"], w=["mstrict"])
        P.op("pool", I("affine_select", out=mincl_b, in_=ones_f, pattern=[[1, 128]], compare_op=ALU.is_ge,
                                               fill=0.0, base=0, channel_multiplier=-1), r=["ones_f"], w=["mincl"])
        P.op("pool", I("affine_select", out=ident_b, in_=ones_f, pattern=[[-1, 128]], compare_op=ALU.is_equal,
                                               fill=0.0, base=0, channel_multiplier=1), r=["ones_f"], w=["ident"])
        P.op("pool", I("memset", bd_f, 0.0), w=["bd"])
        P.op("pool", I("memset", bd_f[0:64, 0:64], 1.0), w=["bd"])
        P.op("pool", I("memset", bd_f[64:128, 64:128], 1.0), w=["bd"])
        A.mark()
        io_i = A.i32(16)
        P.op("pool", I("iota", io_i, pattern=[[1, 16]], base=0, channel_multiplier=0), w=["io_i"])
        P.op("dve", I("tensor_copy", out=iota16, in_=io_i), r=["io_i"], w=["iota16"])
        P.op("sp", I("dma_start", out=gn_sb, in_=gn), w=["gn"], dma=True)
        bfg = A.f32(1)
        P.op("sp", I("dma_start", out=bfg[0:8, :], in_=b_fgate), w=["bfg"], dma=True)
        P.op("dve", I("tensor_scalar", out=nbfg[0:8, :], in0=bfg[0:8, :], scalar1=-1.0, scalar2=None, op0=ALU.mult),
             r=["bfg"], w=["nbfg"])

        c_sb = A.f32(8)
        c_act = A.f32(8)
        cb = A.f32(8 * 128)
        cbv = cb.rearrange("p (k m) -> p k m", k=8)
        P.op("sp", I("dma_start", out=c_sb, in_=cT), w=["c_sb"], dma=True)
        P.op("act", I("activation", out=c_act, in_=c_sb, func=AF.Silu), r=["c_sb"], w=["c_act"])
        P.op("dve", I("tensor_copy", out=cbv, in_=c_act.unsqueeze(2).to_broadcast([128, 8, 128])),
             r=["c_act"], w=["cb"])
        wst = [A.f32(8 * 512), A.f32(8 * 512)]
        bst = [A.f32(512), A.f32(512)]
        gtmp = A.f32(D)
        blocks = [(w_ada, b_ada, MOD, i) for i in range(12)] + [(w_ada_final, b_ada_final, MODF, i) for i in range(4)]
        for bi, (wsrc, bsrc, dst, i) in enumerate(blocks):
            sb = bi % 2
            wv = wst[sb].rearrange("p (k n) -> p k n", k=8)
            P.op("sp", I("dma_start",
                out=wv, in_=wsrc.rearrange("(k p) n -> p k n", p=128)[:, :, i * 512:(i + 1) * 512]),
                w=[("wst", sb)], dma=True)
            P.op("act", I("dma_start",
                out=bst[sb], in_=bsrc[0:1, i * 512:(i + 1) * 512].partition_broadcast(128)),
                w=[("bst", sb)], dma=True)
            for k in range(8):
                P.op("pe", I("matmul", ps[sb], lhsT=cbv[:, k, :], rhs=wv[:, k, :],
                                                                 start=(k == 0), stop=(k == 7)),
                     r=["cb", ("wst", sb)], w=[psr[sb]])
            P.op("dve", I("tensor_tensor", out=dst[:, i * 512:(i + 1) * 512], in0=ps[sb],
                                                                      in1=bst[sb], op=ALU.add),
                 r=[psr[sb], ("bst", sb)], w=[("mod", bi)])
        for gsrc, row, deps in ((norm_mix_g, A1, (2, 3)), (norm_ffn_g, A2, (8, 9)), (norm_final_g, AFr, (14, 15))):
            P.op("sp", I("dma_start", out=gtmp, in_=gsrc.partition_broadcast(128)), w=["gtmp"], dma=True)
            P.op("dve", I("scalar_tensor_tensor", out=row, in0=row, scalar=1.0, in1=gtmp,
                                                                 op0=ALU.add, op1=ALU.mult),
                 r=["gtmp"] + [("mod", d) for d in deps], w=[("mod", d) for d in deps])
        if dbg:
            P.op("sp", I("dma_start", out=dbg_t["d_mod"][:, 0:6 * D], in_=MOD), r=[("mod", i) for i in range(16)], w=["dbg_mod"], dma=True)
            P.op("sp", I("dma_start", out=dbg_t["d_mod"][:, 6 * D:8 * D], in_=MODF), r=[("mod", i) for i in range(16)], w=["dbg_mod"], dma=True)
        P.barrier()
        A.release()

        A.mark()
        w_in_b = A.b16(8 * 3080).rearrange("p (k n) -> p k n", k=8)
        xt = [A.f32(D) for _ in range(8)]
        junkf = A.f32(D)
        tmpf = A.f32(D)
        hb = [A.b16(D), A.b16(D)]
        hT = [A.b16(8 * 512).rearrange("p (k t) -> p k t", k=8) for _ in range(2)]
        ssA = A.f32(4)
        qkst = [A.b16(512) for _ in range(4)]
        vst = [A.b16(D) for _ in range(2)]
        spf = A.f32(512)
        ef = A.f32(512)
        Gs = [A.f32(512), A.f32(512)]
        r1 = A.f32(512)
        r2 = A.f32(512)
        ones8 = A.f32(512)
        augst = [A.b16(2 * 6 * 512).rearrange("p (q r s) -> p q r s", q=2, r=6) for _ in range(2)]
        for k in range(8):
            for (c0, c1) in ((0, 1024), (1024, 2048), (2048, 3080)):
                P.op("pool", I("dma_start", out=w_in_b[:, k, c0:c1],
                                                                       in_=w_in[k * 128:(k + 1) * 128, c0:c1]),
                     w=[("w_in", k, c0)], dma=True, key="w_in")
        w_in_res = [("w_in", k, c0) for k in range(8) for c0 in (0, 1024, 2048)]
        P.op("pool", I("memset", ones8[0:8, :], 1.0), w=["ones8"])
        for b in range(2):
            P.op("pool", I("memset", augst[b][0:8, 0, 3:6, :], 1.0), w=[("augst", b)])
            P.op("pool", I("memset", augst[b][0:8, 1, 0:3, :], 1.0), w=[("augst", b)])
        psT_b = ps[7].bitcast(BF16)

        def qk_col(g):
            pair, isk = g % 8, g // 8
            if pair < 4:
                return (512 if isk else 0) + pair * 128
            return (2048 if isk else 1536) + (pair - 4) * 128

        def x_loads(ch):
            for tt in range(4):
                t = ch * 4 + tt
                P.op("sp", I("dma_start", out=xt[t % 8], in_=x[t * 128:(t + 1) * 128, :]), w=[("xt", t % 8)], dma=True)

        x_loads(0)
        for ch in range(NCH):
            hb_ = ch % 2
            if ch + 1 < NCH:
                x_loads(ch + 1)
            for tt in range(4):
                t = ch * 4 + tt
                xb_ = t % 8
                P.op("act", I("activation", out=junkf, in_=xt[xb_], func=AF.Square, accum_out=ssA[:, 0:1]),
                     r=[("xt", xb_)], w=["junkf", "ssA0"])
                P.op("act", I("activation", out=ssA[:, 1:2], in_=ssA[:, 0:1], func=AF.Sqrt, scale=1.0 / D, bias=EPS),
                     r=["ssA0"], w=["ssA1"])
                P.op("dve", I("reciprocal", out=ssA[:, 2:3], in_=ssA[:, 1:2]), r=["ssA1"], w=["ssA2"])
                P.op("dve", I("scalar_tensor_tensor", out=tmpf, in0=xt[xb_], scalar=ssA[:, 2:3], in1=A1,
                                                                     op0=ALU.mult, op1=ALU.mult),
                     r=[("xt", xb_), "ssA2"], w=["tmpf"])
                P.op("dve", I("tensor_tensor", out=hb[t % 2], in0=tmpf, in1=B1, op=ALU.add),
                     r=["tmpf"], w=[("hb", t % 2)])
                for k in range(8):
                    P.op("pe", I("transpose", out=psT_b[:, k * 128:(k + 1) * 128],
                                                                   in_=hb[t % 2][:, k * 128:(k + 1) * 128], identity=ident_b),
                         r=[("hb", t % 2), "ident"], w=[psr[7]])
                P.op("act", I("copy", out=hT[hb_][:, :, tt * 128:(tt + 1) * 128],
                                                             in_=psT_b.rearrange("p (k t) -> p k t", k=8)),
                     r=[psr[7]], w=[("hT", hb_, tt)])
            hres = [("hT", hb_, tt) for tt in range(4)]
            for g in range(16):
                pair, isk = g % 8, g // 8
                col0 = qk_col(g)
                bank = g % 2
                sb_ = g % 4
                for k in range(8):
                    P.op("pe", I("matmul",
                        ps[bank], lhsT=w_in_b[:, k, col0:col0 + 128], rhs=hT[hb_][:, k, :], start=(k == 0), stop=(k == 7)),
                        r=hres + w_in_res, w=[psr[bank]])
                if g % 2 == 0:
                    P.op("act", I("activation",
                        out=qkst[sb_], in_=ps[bank], func=AF.Copy, scale=(1.0 if isk else 0.125)),
                        r=[psr[bank]], w=[("qkst", sb_)])
                else:
                    P.op("dve", I("tensor_scalar",
                        out=qkst[sb_], in0=ps[bank], scalar1=(1.0 if isk else 0.125), scalar2=None, op0=ALU.mult),
                        r=[psr[bank]], w=[("qkst", sb_)])
                dst = (kT_d if isk else qT_d)[pair, :, ch * 512:(ch + 1) * 512]
                P.op("sp", I("dma_start", out=dst, in_=qkst[sb_]),
                     r=[("qkst", sb_)], w=[("qk_d", g, ch)], dma=True, key=("qkd", sb_))
            for tt in range(4):
                t = ch * 4 + tt
                vb_ = t % 2
                for vb in range(2):
                    c0 = 1024 if vb == 0 else 2560
                    bank = 2 + vb
                    for k in range(8):
                        P.op("pe", I("matmul",
                            ps[bank], lhsT=hT[hb_][:, k, tt * 128:(tt + 1) * 128], rhs=w_in_b[:, k, c0:c0 + 512],
                            start=(k == 0), stop=(k == 7)),
                            r=hres + w_in_res, w=[psr[bank]])
                    if vb == 0:
                        P.op("act", I("copy", out=vst[vb_][:, 0:512], in_=ps[bank]),
                             r=[psr[bank]], w=[("vst", vb_, 0)])
                    else:
                        P.op("dve", I("tensor_copy", out=vst[vb_][:, 512:1024], in_=ps[bank]),
                             r=[psr[bank]], w=[("vst", vb_, 1)])
                P.op("sp", I("dma_start",
                    out=v_d.rearrange("a p n c -> p a n c")[:, :, t, :],
                    in_=vst[vb_].rearrange("p (a c) -> p a c", a=8)),
                    r=[("vst", vb_, 0), ("vst", vb_, 1)], w=[("v_d", t)], dma=True, key=("vd", vb_))
            for k in range(8):
                P.op("pe", I("matmul", ps[4][0:8, :], lhsT=w_in_b[:, k, 3072:3080], rhs=hT[hb_][:, k, :],
                                                            start=(k == 0), stop=(k == 7)),
                     r=hres + w_in_res, w=[psr[4]])
            P.op("act", I("activation", out=ef[0:8, :], in_=ps[4][0:8, :], func=AF.Exp, scale=-1.0, bias=nbfg[0:8, :]),
                 r=[psr[4], "nbfg"], w=["ef"])
            P.op("act", I("activation", out=spf[0:8, :], in_=ef[0:8, :], func=AF.Ln, bias=1.0), r=["ef"], w=["spf"])
            gb = ch % 2
            if ch == 0:
                P.op("dve", I("tensor_tensor_scan", out=Gs[gb][0:8, :], data0=ones8[0:8, :], data1=spf[0:8, :],
                                                                 initial=0.0, op0=ALU.mult, op1=ALU.add),
                     r=["ones8", "spf"], w=[("Gs", gb)])
            else:
                P.op("dve", I("tensor_tensor_scan", out=Gs[gb][0:8, :], data0=ones8[0:8, :], data1=spf[0:8, :],
                                                                 initial=Gs[1 - gb][0:8, 511:512], op0=ALU.mult, op1=ALU.add),
                     r=["ones8", "spf", ("Gs", 1 - gb)], w=[("Gs", gb)])
            ab = augst[gb]
            ar = ("augst", gb)
            P.op("dve", I("tensor_copy", out=ab[0:8, 1, 3, :], in_=Gs[gb][0:8, :]), r=[("Gs", gb)], w=[ar])
            P.op("dve", I("tensor_tensor", out=r1[0:8, :], in0=Gs[gb][0:8, :], in1=ab[0:8, 1, 3, :],
                                                               op=ALU.subtract), r=[("Gs", gb), ar], w=["r1"])
            P.op("dve", I("tensor_copy", out=ab[0:8, 1, 4, :], in_=r1[0:8, :]), r=["r1"], w=[ar])
            P.op("dve", I("tensor_tensor", out=r2[0:8, :], in0=r1[0:8, :], in1=ab[0:8, 1, 4, :], op=ALU.subtract),
                 r=["r1", ar], w=["r2"])
            P.op("dve", I("tensor_copy", out=ab[0:8, 1, 5, :], in_=r2[0:8, :]), r=["r2"], w=[ar])
            P.op("dve", I("tensor_scalar", out=ab[0:8, 0, 0:3, :], in0=ab[0:8, 1, 3:6, :], scalar1=-1.0,
                                                         scalar2=None, op0=ALU.mult), r=[ar], w=[ar])
            P.op("sp", I("dma_start", out=aug_d[:, :, :, ch * 512:(ch + 1) * 512], in_=ab[0:8, :, :, :]),
                 r=[ar], w=[("aug_d", ch)], dma=True, key=("augd", gb))
        P.barrier()
        A.release()

        A.mark()
        qTp = [A.b16(S) for _ in range(2)]
        kTp = [A.b16(S) for _ in range(2)]
        Vp = [A.b16(NT * 128).rearrange("p (n c) -> p n c", c=128) for _ in range(2)]
        augt = [A.b16(2 * S).rearrange("p (q s) -> p q s", q=2) for _ in range(2)]
        ebuf = [[A.f32(512) for _ in range(2)] for _ in range(2)]
        spbuf = [[A.f32(512) for _ in range(2)] for _ in range(2)]
        rbuf = [A.f32(512) for _ in range(2)]
        abuf = [[A.b16(512) for _ in range(3)] for _ in range(2)]
        Sacc = [A.f32(512) for _ in range(2)]
        Shi = [A.b16(512) for _ in range(2)]
        Slo = [A.b16(512) for _ in range(2)]
        on_sb = A.f32(512)
        sq_sb = A.f32(512)
        rt_sb = A.f32(512)
        mst = [A.b16(512) for _ in range(2)]
        ZB = (0, 1)
        IB = (2, 3)
        OB = (4, 5)
        LB = (6, 7)

        def pair_loads(pair):
            fox = pair >= 4
            sb = pair % 2
            if not fox:
                P.op("sp", I("dma_start", out=qTp[sb], in_=qT_d[pair]), w=[("qTp", sb, 0), ("qTp", sb, 1)], dma=True,
                     key=("qTp", sb))
                P.op("sp", I("dma_start", out=kTp[sb], in_=kT_d[pair]), w=[("kTp", sb, 0), ("kTp", sb, 1)], dma=True,
                     key=("kTp", sb))
            else:
                hA, hB = 2 * (pair - 4), 2 * (pair - 4) + 1
                for (dst, src_d, qk, nm) in ((qTp[sb], qT_d, 0, "qTp"), (kTp[sb], kT_d, 1, "kTp")):
                    P.op("sp", I("dma_start", out=dst[0:64, :], in_=src_d[pair, 0:64, :]), w=[(nm, sb, 0)], dma=True,
                         key=(nm, sb, "m"))
                    P.op("pool", I("memset", dst[64:128, :], 0.0), w=[(nm, sb, 1)])
                    P.op("sp", I("dma_start", out=dst[64:70, :], in_=aug_d[hA, qk]), w=[(nm, sb, 1)], dma=True,
                         key=(nm, sb, "a"))
                for (src_d, qk) in ((qT_d, 0), (kT_d, 1)):
                    P.op("sp", I("dma_start", out=augt[sb][0:64, qk, :], in_=src_d[pair, 64:128, :]),
                         w=[("augt", sb, qk, 0)], dma=True, key=("augt", sb, qk, "m"))
                    P.op("pool", I("memset", augt[sb][64:128, qk, :], 0.0), w=[("augt", sb, qk, 1)])
                    P.op("sp", I("dma_start", out=augt[sb][64:70, qk, :], in_=aug_d[hB, qk]),
                         w=[("augt", sb, qk, 1)], dma=True, key=("augt", sb, qk, "a"))
            P.op("sp", I("dma_start", out=Vp[sb], in_=v_d[pair]), w=[("Vp", sb)], dma=True)

        pending_epi = []

        def flush_epi(part=None):
            for epi in pending_epi:
                k = next(i for i, (a, kw) in enumerate(epi) if a[0] == "pe") if epi else 0
                if part in (0, None):
                    for a, kw in epi[:k]:
                        P.op(*a, **kw)
                    del epi[:k]
                if part in (1, None):
                    for a, kw in epi:
                        P.op(*a, **kw)
                    del epi[:]
            if part in (1, None):
                del pending_epi[:]

        pair_loads(0)
        for pair in range(8):
            fox = pair >= 4
            sb = pair % 2
            if pair + 1 < 8:
                pair_loads(pair + 1)
            v_r = ("Vp", sb)
            for c in range(NCH):
                nk = 4 * (c + 1)
                order = list(range(nk)) if fox else list(range(nk - 1, -1, -1))
                gc = pair * NCH + c
                ob = OB[gc % 2]
                lb = LB[gc % 2]
                conv_step(-(-65 // (8 * NCH)))
                if pair == 7 and c == 0:
                    assert A.off <= WQ_OFF, ("phase B overlaps wq_b", A.off, WQ_OFF)
                    for k in range(8):
                        for h in range(2):
                            P.op("pool", I("dma_start", out=wq_b[:, k, h * 1024:(h + 1) * 1024],
                                           in_=w_query[k * 128:(k + 1) * 128, h * 1024:(h + 1) * 1024]),
                                 w=[("wq", k, h)], dma=True, key="wq")
                if not fox:
                    for hh in range(2):
                        P.op("pool", I("memset", Sacc[hh], 0.0), w=[("S", hh)])

                def rng(kb):
                    j = kb - 4 * c
                    q0 = 128 * j if j > 0 else 0
                    return j, q0

                def emit_qk(i):
                    kb = order[i]
                    j, q0 = rng(kb)
                    for hh in range(2):
                        pb = 64 * hh
                        zb = (ZB[hh], IB[hh])[i % 2] if fox else ZB[hh]
                        if not fox:
                            P.op("pe", I("matmul", ps[zb][:, q0:512], lhsT=kTp[sb][pb:pb + 64, kb * 128:(kb + 1) * 128],
                                         rhs=qTp[sb][pb:pb + 64, c * 512 + q0:(c + 1) * 512], start=True, stop=True),
                                 r=[("qTp", sb, 0), ("qTp", sb, 1), ("kTp", sb, 0), ("kTp", sb, 1)], w=[psr[zb]])
                        else:
                            qs = qTp[sb] if hh == 0 else augt[sb][:, 0, :]
                            ks = kTp[sb] if hh == 0 else augt[sb][:, 1, :]
                            rr = ([("qTp", sb, 0), ("qTp", sb, 1), ("kTp", sb, 0), ("kTp", sb, 1)] if hh == 0 else
                                  [("augt", sb, 0, 0), ("augt", sb, 0, 1), ("augt", sb, 1, 0), ("augt", sb, 1, 1)])
                            P.op("pe", I("matmul", ps[zb][:, q0:512], lhsT=ks[:, kb * 128:(kb + 1) * 128],
                                         rhs=qs[:, c * 512 + q0:(c + 1) * 512], start=True, stop=True),
                                 r=rr, w=[psr[zb]])

                def emit_exp(i):
                    kb = order[i]
                    j, q0 = rng(kb)
                    for hh in range(2):
                        zb = (ZB[hh], IB[hh])[i % 2] if fox else ZB[hh]
                        if fox:
                            pbuf = abuf[hh][i % 3]
                            pr = ("abuf", hh, i % 3)
                            P.op("act", I("activation",
                                out=pbuf[:, q0:512], in_=ps[zb][:, q0:512], func=AF.Exp), r=[psr[zb]], w=[pr])
                            if j >= 0:
                                P.op("pool", I("affine_select", out=pbuf[:, q0:q0 + 128], in_=pbuf[:, q0:q0 + 128],
                                               pattern=[[1, 128]], compare_op=ALU.is_ge, fill=0.0, base=0,
                                               channel_multiplier=-1), r=[pr], w=[pr])
                        else:
                            eb = ebuf[hh][i % 2]
                            er = ("ebuf", hh, i % 2)
                            P.op("act", I("activation",
                                out=eb[:, q0:512], in_=ps[zb][:, q0:512], func=AF.Exp), r=[psr[zb]], w=[er])
                            if j >= 0:
                                P.op("dve", I("tensor_tensor",
                                    out=eb[:, q0:q0 + 128], in0=eb[:, q0:q0 + 128], in1=mstrict_f, op=ALU.mult),
                                    r=[er, "mstrict"], w=[er])

                def emit_ln(i):
                    kb = order[i]
                    j, q0 = rng(kb)
                    for hh in range(2):
                        eb, sp_ = ebuf[hh][i % 2], spbuf[hh][i % 2]
                        P.op("act", I("activation",
                            out=sp_[:, q0:512], in_=eb[:, q0:512], func=AF.Ln, bias=1.0),
                            r=[("ebuf", hh, i % 2)], w=[("spbuf", hh, i % 2)])

                def emit_cum(i):
                    kb = order[i]
                    j, q0 = rng(kb)
                    for hh in range(2):
                        sp_ = spbuf[hh][i % 2]
                        ib = IB[hh]
                        P.op("pe", I("matmul",
                            ps[ib][:, q0:512], lhsT=L_f, rhs=sp_[:, q0:512], start=True, stop=(i == 0)),
                            r=[("spbuf", hh, i % 2), "L_f"], w=[psr[ib]])
                        if i > 0:
                            P.op("dve", I("tensor_copy", out=Shi[hh][:, q0:512], in_=Sacc[hh][:, q0:512]),
                                 r=[("S", hh)], w=[("Shi", hh)])
                            P.op("dve", I("tensor_tensor", out=Slo[hh][:, q0:512], in0=Sacc[hh][:, q0:512],
                                          in1=Shi[hh][:, q0:512], op=ALU.subtract),
                                 r=[("S", hh), ("Shi", hh)], w=[("Slo", hh)])
                            P.op("pe", I("matmul", ps[ib][:, q0:512], lhsT=ones_b, rhs=Shi[hh][:, q0:512],
                                         start=False, stop=False), r=[("Shi", hh), "ones_b"], w=[psr[ib]])
                            P.op("pe", I("matmul", ps[ib][:, q0:512], lhsT=ones_b, rhs=Slo[hh][:, q0:512],
                                         start=False, stop=True), r=[("Slo", hh), "ones_b"], w=[psr[ib]])
                        if i < nk - 1:
                            P.op("pool", I("tensor_tensor",
                                out=Sacc[hh][:, q0:512], in0=Sacc[hh][:, q0:512], in1=sp_[:, q0:512], op=ALU.add),
                                r=[("spbuf", hh, i % 2), ("S", hh)], w=[("S", hh)])

                def emit_neg(i):
                    kb = order[i]
                    j, q0 = rng(kb)
                    for hh in range(2):
                        ib = IB[hh]
                        P.op("act", I("activation",
                            out=rbuf[hh][:, q0:512], in_=ps[ib][:, q0:512], func=AF.Exp, scale=-1.0),
                            r=[psr[ib]], w=[("rbuf", hh)])
                        eb, ab_ = ebuf[hh][i % 2], abuf[hh][i % 3]
                        P.op("dve", I("tensor_tensor",
                            out=ab_[:, q0:512], in0=eb[:, q0:512], in1=rbuf[hh][:, q0:512], op=ALU.mult),
                            r=[("ebuf", hh, i % 2), ("rbuf", hh)], w=[("abuf", hh, i % 3)])

                def emit_av(i):
                    kb = order[i]
                    j, q0 = rng(kb)
                    for hh in range(2):
                        pb = 64 * hh
                        ab_ = abuf[hh][i % 3]
                        P.op("pe", I("matmul",
                            ps[ob][pb:pb + 64, q0:512], lhsT=Vp[sb][:, kb, pb:pb + 64], rhs=ab_[:, q0:512],
                            start=(i == 0), stop=(i == nk - 1), skip_group_check=True),
                            r=[("abuf", hh, i % 3), v_r], w=[(psr[ob], hh)])
                        if fox:
                            P.op("pe", I("matmul",
                                ps[lb][pb:pb + 64, q0:512], lhsT=ones_b[:, 0:64], rhs=ab_[:, q0:512],
                                start=(i == 0), stop=(i == nk - 1)),
                                r=[("abuf", hh, i % 3), "ones_b"], w=[(psr[lb], hh)])

                emit_qk(0)
                for i in range(nk):
                    emit_exp(i)
                    if i + 1 < nk:
                        emit_qk(i + 1)
                    if i == 0:
                        flush_epi(0)
                    if i == 2:
                        flush_epi(1)
                    if fox:
                        if i > 0:
                            emit_av(i - 1)
                    else:
                        emit_ln(i)
                        if i > 0:
                            emit_neg(i - 1)
                        emit_cum(i)
                        if i > 0:
                            emit_av(i - 1)
                if not fox:
                    emit_neg(nk - 1)
                emit_av(nk - 1)

                epi = []
                o_r = [(psr[ob], 0), (psr[ob], 1)]
                l_r = [(psr[lb], 0), (psr[lb], 1)]
                if fox:
                    epi.append((("act", I("activation", out=rt_sb, in_=ps[lb], func=AF.Ln)), dict(r=l_r, w=["rt_sb"])))
                    epi.append((("act", I("activation", out=rt_sb, in_=rt_sb, func=AF.Exp, scale=-1.0)),
                                dict(r=["rt_sb"], w=["rt_sb"])))
                    epi.append((("dve", I("tensor_tensor", out=on_sb, in0=ps[ob], in1=rt_sb, op=ALU.mult)),
                                dict(r=o_r + ["rt_sb"], w=["on_sb"])))
                else:
                    epi.append((("act", I("copy", out=on_sb, in_=ps[ob])), dict(r=o_r, w=["on_sb"])))
                epi.append((("dve", I("tensor_tensor", out=sq_sb, in0=on_sb, in1=on_sb, op=ALU.mult)),
                            dict(r=["on_sb"], w=["sq_sb"])))
                epi.append((("pe", I("matmul", ps[lb], lhsT=bd_f, rhs=sq_sb, start=True, stop=True)),
                            dict(r=["sq_sb", "bd"], w=l_r)))
                epi.append((("act", I("activation", out=sq_sb, in_=ps[lb], func=AF.Ln, scale=1.0 / 64, bias=EPS)),
                            dict(r=l_r, w=["sq_sb"])))
                epi.append((("act", I("activation", out=rt_sb, in_=sq_sb, func=AF.Exp, scale=-0.5)),
                            dict(r=["sq_sb"], w=["rt_sb"])))
                mb = gc % 2
                epi.append((("dve", I("scalar_tensor_tensor", out=mst[mb], in0=on_sb, scalar=gn_sb[:, pair:pair + 1],
                                      in1=rt_sb, op0=ALU.mult, op1=ALU.mult)),
                            dict(r=["on_sb", "rt_sb", "gn"], w=[("mst", mb)])))
                epi.append((("sp", I("dma_start", out=md_d[pair, :, c * 512:(c + 1) * 512], in_=mst[mb])),
                            dict(r=[("mst", mb)], w=[("md_d", pair, c)], dma=True, key=("mdd", mb))))
                pending_epi.append(epi)
        flush_epi()
        conv_step(66)
        P.barrier()
        A.release()

        A.mark()
        A.off = cv_off
        w_out_b = A.b16(8 * D).rearrange("p (k n) -> p k n", k=8)
        skT = A.b16(16 * 128).rearrange("p (g n) -> p g n", g=16)
        NSL = 16
        GRP = 2
        NG = 128 // GRP
        UV = [A.b16(2 * D) for _ in range(NSL)]
        mdt = A.b16(8 * 128).rearrange("p (k t) -> p k t", k=8)
        xc = A.f32(D)
        x1 = [A.f32(D), MOD[:, D:2 * D]]
        tmpc = A.f32(D)
        tmpo = MOD[:, 0:D]
        h2b = [A.b16(D) for _ in range(2)]
        h2T = A.b16(8 * 128).rearrange("p (k t) -> p k t", k=8)
        sc_raw = A.f32(2048)
        sc = sc_raw.rearrange("p (g n) -> p g n", g=16)
        qb = sc_raw[:, 0:1024].bitcast(BF16)
        qpT = sc_raw[:, 1024:2048].bitcast(BF16).rearrange("p (g t) -> p g t", g=16)
        sc2s = [A.f32(128) for _ in range(2)]
        m16 = A.f32(256).rearrange("p (g k) -> p g k", g=16)
        i16 = A.u32(256).rearrange("p (g k) -> p g k", g=16)
        i16f = A.f32(256)
        cand = sc_raw.rearrange("p (h q) -> p h q", h=8)
        oh = A.f32(2048)
        cand2 = oh.rearrange("p (h q) -> p h q", h=8)
        bs = A.f32(128).rearrange("p (h k) -> p h k", h=8)
        bpos = A.u32(128)
        aidx = A.u32(128)
        bidx = A.u32(128)
        af_ = A.f32(128)
        bf_ = A.f32(128)
        i1s = A.f32(128)
        i2s = A.f32(128)
        eidf = A.f32(128)
        eidx = [A.i32(128) for _ in range(2)]
        gex = A.f32(128)
        gsm = A.f32(8)
        gates = [A.f32(128) for _ in range(2)]
        araw = [A.f32(128) for _ in range(2)]
        gl = [A.f32(128) for _ in range(2)]
        prod = [A.b16(D) for _ in range(2)]
        dgb = [A.b16(128) for _ in range(4)]
        ssC = A.f32(16)
        skst = sc_raw.rearrange("p (g c) -> p g c", g=16)
        assert A.off <= WQ_OFF, ("phase C overlaps wq_b", A.off, WQ_OFF)
        skb = oh[:, 0:1024].bitcast(BF16).rearrange("p (g c) -> p g c", g=16)

        for k in range(8):
            P.op("pool", I("dma_start", out=w_out_b[:, k, :], in_=w_out[k * 128:(k + 1) * 128, :]),
                 w=[("w_out", k)], dma=True, key="wc")
        wo_res = [("w_out", k) for k in range(8)]
        wq_res = []
        P.op("sp", I("dma_start", out=skst, in_=sub_keys.rearrange("g n c -> n g c")), w=["skst"], dma=True)
        P.op("dve", I("tensor_copy", out=skb, in_=skst), r=["skst"], w=["skb"])
        pT = [ps[4].bitcast(BF16), ps[5].bitcast(BF16)]
        for g in range(16):
            P.op("pe", I("transpose", out=pT[g // 8][:, (g % 8) * 128:(g % 8 + 1) * 128], in_=skb[:, g, :],
                                                  identity=ident_b), r=["skb", "ident"], w=[psr[4 + g // 8]])
        for hf in range(2):
            P.op("act", I("copy", out=skT[:, hf * 8:(hf + 1) * 8, :],
                                                in_=pT[hf].rearrange("p (g n) -> p g n", g=8)),
                 r=[psr[4 + hf]], w=[("skT", hf)])
        sk_res = [("skT", 0), ("skT", 1)]
        ntc = NT if n_tok_tiles_c is None else n_tok_tiles_c

        def fe_loads(t):
            P.op("sp", I("dma_start", out=xc, in_=x[t * 128:(t + 1) * 128, :]), w=["xc"], dma=True)
            P.op("sp", I("dma_start", out=mdt, in_=md_d.rearrange("a p s -> p a s")[:, :, t * 128:(t + 1) * 128]),
                 w=["mdt"], dma=True)

        def FE(t):
            b2 = t % 2
            x1r, h2r, er, gr = ("x1", b2), ("h2b", b2), ("eidx", b2), ("gates", b2)
            fops = []

            def Q(*a, **k):
                fops.append((a, k))
            for hf in range(2):
                for k in range(8):
                    Q("pe", I("matmul", ps[hf], lhsT=mdt[:, k, :], rhs=w_out_b[:, k, hf * 512:(hf + 1) * 512],
                                 start=(k == 0), stop=(k == 7)), r=["mdt"] + wo_res, w=[psr[hf]])
                Q("dve", I("tensor_tensor", out=tmpc[:, hf * 512:(hf + 1) * 512], in0=ps[hf],
                              in1=G1[:, hf * 512:(hf + 1) * 512], op=ALU.mult), r=[psr[hf]], w=[("tmpc", hf)])
            Q("dve", I("tensor_tensor", out=x1[b2], in0=tmpc, in1=xc, op=ALU.add),
                 r=[("tmpc", 0), ("tmpc", 1), "xc"], w=[x1r])
            Q("act", I("activation", out=tmpc, in_=x1[b2], func=AF.Square, accum_out=ssC[:, 0:1]),
                 r=[x1r], w=[("tmpc", 0), ("tmpc", 1), "ssC0"])
            Q("act", I("activation", out=ssC[:, 1:2], in_=ssC[:, 0:1], func=AF.Sqrt, scale=1.0 / D, bias=EPS),
                 r=["ssC0"], w=["ssC1"])
            Q("dve", I("reciprocal", out=ssC[:, 2:3], in_=ssC[:, 1:2]), r=["ssC1"], w=["ssC2"])
            Q("dve", I("scalar_tensor_tensor", out=tmpc, in0=x1[b2], scalar=ssC[:, 2:3], in1=A2,
                          op0=ALU.mult, op1=ALU.mult), r=[x1r, "ssC2"], w=[("tmpc", 0), ("tmpc", 1)])
            Q("dve", I("tensor_tensor", out=h2b[b2], in0=tmpc, in1=B2, op=ALU.add),
                 r=[("tmpc", 0), ("tmpc", 1)], w=[h2r])
            for k in range(8):
                Q("pe", I("transpose", out=pT[0][:, k * 128:(k + 1) * 128], in_=h2b[b2][:, k * 128:(k + 1) * 128],
                             identity=ident_b), r=[h2r, "ident"], w=[psr[4]])
            Q("act", I("copy", out=h2T, in_=pT[0].rearrange("p (k t) -> p k t", k=8)), r=[psr[4]], w=["h2T"])
            if dbg and t == 0:
                Q("sp", I("dma_start", out=dbg_t["d_x1"], in_=x1[b2]), r=[x1r], w=["dbg1"], dma=True, key="dbg")
                Q("sp", I("dma_start", out=dbg_t["d_h2"], in_=h2b[b2]), r=[h2r], w=["dbg2"], dma=True, key="dbg")
            for blk in range(4):
                bank = 4 + blk
                for k in range(8):
                    Q("pe", I("matmul", ps[bank], lhsT=h2T[:, k, :], rhs=wq_b[:, k, blk * 512:(blk + 1) * 512],
                                 start=(k == 0), stop=(k == 7)), r=["h2T"] + wq_res, w=[psr[bank]])
                if blk % 2 == 0:
                    Q("act", I("copy", out=qb[:, blk * 512:(blk + 1) * 512], in_=ps[bank]), r=[psr[bank]], w=[("qb", blk)])
                else:
                    Q("dve", I("tensor_copy", out=qb[:, blk * 512:(blk + 1) * 512], in_=ps[bank]),
                         r=[psr[bank]], w=[("qb", blk)])
            for g in range(16):
                Q("pe", I("transpose", out=pT[g // 8][:, (g % 8) * 128:(g % 8 + 1) * 128],
                             in_=qb[:, g * 128:(g + 1) * 128], identity=ident_b),
                     r=[("qb", g // 4), "ident"], w=[psr[4 + g // 8]])
            Q("act", I("copy", out=qpT[:, 0:8, :], in_=pT[0].rearrange("p (g n) -> p g n", g=8)),
                 r=[psr[4]], w=[("qpT", 0)])
            Q("dve", I("tensor_copy", out=qpT[:, 8:16, :], in_=pT[1].rearrange("p (g n) -> p g n", g=8)),
                 r=[psr[5]], w=[("qpT", 1)])
            for g in range(16):
                bank = 4 + g // 4
                Q("pe", I("matmul", ps[bank][:, (g % 4) * 128:(g % 4 + 1) * 128], lhsT=qpT[:, g, :], rhs=skT[:, g, :],
                             start=True, stop=True), r=[("qpT", g // 8)] + sk_res, w=[psr[bank]])
            for q4 in range(4):
                if q4 % 2 == 0:
                    Q("act", I("copy", out=sc[:, q4 * 4:(q4 + 1) * 4, :], in_=ps[4 + q4].rearrange("p (g n) -> p g n", g=4)),
                         r=[psr[4 + q4], ("qpT", 0), ("qpT", 1)] + [("qb", b_) for b_ in range(4)], w=[("sc", q4), "cand"])
                else:
                    Q("dve", I("tensor_copy", out=sc[:, q4 * 4:(q4 + 1) * 4, :],
                                  in_=ps[4 + q4].rearrange("p (g n) -> p g n", g=4)),
                         r=[psr[4 + q4], ("qpT", 0), ("qpT", 1)] + [("qb", b_) for b_ in range(4)], w=[("sc", q4), "cand"])
            for g in range(16):
                sr = ("sc", g // 4)
                s2 = sc2s[g % 2]
                s2r = ("sc2", g % 2)
                Q("dve", I("max", out=m16[:, g, 0:8], in_=sc[:, g, :]), r=[sr], w=[("m16a", g)])
                Q("dve", I("max_index", out=i16[:, g, 0:8], in_max=m16[:, g, 0:8], in_values=sc[:, g, :]),
                     r=[sr, ("m16a", g)], w=[("i16a", g)])
                Q("dve", I("match_replace", out=s2, in_to_replace=m16[:, g, 0:8], in_values=sc[:, g, :], imm_value=NEG),
                     r=[sr, ("m16a", g)], w=[s2r])
                Q("dve", I("max", out=m16[:, g, 8:16], in_=s2), r=[s2r], w=[("m16b", g)])
                Q("dve", I("max_index", out=i16[:, g, 8:16], in_max=m16[:, g, 8:16], in_values=s2),
                     r=[s2r, ("m16b", g)], w=[("i16b", g)])
            m_all = [("m16a", g) for g in range(16)] + [("m16b", g) for g in range(16)]
            i_all = [("i16a", g) for g in range(16)] + [("i16b", g) for g in range(16)]
            Q("dve", I("tensor_copy", out=i16f, in_=i16.rearrange("p g k -> p (g k)")), r=i_all, w=["i16f"])
            m16v = m16.rearrange("p (h two) k -> p h two k", two=2)
            candv = cand.rearrange("p h (a b) -> p h a b", a=16)
            Q("dve", I("tensor_tensor", out=candv, in0=m16v[:, :, 0, :].unsqueeze(3).to_broadcast([128, 8, 16, 16]),
                          in1=m16v[:, :, 1, :].unsqueeze(2).to_broadcast([128, 8, 16, 16]), op=ALU.add),
                 r=m_all, w=["cand"] + [("sc", q4) for q4 in range(4)])
            bposv = bpos.rearrange("p (h k) -> p h k", h=8)
            for h in range(8):
                Q("dve", I("max", out=bs[:, h, 0:8], in_=cand[:, h, :]), r=["cand"], w=[("bsa", h)])
                Q("dve", I("max_index", out=bposv[:, h, 0:8], in_max=bs[:, h, 0:8], in_values=cand[:, h, :]),
                     r=["cand", ("bsa", h)], w=[("bpa", h)])
                Q("dve", I("match_replace", out=cand2[:, h, :], in_to_replace=bs[:, h, 0:8], in_values=cand[:, h, :],
                              imm_value=NEG), r=["cand", ("bsa", h)], w=[("cand2", h), "oh"])
                Q("dve", I("max", out=bs[:, h, 8:16], in_=cand2[:, h, :]), r=[("cand2", h)], w=[("bsb", h)])
                Q("dve", I("max_index", out=bposv[:, h, 8:16], in_max=bs[:, h, 8:16], in_values=cand2[:, h, :]),
                     r=[("cand2", h), ("bsb", h)], w=[("bpb", h)])
            bs_all = [("bsa", h) for h in range(8)] + [("bsb", h) for h in range(8)]
            bp_all = [("bpa", h) for h in range(8)] + [("bpb", h) for h in range(8)]
            Q("dve", I("tensor_single_scalar", out=aidx, in_=bpos, scalar=4, op=ALU.logical_shift_right), r=bp_all, w=["aidx"])
            Q("dve", I("tensor_single_scalar", out=bidx, in_=bpos, scalar=15, op=ALU.bitwise_and), r=bp_all, w=["bidx"])
            Q("dve", I("tensor_copy", out=af_, in_=aidx), r=["aidx"], w=["af"])
            Q("dve", I("tensor_copy", out=bf_, in_=bidx), r=["bidx"], w=["bf"])
            i16fv = i16f.rearrange("p (h two k) -> p h two k", h=8, two=2)
            ohv = oh.rearrange("p (h k a) -> p h k a", h=8, k=16)
            io_b = iota16.unsqueeze(1).unsqueeze(1).to_broadcast([128, 8, 16, 16])
            for which, (posf, dst) in enumerate(((af_, i1s), (bf_, i2s))):
                pv = posf.rearrange("p (h k) -> p h k", h=8)
                Q("dve", I("tensor_tensor", out=ohv, in0=io_b, in1=pv.unsqueeze(3).to_broadcast([128, 8, 16, 16]),
                              op=ALU.is_equal), r=["iota16", "af", "bf"] + [("cand2", h) for h in range(8)], w=["oh"])
                Q("dve", I("tensor_tensor", out=ohv, in0=ohv,
                              in1=i16fv[:, :, which, :].unsqueeze(2).to_broadcast([128, 8, 16, 16]), op=ALU.mult),
                     r=["oh", "i16f"], w=["oh"])
                Q("dve", I("tensor_reduce", out=dst.rearrange("p (h k) -> p h k", h=8), in_=ohv, axis=AX.X, op=ALU.add),
                     r=["oh"], w=[("isel", which)])
            Q("dve", I("scalar_tensor_tensor", out=eidf, in0=i1s, scalar=128.0, in1=i2s, op0=ALU.mult, op1=ALU.add),
                 r=[("isel", 0), ("isel", 1)], w=["eidf"])
            Q("dve", I("tensor_copy", out=eidx[b2], in_=eidf), r=["eidf"], w=[er])
            gexv = gex.rearrange("p (h k) -> p h k", h=8)
            Q("dve", I("tensor_tensor", out=gexv, in0=bs, in1=bs[:, :, 0:1].to_broadcast([128, 8, 16]), op=ALU.subtract),
                 r=bs_all, w=["gex"])
            Q("act", I("activation", out=gex, in_=gex, func=AF.Exp), r=["gex"], w=["gex"])
            Q("dve", I("tensor_reduce", out=gsm, in_=gexv, axis=AX.X, op=ALU.add), r=["gex"], w=["gsm"])
            Q("dve", I("reciprocal", out=gsm, in_=gsm), r=["gsm"], w=["gsm"])
            Q("dve", I("tensor_tensor", out=gates[b2].rearrange("p (h k) -> p h k", h=8), in0=gexv,
                          in1=gsm.unsqueeze(2).to_broadcast([128, 8, 16]), op=ALU.mult), r=["gex", "gsm"], w=[gr])
            if dbg and t == 0:
                Q("sp", I("dma_start", out=dbg_t["d_eidx"], in_=eidx[b2]), r=[er], w=["dbg3"], dma=True, key="dbg")
                Q("sp", I("dma_start", out=dbg_t["d_gates"], in_=gates[b2]), r=[gr], w=["dbg4"], dma=True, key="dbg")
                Q("sp", I("dma_start", out=dbg_t["d_m16"], in_=m16.rearrange("p g k -> p (g k)")), r=m_all, w=["dbg7"],
                     dma=True, key="dbg")
            return fops

        def pump(fops, n=None):
            if not fops:
                return
            k = len(fops) if n is None else min(n, len(fops))
            for a, kw in fops[:k]:
                P.op(*a, **kw)
            del fops[:k]

        def BE(t, nxt):
            b2 = t % 2
            x1r, h2r, er, gr = ("x1", b2), ("h2b", b2), ("eidx", b2), ("gates", b2)

            rate = (len(nxt) / (0.85 * 256)) if nxt else 0.0
            acc = [0.0]

            def gelu_part(g):
                P.op("act", I("activation", out=gl[b2][:, g * GRP:(g + 1) * GRP], in_=araw[b2][:, g * GRP:(g + 1) * GRP],
                              func=AF.Gelu), r=[("araw", b2, j) for j in range(g * GRP, (g + 1) * GRP)], w=[("gl", b2, g)])

            def diag_part(g):
                for j in range(g * GRP, (g + 1) * GRP):
                    s_, d_ = j % NSL, j % 4
                    P.op("dve", I("tensor_scalar", out=dgb[d_], in0=ident_b, scalar1=gl[b2][:, j:j + 1],
                                  scalar2=gates[b2][:, j:j + 1], op0=ALU.mult, op1=ALU.mult),
                         r=[("gl", b2, g), gr, "ident"], w=[("dgb", d_)])
                    for hf in range(2):
                        P.op("pe", I("matmul", ps[2 + hf], lhsT=dgb[d_], rhs=UV[s_][:, D + hf * 512:D + (hf + 1) * 512],
                                     start=(j == 0), stop=(j == 127)), r=[("dgb", d_), ("UV", s_)], w=[psr[2 + hf]])
                    acc[0] += rate
                    pump(nxt, int(acc[0]))
                    acc[0] -= int(acc[0])

            for g in range(NG):
                for j in range(g * GRP, (g + 1) * GRP):
                    s_, p_ = j % NSL, j % 2
                    P.op("pool", I("indirect_dma_start", out=UV[s_], out_offset=None, in_=uvb,
                                   in_offset=bass.IndirectOffsetOnAxis(ap=eidx[b2][:, j:j + 1], axis=0)),
                         r=[er], w=[("UV", s_)], dma=True)
                    P.op("dve", I("tensor_tensor", out=prod[p_], in0=UV[s_][:, 0:D], in1=h2b[b2], op=ALU.mult),
                         r=[("UV", s_), h2r], w=[("prod", p_)])
                    P.op("act", I("activation", out=prod[p_], in_=prod[p_], func=AF.Identity, accum_out=araw[b2][:, j:j + 1]),
                         r=[("prod", p_)], w=[("prod", p_), ("araw", b2, j)])
                    acc[0] += rate
                    pump(nxt, int(acc[0]))
                    acc[0] -= int(acc[0])
                if g >= 1:
                    gelu_part(g - 1)
                if g >= 2:
                    diag_part(g - 2)
            gelu_part(NG - 1)
            diag_part(NG - 2)
            diag_part(NG - 1)
            if dbg and t == 0:
                P.op("sp", I("dma_start", out=dbg_t["d_araw"], in_=araw[b2]), r=[("araw", b2, j) for j in range(128)],
                     w=["dbg6"], dma=True, key="dbg")
            pump(nxt)
            if t + 2 < ntc:
                fe_loads(t + 2)
            for hf in range(2):
                P.op("dve", I("tensor_tensor", out=tmpo[:, hf * 512:(hf + 1) * 512], in0=ps[2 + hf],
                              in1=G2[:, hf * 512:(hf + 1) * 512], op=ALU.mult), r=[psr[2 + hf]], w=[("tmpo", hf)])
            P.op("dve", I("tensor_tensor", out=x1[b2], in0=tmpo, in1=x1[b2], op=ALU.add),
                 r=[("tmpo", 0), ("tmpo", 1), x1r], w=[x1r])
            P.op("act", I("activation", out=tmpo, in_=x1[b2], func=AF.Square, accum_out=ssC[:, 4:5]),
                 r=[x1r], w=[("tmpo", 0), ("tmpo", 1), "ssC4"])
            P.op("act", I("activation", out=ssC[:, 5:6], in_=ssC[:, 4:5], func=AF.Sqrt, scale=1.0 / D, bias=EPS),
                 r=["ssC4"], w=["ssC5"])
            P.op("dve", I("reciprocal", out=ssC[:, 6:7], in_=ssC[:, 5:6]), r=["ssC5"], w=["ssC6"])
            P.op("dve", I("scalar_tensor_tensor", out=tmpo, in0=x1[b2], scalar=ssC[:, 6:7], in1=AFr,
                          op0=ALU.mult, op1=ALU.mult), r=[x1r, "ssC6"], w=[("tmpo", 0), ("tmpo", 1)])
            P.op("dve", I("tensor_tensor", out=tmpo, in0=tmpo, in1=BFr, op=ALU.add),
                 r=[("tmpo", 0), ("tmpo", 1)], w=[("tmpo", 0), ("tmpo", 1)])
            P.op("sp", I("dma_start", out=out[t * 128:(t + 1) * 128, :], in_=tmpo),
                 r=[("tmpo", 0), ("tmpo", 1)], w=[("out_d", t)], dma=True, key="outd")

        fe_loads(0)
        pump(FE(0))
        if ntc > 1:
            fe_loads(1)
        for t in range(ntc):
            nxt = FE(t + 1) if t + 1 < ntc else None
            BE(t, nxt)
        info = P.emit(final_wait_keys=["outd"])
        A.release()
    return nc, info


def make_in_maps(inputs, S=SEQ, n_cores=N_CORES):
    f = lambda a: np.ascontiguousarray(np.asarray(a, dtype=np.float32))
    x = f(inputs["x"])
    c = f(inputs["c"])
    gn = np.concatenate([f(inputs["gn_sb_g"])[0], f(inputs["gn_fox_g"])[0]], axis=0)
    gn = np.ascontiguousarray(gn.reshape(8, 128).T)
    shared = {
        "w_ada": f(inputs["w_ada"])[0],
        "b_ada": f(inputs["b_ada"])[0].reshape(1, -1),
        "norm_mix_g": f(inputs["norm_mix_g"])[0].reshape(1, -1),
        "w_in": f(inputs["w_in"])[0],
        "b_fgate": f(inputs["b_fgate"])[0].reshape(8, 1),
        "gn": gn,
        "w_out": f(inputs["w_out"])[0],
        "norm_ffn_g": f(inputs["norm_ffn_g"])[0].reshape(1, -1),
        "w_query": f(inputs["w_query"])[0],
        "sub_keys": f(inputs["sub_keys"])[0].reshape(16, 128, 128),
        "expert_uv": np.ascontiguousarray(np.concatenate([f(inputs["expert_u"])[0], f(inputs["expert_v"])[0]], axis=1)),
        "w_ada_final": f(inputs["w_ada_final"]),
        "b_ada_final": f(inputs["b_ada_final"]).reshape(1, -1),
        "norm_final_g": f(inputs["norm_final_g"]).reshape(1, -1),
    }
    maps = []
    for b in range(n_cores):
        m = dict(shared)
        m["x"] = np.ascontiguousarray(x[b, :S])
        m["cT"] = np.ascontiguousarray(c[b].reshape(8, 128).T)
        maps.append(m)
    return maps


def kernel(**inputs):
    nc, _ = build(SEQ)
    in_maps = make_in_maps(inputs, SEQ, N_CORES)
    res = run_bass_kernel_spmd(nc, in_maps, core_ids=list(range(N_CORES)))
    return np.stack([np.asarray(r["out"], dtype=np.float32) for r in res.results], axis=0)
```

```python
import contextlib
import math

import numpy as np
import concourse.bass as bass
import concourse.mybir as mybir
from concourse.bass_utils import run_bass_kernel_spmd

F32 = mybir.dt.float32
BF16 = mybir.dt.bfloat16
I32 = mybir.dt.int32
U32 = mybir.dt.uint32
AF = mybir.ActivationFunctionType
ALU = mybir.AluOpType
AX = mybir.AxisListType

D = 1024
EPS = 1e-6
N_CORES = 8
SEQ = 4096
NEG = -1.0e30


class _Op:
    __slots__ = ("eng", "fn", "deps", "dma", "semkey", "signal", "semval", "waits")

    def __init__(self, eng, fn, deps, dma, semkey):
        self.eng = eng
        self.fn = fn
        self.deps = deps
        self.dma = dma
        self.semkey = semkey
        self.signal = False
        self.semval = 0
        self.waits = ()


class Prog:
    ENGS = ("pe", "act", "dve", "pool", "sp")

    def __init__(self, nc):
        self.nc = nc
        self.ops = []
        self.last_w = {}
        self.readers = {}
        self.pending = {e: set() for e in self.ENGS}
        self.last_eng = {}
        self.last_dma = {}

    def op(self, eng, fn, r=(), w=(), dma=False, key=None):
        idx = len(self.ops)
        deps = set()
        for res in r:
            lw = self.last_w.get(res)
            if lw is not None:
                deps.add(lw)
        for res in w:
            lw = self.last_w.get(res)
            if lw is not None:
                deps.add(lw)
            deps.update(self.readers.get(res, ()))
        if self.pending[eng]:
            deps |= self.pending[eng]
            self.pending[eng] = set()
        for res in r:
            self.readers.setdefault(res, []).append(idx)
        for res in w:
            self.last_w[res] = idx
            self.readers[res] = []
        semkey = None
        if dma:
            semkey = key if key is not None else (w[0] if w else ("dma", idx))
            self.last_dma[semkey] = idx
        else:
            self.last_eng[eng] = idx
        self.ops.append(_Op(eng, fn, deps, dma, semkey))
        return idx

    def barrier(self):
        deps = set(self.last_eng.values()) | set(self.last_dma.values())
        for e in self.ENGS:
            self.pending[e] |= deps
        self.last_w.clear()
        self.readers.clear()

    def emit(self, final_wait_keys=()):
        nc = self.nc
        ops = self.ops
        cnt = {e: 0 for e in self.ENGS}
        for o in ops:
            best = {}
            keep = set()
            for d in o.deps:
                p = ops[d]
                if p.dma:
                    keep.add(d)
                else:
                    if p.eng == "pe" and o.eng == "pe" and not o.dma:
                        continue
                    if p.eng not in best or best[p.eng] < d:
                        best[p.eng] = d
            keep.update(best.values())
            o.deps = keep
            for d in keep:
                ops[d].signal = True
        dma_cnt = {}
        dma_keys = []
        for o in ops:
            waits = {}
            for d in o.deps:
                p = ops[d]
                if p.dma:
                    waits[("d", p.semkey)] = dma_cnt[p.semkey]
                else:
                    k = ("e", p.eng)
                    if waits.get(k, 0) < p.semval:
                        waits[k] = p.semval
            o.waits = waits
            if o.dma:
                if o.semkey not in dma_cnt:
                    dma_cnt[o.semkey] = 0
                    dma_keys.append(o.semkey)
                dma_cnt[o.semkey] += 16
                o.semval = dma_cnt[o.semkey]
                o.signal = True
            elif o.signal:
                cnt[o.eng] += 1
                o.semval = cnt[o.eng]
        with contextlib.ExitStack() as st:
            esem = {e: st.enter_context(nc.semaphore("s_" + e)) for e in self.ENGS}
            dsem = {}
            for k in dma_keys:
                dsem[k] = st.enter_context(nc.semaphore("d%d" % len(dsem)))
            block = st.enter_context(nc.Block())

            def run(engname, e):
                waited = {}
                for o in ops:
                    if o.eng != engname:
                        continue
                    for k, v in o.waits.items():
                        if waited.get(k, 0) >= v:
                            continue
                        s = dsem[k[1]] if k[0] == "d" else esem[k[1]]
                        e.wait_ge(s, v)
                        waited[k] = v
                    inst = o.fn(e)
                    if o.signal:
                        if o.dma:
                            inst.then_inc(dsem[o.semkey], 16)
                        else:
                            inst.then_inc(esem[o.eng], 1)
                if engname == "sp":
                    for k in final_wait_keys:
                        e.wait_ge(dsem[k], dma_cnt[k])

            @block.tensor
            def _(e):
                run("pe", e)

            @block.scalar
            def _(e):
                run("act", e)

            @block.vector
            def _(e):
                run("dve", e)

            @block.gpsimd
            def _(e):
                run("pool", e)

            @block.sync
            def _(e):
                run("sp", e)
        return len(dsem), dict(cnt)


def I(name, *args, **kwargs):
    return lambda e: getattr(e, name)(*args, **kwargs)


class Arena:
    def __init__(self, ap, nwords):
        self.ap = ap
        self.n = nwords
        self.off = 0
        self.marks = []

    def f32(self, n):
        assert self.off + n <= self.n, ("arena overflow", self.off, n, self.n)
        a = self.ap[:, self.off:self.off + n]
        self.off += n
        return a

    def b16(self, n):
        assert n % 2 == 0
        return self.f32(n // 2).bitcast(BF16)

    def i32(self, n):
        return self.f32(n).bitcast(I32)

    def u32(self, n):
        return self.f32(n).bitcast(U32)

    def mark(self):
        self.marks.append(self.off)

    def release(self):
        self.off = self.marks.pop()


def build(S=SEQ, dbg=False, n_tok_tiles_c=None):
    NT = S // 128
    NCH = S // 512
    nc = bass.Bass("TRN2", target_bir_lowering=False)

    def din(name, shape, dt=F32):
        return nc.dram_tensor(name, list(shape), dt, kind="ExternalInput").ap()

    x = din("x", [S, D])
    cT = din("cT", [128, 8])
    w_ada = din("w_ada", [D, 6 * D])
    b_ada = din("b_ada", [1, 6 * D])
    norm_mix_g = din("norm_mix_g", [1, D])
    w_in = din("w_in", [D, 3080])
    b_fgate = din("b_fgate", [8, 1])
    gn = din("gn", [128, 8])
    w_out = din("w_out", [D, D])
    norm_ffn_g = din("norm_ffn_g", [1, D])
    w_query = din("w_query", [D, 2048])
    sub_keys = din("sub_keys", [16, 128, 128])
    expert_uv = din("expert_uv", [16384, 2 * D])
    w_ada_final = din("w_ada_final", [D, 2 * D])
    b_ada_final = din("b_ada_final", [1, 2 * D])
    norm_final_g = din("norm_final_g", [1, D])
    out = nc.dram_tensor("out", [S, D], F32, kind="ExternalOutput").ap()

    def dscr(name, shape, dt=BF16):
        return nc.dram_tensor(name, list(shape), dt, kind=("ExternalOutput" if dbg else "Internal")).ap()

    dbg_t = {}
    if dbg:
        for nm, shp, dt in (("d_mod", [128, 8 * D], F32), ("d_x1", [128, D], F32), ("d_h2", [128, D], BF16),
                            ("d_eidx", [128, 128], I32), ("d_gates", [128, 128], F32), ("d_araw", [128, 128], F32),
                            ("d_sc", [128, 2048], F32), ("d_peer", [128, D], F32), ("d_m16", [128, 256], F32)):
            dbg_t[nm] = nc.dram_tensor(nm, shp, dt, kind="ExternalOutput").ap()

    qT_d = dscr("qT_d", [8, 128, S])
    kT_d = dscr("kT_d", [8, 128, S])
    v_d = dscr("v_d", [8, 128, NT, 128])
    aug_d = dscr("aug_d", [8, 2, 6, S])
    md_d = dscr("md_d", [8, 128, S])
    uvb = nc.dram_tensor("uvb", [16384, 2 * D], BF16, kind="Internal").ap()

    P = Prog(nc)
    ARENA_WORDS = 53000
    with contextlib.ExitStack() as st:
        arena_t = st.enter_context(nc.sbuf_tensor("arena", [128, ARENA_WORDS], F32))
        A = Arena(arena_t, ARENA_WORDS)
        ps = [st.enter_context(nc.psum_tensor("ps%d" % i, [128, 512], F32))[:, :] for i in range(8)]
        psr = ["ps%d" % i for i in range(8)]

        ones_f = A.f32(128)
        L_f = A.f32(128)
        bd_f = A.f32(128)
        mstrict_f = A.f32(128)
        ident_b = A.b16(128)
        ones_b = A.b16(128)
        mincl_b = A.b16(128)
        iota16 = A.f32(16)
        gn_sb = A.f32(8)
        nbfg = A.f32(1)
        MOD = A.f32(6 * D)
        MODF = A.f32(2 * D)
        B1, A1, G1 = MOD[:, 0:D], MOD[:, D:2 * D], MOD[:, 2 * D:3 * D]
        B2, A2, G2 = MOD[:, 3 * D:4 * D], MOD[:, 4 * D:5 * D], MOD[:, 5 * D:6 * D]
        BFr, AFr = MODF[:, 0:D], MODF[:, D:2 * D]
        WQ_OFF = ARENA_WORDS - 8192
        wq_b = arena_t[:, WQ_OFF:ARENA_WORDS].bitcast(BF16).rearrange("p (k n) -> p k n", k=8)
        cv_off = A.off
        cvb = [A.b16(2 * 2 * D).rearrange("p (r n) -> p r n", r=2) for _ in range(2)]
        uv_v = expert_uv.rearrange("(p r) n -> p r n", p=128)
        uvb_v = uvb.rearrange("(p r) n -> p r n", p=128)
        conv_state = {"i": 0}

        def conv_step(n):
            for _ in range(n):
                i = conv_state["i"]
                if i > 0 and i <= 64:
                    j = i - 1
                    P.op("pool", I("dma_start", out=uvb_v[:, 2 * j:2 * j + 2, :], in_=cvb[j % 2]),
                         r=[("cvb", j % 2)], w=[("uvb", j)], dma=True, key=("cvs", j % 2))
                if i < 64:
                    P.op("pool", I("dma_start", out=cvb[i % 2], in_=uv_v[:, 2 * i:2 * i + 2, :]),
                         w=[("cvb", i % 2)], dma=True, key=("cvl", i % 2))
                conv_state["i"] = i + 1

        P.op("pool", I("memset", ones_f, 1.0), w=["ones_f"])
        P.op("pool", I("memset", ones_b, 1.0), w=["ones_b"])
        P.op("pool", I("affine_select", out=L_f, in_=ones_f, pattern=[[-1, 128]], compare_op=ALU.is_ge,
                                               fill=0.0, base=0, channel_multiplier=1), r=["ones_f"], w=["L_f"])
        P.op("pool", I("affine_select", out=mstrict_f, in_=ones_f, pattern=[[1, 128]], compare_op=ALU.is_gt,
                                               fill=0.0, base=0, channel_multiplier=-1), r=["ones_f"], w=["mstrict"])
        P.op("pool", I("affine_select", out=mincl_b, in_=ones_f, pattern=[[1, 128]], compare_op=ALU.is_ge,
                                               fill=0.0, base=0, channel_multiplier=-1), r=["ones_f"], w=["mincl"])
        P.op("pool", I("affine_select", out=ident_b, in_=ones_f, pattern=[[-1, 128]], compare_op=ALU.is_equal,
                                               fill=0.0, base=0, channel_multiplier=1), r=["ones_f"], w=["ident"])
        P.op("pool", I("memset", bd_f, 0.0), w=["bd"])
        P.op("pool", I("memset", bd_f[0:64, 0:64], 1.0), w=["bd"])
        P.op("pool", I("memset", bd_f[64:128, 64:128], 1.0), w=["bd"])
        A.mark()
        io_i = A.i32(16)
        P.op("pool", I("iota", io_i, pattern=[[1, 16]], base=0, channel_multiplier=0), w=["io_i"])
        P.op("dve", I("tensor_copy", out=iota16, in_=io_i), r=["io_i"], w=["iota16"])
        P.op("sp", I("dma_start", out=gn_sb, in_=gn), w=["gn"], dma=True)
        bfg = A.f32(1)
        P.op("sp", I("dma_start", out=bfg[0:8, :], in_=b_fgate), w=["bfg"], dma=True)
        P.op("dve", I("tensor_scalar", out=nbfg[0:8, :], in0=bfg[0:8, :], scalar1=-1.0, scalar2=None, op0=ALU.mult),
             r=["bfg"], w=["nbfg"])

        c_sb = A.f32(8)
        c_act = A.f32(8)
        cb = A.f32(8 * 128)
        cbv = cb.rearrange("p (k m) -> p k m", k=8)
        P.op("sp", I("dma_start", out=c_sb, in_=cT), w=["c_sb"], dma=True)
        P.op("act", I("activation", out=c_act, in_=c_sb, func=AF.Silu), r=["c_sb"], w=["c_act"])
        P.op("dve", I("tensor_copy", out=cbv, in_=c_act.unsqueeze(2).to_broadcast([128, 8, 128])),
             r=["c_act"], w=["cb"])
        wst = [A.f32(8 * 512), A.f32(8 * 512)]
        bst = [A.f32(512), A.f32(512)]
        gtmp = A.f32(D)
        blocks = [(w_ada, b_ada, MOD, i) for i in range(12)] + [(w_ada_final, b_ada_final, MODF, i) for i in range(4)]
        for bi, (wsrc, bsrc, dst, i) in enumerate(blocks):
            sb = bi % 2
            wv = wst[sb].rearrange("p (k n) -> p k n", k=8)
            P.op("sp", I("dma_start",
                out=wv, in_=wsrc.rearrange("(k p) n -> p k n", p=128)[:, :, i * 512:(i + 1) * 512]),
                w=[("wst", sb)], dma=True)
            P.op("act", I("dma_start",
                out=bst[sb], in_=bsrc[0:1, i * 512:(i + 1) * 512].partition_broadcast(128)),
                w=[("bst", sb)], dma=True)
            for k in range(8):
                P.op("pe", I("matmul", ps[sb], lhsT=cbv[:, k, :], rhs=wv[:, k, :],
                                                                 start=(k == 0), stop=(k == 7)),
                     r=["cb", ("wst", sb)], w=[psr[sb]])
            P.op("dve", I("tensor_tensor", out=dst[:, i * 512:(i + 1) * 512], in0=ps[sb],
                                                                      in1=bst[sb], op=ALU.add),
                 r=[psr[sb], ("bst", sb)], w=[("mod", bi)])
        for gsrc, row, deps in ((norm_mix_g, A1, (2, 3)), (norm_ffn_g, A2, (8, 9)), (norm_final_g, AFr, (14, 15))):
            P.op("sp", I("dma_start", out=gtmp, in_=gsrc.partition_broadcast(128)), w=["gtmp"], dma=True)
            P.op("dve", I("scalar_tensor_tensor", out=row, in0=row, scalar=1.0, in1=gtmp,
                                                                 op0=ALU.add, op1=ALU.mult),
                 r=["gtmp"] + [("mod", d) for d in deps], w=[("mod", d) for d in deps])
        if dbg:
            P.op("sp", I("dma_start", out=dbg_t["d_mod"][:, 0:6 * D], in_=MOD), r=[("mod", i) for i in range(16)], w=["dbg_mod"], dma=True)
            P.op("sp", I("dma_start", out=dbg_t["d_mod"][:, 6 * D:8 * D], in_=MODF), r=[("mod", i) for i in range(16)], w=["dbg_mod"], dma=True)
        P.barrier()
        A.release()

        A.mark()
        w_in_b = A.b16(8 * 3080).rearrange("p (k n) -> p k n", k=8)
        xt = [A.f32(D) for _ in range(8)]
        junkf = A.f32(D)
        tmpf = A.f32(D)
        hb = [A.b16(D), A.b16(D)]
        hT = [A.b16(8 * 512).rearrange("p (k t) -> p k t", k=8) for _ in range(2)]
        ssA = A.f32(4)
        qkst = [A.b16(512) for _ in range(4)]
        vst = [A.b16(D) for _ in range(2)]
        spf = A.f32(512)
        ef = A.f32(512)
        Gs = [A.f32(512), A.f32(512)]
        r1 = A.f32(512)
        r2 = A.f32(512)
        ones8 = A.f32(512)
        augst = [A.b16(2 * 6 * 512).rearrange("p (q r s) -> p q r s", q=2, r=6) for _ in range(2)]
        for k in range(8):
            for (c0, c1) in ((0, 1024), (1024, 2048), (2048, 3080)):
                P.op("pool", I("dma_start", out=w_in_b[:, k, c0:c1],
                                                                       in_=w_in[k * 128:(k + 1) * 128, c0:c1]),
                     w=[("w_in", k, c0)], dma=True, key="w_in")
        w_in_res = [("w_in", k, c0) for k in range(8) for c0 in (0, 1024, 2048)]
        P.op("pool", I("memset", ones8[0:8, :], 1.0), w=["ones8"])
        for b in range(2):
            P.op("pool", I("memset", augst[b][0:8, 0, 3:6, :], 1.0), w=[("augst", b)])
            P.op("pool", I("memset", augst[b][0:8, 1, 0:3, :], 1.0), w=[("augst", b)])
        psT_b = ps[7].bitcast(BF16)

        def qk_col(g):
            pair, isk = g % 8, g // 8
            if pair < 4:
                return (512 if isk else 0) + pair * 128
            return (2048 if isk else 1536) + (pair - 4) * 128

        def x_loads(ch):
            for tt in range(4):
                t = ch * 4 + tt
                P.op("sp", I("dma_start", out=xt[t % 8], in_=x[t * 128:(t + 1) * 128, :]), w=[("xt", t % 8)], dma=True)

        x_loads(0)
        for ch in range(NCH):
            hb_ = ch % 2
            if ch + 1 < NCH:
                x_loads(ch + 1)
            for tt in range(4):
                t = ch * 4 + tt
                xb_ = t % 8
                P.op("act", I("activation", out=junkf, in_=xt[xb_], func=AF.Square, accum_out=ssA[:, 0:1]),
                     r=[("xt", xb_)], w=["junkf", "ssA0"])
                P.op("act", I("activation", out=ssA[:, 1:2], in_=ssA[:, 0:1], func=AF.Sqrt, scale=1.0 / D, bias=EPS),
                     r=["ssA0"], w=["ssA1"])
                P.op("dve", I("reciprocal", out=ssA[:, 2:3], in_=ssA[:, 1:2]), r=["ssA1"], w=["ssA2"])
                P.op("dve", I("scalar_tensor_tensor", out=tmpf, in0=xt[xb_], scalar=ssA[:, 2:3], in1=A1,
                                                                     op0=ALU.mult, op1=ALU.mult),
                     r=[("xt", xb_), "ssA2"], w=["tmpf"])
                P.op("dve", I("tensor_tensor", out=hb[t % 2], in0=tmpf, in1=B1, op=ALU.add),
                     r=["tmpf"], w=[("hb", t % 2)])
                for k in range(8):
                    P.op("pe", I("transpose", out=psT_b[:, k * 128:(k + 1) * 128],
                                                                   in_=hb[t % 2][:, k * 128:(k + 1) * 128], identity=ident_b),
                         r=[("hb", t % 2), "ident"], w=[psr[7]])
                P.op("act", I("copy", out=hT[hb_][:, :, tt * 128:(tt + 1) * 128],
                                                             in_=psT_b.rearrange("p (k t) -> p k t", k=8)),
                     r=[psr[7]], w=[("hT", hb_, tt)])
            hres = [("hT", hb_, tt) for tt in range(4)]
            for g in range(16):
                pair, isk = g % 8, g // 8
                col0 = qk_col(g)
                bank = g % 2
                sb_ = g % 4
                for k in range(8):
                    P.op("pe", I("matmul",
                        ps[bank], lhsT=w_in_b[:, k, col0:col0 + 128], rhs=hT[hb_][:, k, :], start=(k == 0), stop=(k == 7)),
                        r=hres + w_in_res, w=[psr[bank]])
                if g % 2 == 0:
                    P.op("act", I("activation",
                        out=qkst[sb_], in_=ps[bank], func=AF.Copy, scale=(1.0 if isk else 0.125)),
                        r=[psr[bank]], w=[("qkst", sb_)])
                else:
                    P.op("dve", I("tensor_scalar",
                        out=qkst[sb_], in0=ps[bank], scalar1=(1.0 if isk else 0.125), scalar2=None, op0=ALU.mult),
                        r=[psr[bank]], w=[("qkst", sb_)])
                dst = (kT_d if isk else qT_d)[pair, :, ch * 512:(ch + 1) * 512]
                P.op("sp", I("dma_start", out=dst, in_=qkst[sb_]),
                     r=[("qkst", sb_)], w=[("qk_d", g, ch)], dma=True, key=("qkd", sb_))
            for tt in range(4):
                t = ch * 4 + tt
                vb_ = t % 2
                for vb in range(2):
                    c0 = 1024 if vb == 0 else 2560
                    bank = 2 + vb
                    for k in range(8):
                        P.op("pe", I("matmul",
                            ps[bank], lhsT=hT[hb_][:, k, tt * 128:(tt + 1) * 128], rhs=w_in_b[:, k, c0:c0 + 512],
                            start=(k == 0), stop=(k == 7)),
                            r=hres + w_in_res, w=[psr[bank]])
                    if vb == 0:
                        P.op("act", I("copy", out=vst[vb_][:, 0:512], in_=ps[bank]),
                             r=[psr[bank]], w=[("vst", vb_, 0)])
                    else:
                        P.op("dve", I("tensor_copy", out=vst[vb_][:, 512:1024], in_=ps[bank]),
                             r=[psr[bank]], w=[("vst", vb_, 1)])
                P.op("sp", I("dma_start",
                    out=v_d.rearrange("a p n c -> p a n c")[:, :, t, :],
                    in_=vst[vb_].rearrange("p (a c) -> p a c", a=8)),
                    r=[("vst", vb_, 0), ("vst", vb_, 1)], w=[("v_d", t)], dma=True, key=("vd", vb_))
            for k in range(8):
                P.op("pe", I("matmul", ps[4][0:8, :], lhsT=w_in_b[:, k, 3072:3080], rhs=hT[hb_][:, k, :],
                                                            start=(k == 0), stop=(k == 7)),
                     r=hres + w_in_res, w=[psr[4]])
            P.op("act", I("activation", out=ef[0:8, :], in_=ps[4][0:8, :], func=AF.Exp, scale=-1.0, bias=nbfg[0:8, :]),
                 r=[psr[4], "nbfg"], w=["ef"])
            P.op("act", I("activation", out=spf[0:8, :], in_=ef[0:8, :], func=AF.Ln, bias=1.0), r=["ef"], w=["spf"])
            gb = ch % 2
            if ch == 0:
                P.op("dve", I("tensor_tensor_scan", out=Gs[gb][0:8, :], data0=ones8[0:8, :], data1=spf[0:8, :],
                                                                 initial=0.0, op0=ALU.mult, op1=ALU.add),
                     r=["ones8", "spf"], w=[("Gs", gb)])
            else:
                P.op("dve", I("tensor_tensor_scan", out=Gs[gb][0:8, :], data0=ones8[0:8, :], data1=spf[0:8, :],
                                                                 initial=Gs[1 - gb][0:8, 511:512], op0=ALU.mult, op1=ALU.add),
                     r=["ones8", "spf", ("Gs", 1 - gb)], w=[("Gs", gb)])
            ab = augst[gb]
            ar = ("augst", gb)
            P.op("dve", I("tensor_copy", out=ab[0:8, 1, 3, :], in_=Gs[gb][0:8, :]), r=[("Gs", gb)], w=[ar])
            P.op("dve", I("tensor_tensor", out=r1[0:8, :], in0=Gs[gb][0:8, :], in1=ab[0:8, 1, 3, :],
                                                               op=ALU.subtract), r=[("Gs", gb), ar], w=["r1"])
            P.op("dve", I("tensor_copy", out=ab[0:8, 1, 4, :], in_=r1[0:8, :]), r=["r1"], w=[ar])
            P.op("dve", I("tensor_tensor", out=r2[0:8, :], in0=r1[0:8, :], in1=ab[0:8, 1, 4, :], op=ALU.subtract),
                 r=["r1", ar], w=["r2"])
            P.op("dve", I("tensor_copy", out=ab[0:8, 1, 5, :], in_=r2[0:8, :]), r=["r2"], w=[ar])
            P.op("dve", I("tensor_scalar", out=ab[0:8, 0, 0:3, :], in0=ab[0:8, 1, 3:6, :], scalar1=-1.0,
                                                         scalar2=None, op0=ALU.mult), r=[ar], w=[ar])
            P.op("sp", I("dma_start", out=aug_d[:, :, :, ch * 512:(ch + 1) * 512], in_=ab[0:8, :, :, :]),
                 r=[ar], w=[("aug_d", ch)], dma=True, key=("augd", gb))
        P.barrier()
        A.release()

        A.mark()
        qTp = [A.b16(S) for _ in range(2)]
        kTp = [A.b16(S) for _ in range(2)]
        Vp = [A.b16(NT * 128).rearrange("p (n c) -> p n c", c=128) for _ in range(2)]
        augt = [A.b16(2 * S).rearrange("p (q s) -> p q s", q=2) for _ in range(2)]
        eb_off = A.off
        ebuf = [[A.f32(512) for _ in range(2)] for _ in range(2)]
        spbuf = [[A.f32(512) for _ in range(2)] for _ in range(2)]
        Vx = [arena_t[:, eb_off + 2048 * b_:eb_off + 2048 * b_ + S // 2].bitcast(BF16).rearrange("p (n c) -> p n c", c=128)
              for b_ in range(2)]
        VX_RES = [[("ebuf", h_, k_) for h_ in range(2) for k_ in range(2)],
                  [("spbuf", h_, k_) for h_ in range(2) for k_ in range(2)]]
        rbuf = [A.f32(512) for _ in range(2)]
        abuf = [[A.b16(512) for _ in range(3)] for _ in range(2)]
        Sacc = [A.f32(512) for _ in range(2)]
        on_sb = A.f32(512)
        sq_sb = A.f32(512)
        rt_sb = A.f32(512)
        mst = [A.b16(512) for _ in range(2)]
        ZB = (0, 1)
        IB = (2, 3)
        OB = (4, 5)
        LB = (6, 7)

        def pair_loads(pair):
            fox = pair >= 4
            sb = pair % 2
            if not fox:
                P.op("sp", I("dma_start", out=qTp[sb], in_=qT_d[pair]), w=[("qTp", sb, 0), ("qTp", sb, 1)], dma=True,
                     key=("qTp", sb))
                P.op("sp", I("dma_start", out=kTp[sb], in_=kT_d[pair]), w=[("kTp", sb, 0), ("kTp", sb, 1)], dma=True,
                     key=("kTp", sb))
            else:
                hA, hB = 2 * (pair - 4), 2 * (pair - 4) + 1
                for (dst, src_d, qk, nm) in ((qTp[sb], qT_d, 0, "qTp"), (kTp[sb], kT_d, 1, "kTp")):
                    P.op("sp", I("dma_start", out=dst[0:64, :], in_=src_d[pair, 0:64, :]), w=[(nm, sb, 0)], dma=True,
                         key=(nm, sb, "m"))
                    P.op("pool", I("memset", dst[64:128, :], 0.0), w=[(nm, sb, 1)])
                    P.op("sp", I("dma_start", out=dst[64:70, :], in_=aug_d[hA, qk]), w=[(nm, sb, 1)], dma=True,
                         key=(nm, sb, "a"))
                for (src_d, qk) in ((qT_d, 0), (kT_d, 1)):
                    P.op("sp", I("dma_start", out=augt[sb][0:64, qk, :], in_=src_d[pair, 64:128, :]),
                         w=[("augt", sb, qk, 0)], dma=True, key=("augt", sb, qk, "m"))
                    P.op("pool", I("memset", augt[sb][64:128, qk, :], 0.0), w=[("augt", sb, qk, 1)])
                    P.op("sp", I("dma_start", out=augt[sb][64:70, qk, :], in_=aug_d[hB, qk]),
                         w=[("augt", sb, qk, 1)], dma=True, key=("augt", sb, qk, "a"))
            P.op("sp", I("dma_start", out=Vp[sb], in_=v_d[pair]), w=[("Vp", sb)], dma=True)

        pending_epi = []

        def flush_epi(part=None):
            for epi in pending_epi:
                k = next(i for i, (a, kw) in enumerate(epi) if a[0] == "pe") if epi else 0
                if part in (0, None):
                    for a, kw in epi[:k]:
                        P.op(*a, **kw)
                    del epi[:k]
                if part in (1, None):
                    for a, kw in epi:
                        P.op(*a, **kw)
                    del epi[:]
            if part in (1, None):
                del pending_epi[:]

        pair_loads(0)
        for pair in range(8):
            fox = pair >= 4
            sb = pair % 2
            if pair + 1 < 8:
                pair_loads(pair + 1)
            if fox:
                P.op("pool", I("memset", Vx[sb][:, :, 0:64], 1.0), w=VX_RES[sb])
                P.op("pool", I("tensor_copy", out=Vx[sb][:, :, 64:128], in_=Vp[sb][:, :, 64:128]),
                     r=[("Vp", sb)], w=VX_RES[sb])
                P.op("pool", I("memset", Vp[sb][:, :, 64:128], 1.0), w=[("Vp", sb)])
            v_r = ("Vp", sb)
            for c in range(NCH):
                nk = 4 * (c + 1)
                order = list(range(nk)) if fox else list(range(nk - 1, -1, -1))
                gc = pair * NCH + c
                ob = OB[gc % 2]
                lb = LB[gc % 2]
                conv_step(-(-65 // (8 * NCH)))
                if pair == 7 and c == 0:
                    assert A.off <= WQ_OFF, ("phase B overlaps wq_b", A.off, WQ_OFF)
                    for k in range(8):
                        for h in range(2):
                            P.op("pool", I("dma_start", out=wq_b[:, k, h * 1024:(h + 1) * 1024],
                                           in_=w_query[k * 128:(k + 1) * 128, h * 1024:(h + 1) * 1024]),
                                 w=[("wq", k, h)], dma=True, key="wq")
                if not fox:
                    for hh in range(2):
                        P.op("pool", I("memset", Sacc[hh], 0.0), w=[("S", hh)])

                def rng(kb):
                    j = kb - 4 * c
                    q0 = 128 * j if j > 0 else 0
                    return j, q0

                def emit_qk(i):
                    kb = order[i]
                    j, q0 = rng(kb)
                    for hh in range(2):
                        pb = 64 * hh
                        zb = (ZB[hh], IB[hh])[i % 2] if fox else ZB[hh]
                        if not fox:
                            P.op("pe", I("matmul", ps[zb][:, q0:512], lhsT=kTp[sb][pb:pb + 64, kb * 128:(kb + 1) * 128],
                                         rhs=qTp[sb][pb:pb + 64, c * 512 + q0:(c + 1) * 512], start=True, stop=True),
                                 r=[("qTp", sb, 0), ("qTp", sb, 1), ("kTp", sb, 0), ("kTp", sb, 1)], w=[psr[zb]])
                        else:
                            qs = qTp[sb] if hh == 0 else augt[sb][:, 0, :]
                            ks = kTp[sb] if hh == 0 else augt[sb][:, 1, :]
                            rr = ([("qTp", sb, 0), ("qTp", sb, 1), ("kTp", sb, 0), ("kTp", sb, 1)] if hh == 0 else
                                  [("augt", sb, 0, 0), ("augt", sb, 0, 1), ("augt", sb, 1, 0), ("augt", sb, 1, 1)])
                            P.op("pe", I("matmul", ps[zb][:, q0:512], lhsT=ks[:, kb * 128:(kb + 1) * 128],
                                         rhs=qs[:, c * 512 + q0:(c + 1) * 512], start=True, stop=True),
                                 r=rr, w=[psr[zb]])

                def emit_exp(i):
                    kb = order[i]
                    j, q0 = rng(kb)
                    for hh in range(2):
                        zb = (ZB[hh], IB[hh])[i % 2] if fox else ZB[hh]
                        if fox:
                            pbuf = abuf[hh][i % 3]
                            pr = ("abuf", hh, i % 3)
                            P.op("act", I("activation",
                                out=pbuf[:, q0:512], in_=ps[zb][:, q0:512], func=AF.Exp), r=[psr[zb]], w=[pr])
                            if j >= 0:
                                P.op("pool", I("affine_select", out=pbuf[:, q0:q0 + 128], in_=pbuf[:, q0:q0 + 128],
                                               pattern=[[1, 128]], compare_op=ALU.is_ge, fill=0.0, base=0,
                                               channel_multiplier=-1), r=[pr], w=[pr])
                        else:
                            eb = ebuf[hh][i % 2]
                            er = ("ebuf", hh, i % 2)
                            P.op("act", I("activation",
                                out=eb[:, q0:512], in_=ps[zb][:, q0:512], func=AF.Exp), r=[psr[zb]], w=[er])
                            if j >= 0:
                                P.op("dve", I("tensor_tensor",
                                    out=eb[:, q0:q0 + 128], in0=eb[:, q0:q0 + 128], in1=mstrict_f, op=ALU.mult),
                                    r=[er, "mstrict"], w=[er])

                def emit_ln(i):
                    kb = order[i]
                    j, q0 = rng(kb)
                    for hh in range(2):
                        eb, sp_ = ebuf[hh][i % 2], spbuf[hh][i % 2]
                        P.op("act", I("activation",
                            out=sp_[:, q0:512], in_=eb[:, q0:512], func=AF.Ln, bias=1.0),
                            r=[("ebuf", hh, i % 2)], w=[("spbuf", hh, i % 2)])

                def emit_cum(i):
                    kb = order[i]
                    j, q0 = rng(kb)
                    for hh in range(2):
                        sp_ = spbuf[hh][i % 2]
                        ib = IB[hh]
                        P.op("pe", I("matmul",
                            ps[ib][:, q0:512], lhsT=L_f, rhs=sp_[:, q0:512], start=True, stop=(i == 0)),
                            r=[("spbuf", hh, i % 2), "L_f"], w=[psr[ib]])
                        if i > 0:
                            P.op("pe", I("matmul",
                                ps[ib][:, q0:512], lhsT=ones_f, rhs=Sacc[hh][:, q0:512], start=False, stop=True),
                                r=[("S", hh), "ones_f"], w=[psr[ib]])
                        if i < nk - 1:
                            P.op("pool", I("tensor_tensor",
                                out=Sacc[hh][:, q0:512], in0=Sacc[hh][:, q0:512], in1=sp_[:, q0:512], op=ALU.add),
                                r=[("spbuf", hh, i % 2), ("S", hh)], w=[("S", hh)])

                def emit_neg(i):
                    kb = order[i]
                    j, q0 = rng(kb)
                    for hh in range(2):
                        ib = IB[hh]
                        P.op("act", I("activation",
                            out=rbuf[hh][:, q0:512], in_=ps[ib][:, q0:512], func=AF.Exp, scale=-1.0),
                            r=[psr[ib]], w=[("rbuf", hh)])
                        eb, ab_ = ebuf[hh][i % 2], abuf[hh][i % 3]
                        P.op("dve", I("tensor_tensor",
                            out=ab_[:, q0:512], in0=eb[:, q0:512], in1=rbuf[hh][:, q0:512], op=ALU.mult),
                            r=[("ebuf", hh, i % 2), ("rbuf", hh)], w=[("abuf", hh, i % 3)])

                def emit_av(i):
                    kb = order[i]
                    j, q0 = rng(kb)
                    for hh in range(2):
                        pb = 64 * hh
                        ab_ = abuf[hh][i % 3]
                        if fox:
                            bank = (ob, lb)[hh]
                            lhs = (Vp[sb], Vx[sb])[hh]
                            P.op("pe", I("matmul", ps[bank][:, q0:512], lhsT=lhs[:, kb, :], rhs=ab_[:, q0:512],
                                         start=(i == 0), stop=(i == nk - 1), skip_group_check=True),
                                 r=[("abuf", hh, i % 3), v_r] + (VX_RES[sb] if hh == 1 else []),
                                 w=[(psr[bank], 0), (psr[bank], 1)])
                        else:
                            P.op("pe", I("matmul", ps[ob][pb:pb + 64, q0:512], lhsT=Vp[sb][:, kb, pb:pb + 64],
                                         rhs=ab_[:, q0:512], start=(i == 0), stop=(i == nk - 1), skip_group_check=True),
                                 r=[("abuf", hh, i % 3), v_r], w=[(psr[ob], hh)])

                emit_qk(0)
                for i in range(nk):
                    emit_exp(i)
                    if i + 1 < nk:
                        emit_qk(i + 1)
                    if i == 0:
                        flush_epi(0)
                    if i == 2:
                        flush_epi(1)
                    if fox:
                        if i > 0:
                            emit_av(i - 1)
                    else:
                        emit_ln(i)
                        if i > 0:
                            emit_neg(i - 1)
                        emit_cum(i)
                        if i > 0:
                            emit_av(i - 1)
                if not fox:
                    emit_neg(nk - 1)
                emit_av(nk - 1)

                epi = []
                o_r = [(psr[ob], 0), (psr[ob], 1)]
                l_r = [(psr[lb], 0), (psr[lb], 1)]
                if fox:
                    epi.append((("act", I("activation", out=rt_sb[0:64, :], in_=ps[ob][64:128, :], func=AF.Ln)),
                                dict(r=o_r, w=["rt_sb"])))
                    epi.append((("act", I("activation", out=rt_sb[64:128, :], in_=ps[lb][0:64, :], func=AF.Ln)),
                                dict(r=l_r, w=["rt_sb"])))
                    epi.append((("act", I("activation", out=rt_sb, in_=rt_sb, func=AF.Exp, scale=-1.0)),
                                dict(r=["rt_sb"], w=["rt_sb"])))
                    epi.append((("dve", I("tensor_tensor", out=on_sb[0:64, :], in0=ps[ob][0:64, :], in1=rt_sb[0:64, :],
                                          op=ALU.mult)), dict(r=o_r + ["rt_sb"], w=["on_sb"])))
                    epi.append((("dve", I("tensor_tensor", out=on_sb[64:128, :], in0=ps[lb][64:128, :],
                                          in1=rt_sb[64:128, :], op=ALU.mult)), dict(r=l_r + ["rt_sb"], w=["on_sb"])))
                else:
                    epi.append((("act", I("copy", out=on_sb, in_=ps[ob])), dict(r=o_r, w=["on_sb"])))
                epi.append((("dve", I("tensor_tensor", out=sq_sb, in0=on_sb, in1=on_sb, op=ALU.mult)),
                            dict(r=["on_sb"], w=["sq_sb"])))
                epi.append((("pe", I("matmul", ps[lb], lhsT=bd_f, rhs=sq_sb, start=True, stop=True)),
                            dict(r=["sq_sb", "bd"], w=l_r)))
                epi.append((("act", I("activation", out=sq_sb, in_=ps[lb], func=AF.Ln, scale=1.0 / 64, bias=EPS)),
                            dict(r=l_r, w=["sq_sb"])))
                epi.append((("act", I("activation", out=rt_sb, in_=sq_sb, func=AF.Exp, scale=-0.5)),
                            dict(r=["sq_sb"], w=["rt_sb"])))
                mb = gc % 2
                epi.append((("dve", I("scalar_tensor_tensor", out=mst[mb], in0=on_sb, scalar=gn_sb[:, pair:pair + 1],
                                      in1=rt_sb, op0=ALU.mult, op1=ALU.mult)),
                            dict(r=["on_sb", "rt_sb", "gn"], w=[("mst", mb)])))
                epi.append((("sp", I("dma_start", out=md_d[pair, :, c * 512:(c + 1) * 512], in_=mst[mb])),
                            dict(r=[("mst", mb)], w=[("md_d", pair, c)], dma=True, key=("mdd", mb))))
                pending_epi.append(epi)
        flush_epi()
        conv_step(66)
        P.barrier()
        A.release()

        A.mark()
        A.off = cv_off
        w_out_b = A.b16(8 * D).rearrange("p (k n) -> p k n", k=8)
        skT = A.b16(16 * 128).rearrange("p (g n) -> p g n", g=16)
        NSL = 16
        GRP = 2
        NG = 128 // GRP
        UV = [A.b16(2 * D) for _ in range(NSL)]
        mdt = A.b16(8 * 128).rearrange("p (k t) -> p k t", k=8)
        xc = A.f32(D)
        x1 = [A.f32(D), MOD[:, D:2 * D]]
        tmpc = A.f32(D)
        tmpo = MOD[:, 0:D]
        h2b = [A.b16(D) for _ in range(2)]
        h2T = A.b16(8 * 128).rearrange("p (k t) -> p k t", k=8)
        sc_raw = A.f32(2048)
        sc = sc_raw.rearrange("p (g n) -> p g n", g=16)
        qb = sc_raw[:, 0:1024].bitcast(BF16)
        qpT = sc_raw[:, 1024:2048].bitcast(BF16).rearrange("p (g t) -> p g t", g=16)
        sc2s = [A.f32(128) for _ in range(2)]
        m16 = A.f32(256).rearrange("p (g k) -> p g k", g=16)
        i16 = A.u32(256).rearrange("p (g k) -> p g k", g=16)
        i16f = A.f32(256)
        cand = sc_raw.rearrange("p (h q) -> p h q", h=8)
        oh = A.f32(2048)
        cand2 = oh.rearrange("p (h q) -> p h q", h=8)
        bs = A.f32(128).rearrange("p (h k) -> p h k", h=8)
        bpos = A.u32(128)
        aidx = A.u32(128)
        bidx = A.u32(128)
        af_ = A.f32(128)
        bf_ = A.f32(128)
        i1s = A.f32(128)
        i2s = A.f32(128)
        eidf = A.f32(128)
        eidx = [A.i32(128) for _ in range(2)]
        gex = A.f32(128)
        gsm = A.f32(8)
        gates = [A.f32(128) for _ in range(2)]
        araw = [A.f32(128) for _ in range(2)]
        gl = [A.f32(128) for _ in range(2)]
        prod = [A.b16(D) for _ in range(2)]
        dgb = [A.b16(128) for _ in range(4)]
        ssC = A.f32(16)
        skst = sc_raw.rearrange("p (g c) -> p g c", g=16)
        assert A.off <= WQ_OFF, ("phase C overlaps wq_b", A.off, WQ_OFF)
        skb = oh[:, 0:1024].bitcast(BF16).rearrange("p (g c) -> p g c", g=16)

        for k in range(8):
            P.op("pool", I("dma_start", out=w_out_b[:, k, :], in_=w_out[k * 128:(k + 1) * 128, :]),
                 w=[("w_out", k)], dma=True, key="wc")
        wo_res = [("w_out", k) for k in range(8)]
        wq_res = []
        P.op("sp", I("dma_start", out=skst, in_=sub_keys.rearrange("g n c -> n g c")), w=["skst"], dma=True)
        P.op("dve", I("tensor_copy", out=skb, in_=skst), r=["skst"], w=["skb"])
        pT = [ps[4].bitcast(BF16), ps[5].bitcast(BF16)]
        for g in range(16):
            P.op("pe", I("transpose", out=pT[g // 8][:, (g % 8) * 128:(g % 8 + 1) * 128], in_=skb[:, g, :],
                                                  identity=ident_b), r=["skb", "ident"], w=[psr[4 + g // 8]])
        for hf in range(2):
            P.op("act", I("copy", out=skT[:, hf * 8:(hf + 1) * 8, :],
                                                in_=pT[hf].rearrange("p (g n) -> p g n", g=8)),
                 r=[psr[4 + hf]], w=[("skT", hf)])
        sk_res = [("skT", 0), ("skT", 1)]
        ntc = NT if n_tok_tiles_c is None else n_tok_tiles_c

        def fe_loads(t):
            P.op("sp", I("dma_start", out=xc, in_=x[t * 128:(t + 1) * 128, :]), w=["xc"], dma=True)
            P.op("sp", I("dma_start", out=mdt, in_=md_d.rearrange("a p s -> p a s")[:, :, t * 128:(t + 1) * 128]),
                 w=["mdt"], dma=True)

        def FE(t):
            b2 = t % 2
            x1r, h2r, er, gr = ("x1", b2), ("h2b", b2), ("eidx", b2), ("gates", b2)
            fops = []

            def Q(*a, **k):
                fops.append((a, k))
            for hf in range(2):
                for k in range(8):
                    Q("pe", I("matmul", ps[hf], lhsT=mdt[:, k, :], rhs=w_out_b[:, k, hf * 512:(hf + 1) * 512],
                                 start=(k == 0), stop=(k == 7)), r=["mdt"] + wo_res, w=[psr[hf]])
                Q("dve", I("tensor_tensor", out=tmpc[:, hf * 512:(hf + 1) * 512], in0=ps[hf],
                              in1=G1[:, hf * 512:(hf + 1) * 512], op=ALU.mult), r=[psr[hf]], w=[("tmpc", hf)])
            Q("dve", I("tensor_tensor", out=x1[b2], in0=tmpc, in1=xc, op=ALU.add),
                 r=[("tmpc", 0), ("tmpc", 1), "xc"], w=[x1r])
            Q("act", I("activation", out=tmpc, in_=x1[b2], func=AF.Square, accum_out=ssC[:, 0:1]),
                 r=[x1r], w=[("tmpc", 0), ("tmpc", 1), "ssC0"])
            Q("act", I("activation", out=ssC[:, 1:2], in_=ssC[:, 0:1], func=AF.Sqrt, scale=1.0 / D, bias=EPS),
                 r=["ssC0"], w=["ssC1"])
            Q("dve", I("reciprocal", out=ssC[:, 2:3], in_=ssC[:, 1:2]), r=["ssC1"], w=["ssC2"])
            Q("dve", I("scalar_tensor_tensor", out=tmpc, in0=x1[b2], scalar=ssC[:, 2:3], in1=A2,
                          op0=ALU.mult, op1=ALU.mult), r=[x1r, "ssC2"], w=[("tmpc", 0), ("tmpc", 1)])
            Q("dve", I("tensor_tensor", out=h2b[b2], in0=tmpc, in1=B2, op=ALU.add),
                 r=[("tmpc", 0), ("tmpc", 1)], w=[h2r])
            for k in range(8):
                Q("pe", I("transpose", out=pT[0][:, k * 128:(k + 1) * 128], in_=h2b[b2][:, k * 128:(k + 1) * 128],
                             identity=ident_b), r=[h2r, "ident"], w=[psr[4]])
            Q("act", I("copy", out=h2T, in_=pT[0].rearrange("p (k t) -> p k t", k=8)), r=[psr[4]], w=["h2T"])
            if dbg and t == 0:
                Q("sp", I("dma_start", out=dbg_t["d_x1"], in_=x1[b2]), r=[x1r], w=["dbg1"], dma=True, key="dbg")
                Q("sp", I("dma_start", out=dbg_t["d_h2"], in_=h2b[b2]), r=[h2r], w=["dbg2"], dma=True, key="dbg")
            for blk in range(4):
                bank = 4 + blk
                for k in range(8):
                    Q("pe", I("matmul", ps[bank], lhsT=h2T[:, k, :], rhs=wq_b[:, k, blk * 512:(blk + 1) * 512],
                                 start=(k == 0), stop=(k == 7)), r=["h2T"] + wq_res, w=[psr[bank]])
                if blk % 2 == 0:
                    Q("act", I("copy", out=qb[:, blk * 512:(blk + 1) * 512], in_=ps[bank]), r=[psr[bank]], w=[("qb", blk)])
                else:
                    Q("dve", I("tensor_copy", out=qb[:, blk * 512:(blk + 1) * 512], in_=ps[bank]),
                         r=[psr[bank]], w=[("qb", blk)])
            for g in range(16):
                Q("pe", I("transpose", out=pT[g // 8][:, (g % 8) * 128:(g % 8 + 1) * 128],
                             in_=qb[:, g * 128:(g + 1) * 128], identity=ident_b),
                     r=[("qb", g // 4), "ident"], w=[psr[4 + g // 8]])
            Q("act", I("copy", out=qpT[:, 0:8, :], in_=pT[0].rearrange("p (g n) -> p g n", g=8)),
                 r=[psr[4]], w=[("qpT", 0)])
            Q("dve", I("tensor_copy", out=qpT[:, 8:16, :], in_=pT[1].rearrange("p (g n) -> p g n", g=8)),
                 r=[psr[5]], w=[("qpT", 1)])
            for g in range(16):
                bank = 4 + g // 4
                Q("pe", I("matmul", ps[bank][:, (g % 4) * 128:(g % 4 + 1) * 128], lhsT=qpT[:, g, :], rhs=skT[:, g, :],
                             start=True, stop=True), r=[("qpT", g // 8)] + sk_res, w=[psr[bank]])
            for q4 in range(4):
                if q4 % 2 == 0:
                    Q("act", I("copy", out=sc[:, q4 * 4:(q4 + 1) * 4, :], in_=ps[4 + q4].rearrange("p (g n) -> p g n", g=4)),
                         r=[psr[4 + q4], ("qpT", 0), ("qpT", 1)] + [("qb", b_) for b_ in range(4)], w=[("sc", q4), "cand"])
                else:
                    Q("dve", I("tensor_copy", out=sc[:, q4 * 4:(q4 + 1) * 4, :],
                                  in_=ps[4 + q4].rearrange("p (g n) -> p g n", g=4)),
                         r=[psr[4 + q4], ("qpT", 0), ("qpT", 1)] + [("qb", b_) for b_ in range(4)], w=[("sc", q4), "cand"])
            for g in range(16):
                sr = ("sc", g // 4)
                s2 = sc2s[g % 2]
                s2r = ("sc2", g % 2)
                Q("dve", I("max", out=m16[:, g, 0:8], in_=sc[:, g, :]), r=[sr], w=[("m16a", g)])
                Q("dve", I("max_index", out=i16[:, g, 0:8], in_max=m16[:, g, 0:8], in_values=sc[:, g, :]),
                     r=[sr, ("m16a", g)], w=[("i16a", g)])
                Q("dve", I("match_replace", out=s2, in_to_replace=m16[:, g, 0:8], in_values=sc[:, g, :], imm_value=NEG),
                     r=[sr, ("m16a", g)], w=[s2r])
                Q("dve", I("max", out=m16[:, g, 8:16], in_=s2), r=[s2r], w=[("m16b", g)])
                Q("dve", I("max_index", out=i16[:, g, 8:16], in_max=m16[:, g, 8:16], in_values=s2),
                     r=[s2r, ("m16b", g)], w=[("i16b", g)])
            m_all = [("m16a", g) for g in range(16)] + [("m16b", g) for g in range(16)]
            i_all = [("i16a", g) for g in range(16)] + [("i16b", g) for g in range(16)]
            Q("dve", I("tensor_copy", out=i16f, in_=i16.rearrange("p g k -> p (g k)")), r=i_all, w=["i16f"])
            m16v = m16.rearrange("p (h two) k -> p h two k", two=2)
            candv = cand.rearrange("p h (a b) -> p h a b", a=16)
            Q("dve", I("tensor_tensor", out=candv, in0=m16v[:, :, 0, :].unsqueeze(3).to_broadcast([128, 8, 16, 16]),
                          in1=m16v[:, :, 1, :].unsqueeze(2).to_broadcast([128, 8, 16, 16]), op=ALU.add),
                 r=m_all, w=["cand"] + [("sc", q4) for q4 in range(4)])
            bposv = bpos.rearrange("p (h k) -> p h k", h=8)
            for h in range(8):
                Q("dve", I("max", out=bs[:, h, 0:8], in_=cand[:, h, :]), r=["cand"], w=[("bsa", h)])
                Q("dve", I("max_index", out=bposv[:, h, 0:8], in_max=bs[:, h, 0:8], in_values=cand[:, h, :]),
                     r=["cand", ("bsa", h)], w=[("bpa", h)])
                Q("dve", I("match_replace", out=cand2[:, h, :], in_to_replace=bs[:, h, 0:8], in_values=cand[:, h, :],
                              imm_value=NEG), r=["cand", ("bsa", h)], w=[("cand2", h), "oh"])
                Q("dve", I("max", out=bs[:, h, 8:16], in_=cand2[:, h, :]), r=[("cand2", h)], w=[("bsb", h)])
                Q("dve", I("max_index", out=bposv[:, h, 8:16], in_max=bs[:, h, 8:16], in_values=cand2[:, h, :]),
                     r=[("cand2", h), ("bsb", h)], w=[("bpb", h)])
            bs_all = [("bsa", h) for h in range(8)] + [("bsb", h) for h in range(8)]
            bp_all = [("bpa", h) for h in range(8)] + [("bpb", h) for h in range(8)]
            Q("dve", I("tensor_single_scalar", out=aidx, in_=bpos, scalar=4, op=ALU.logical_shift_right), r=bp_all, w=["aidx"])
            Q("dve", I("tensor_single_scalar", out=bidx, in_=bpos, scalar=15, op=ALU.bitwise_and), r=bp_all, w=["bidx"])
            Q("dve", I("tensor_copy", out=af_, in_=aidx), r=["aidx"], w=["af"])
            Q("dve", I("tensor_copy", out=bf_, in_=bidx), r=["bidx"], w=["bf"])
            i16fv = i16f.rearrange("p (h two k) -> p h two k", h=8, two=2)
            ohv = oh.rearrange("p (h k a) -> p h k a", h=8, k=16)
            io_b = iota16.unsqueeze(1).unsqueeze(1).to_broadcast([128, 8, 16, 16])
            for which, (posf, dst) in enumerate(((af_, i1s), (bf_, i2s))):
                pv = posf.rearrange("p (h k) -> p h k", h=8)
                Q("dve", I("tensor_tensor", out=ohv, in0=io_b, in1=pv.unsqueeze(3).to_broadcast([128, 8, 16, 16]),
                              op=ALU.is_equal), r=["iota16", "af", "bf"] + [("cand2", h) for h in range(8)], w=["oh"])
                Q("dve", I("tensor_tensor", out=ohv, in0=ohv,
                              in1=i16fv[:, :, which, :].unsqueeze(2).to_broadcast([128, 8, 16, 16]), op=ALU.mult),
                     r=["oh", "i16f"], w=["oh"])
                Q("dve", I("tensor_reduce", out=dst.rearrange("p (h k) -> p h k", h=8), in_=ohv, axis=AX.X, op=ALU.add),
                     r=["oh"], w=[("isel", which)])
            Q("dve", I("scalar_tensor_tensor", out=eidf, in0=i1s, scalar=128.0, in1=i2s, op0=ALU.mult, op1=ALU.add),
                 r=[("isel", 0), ("isel", 1)], w=["eidf"])
            Q("dve", I("tensor_copy", out=eidx[b2], in_=eidf), r=["eidf"], w=[er])
            gexv = gex.rearrange("p (h k) -> p h k", h=8)
            Q("dve", I("tensor_tensor", out=gexv, in0=bs, in1=bs[:, :, 0:1].to_broadcast([128, 8, 16]), op=ALU.subtract),
                 r=bs_all, w=["gex"])
            Q("act", I("activation", out=gex, in_=gex, func=AF.Exp), r=["gex"], w=["gex"])
            Q("dve", I("tensor_reduce", out=gsm, in_=gexv, axis=AX.X, op=ALU.add), r=["gex"], w=["gsm"])
            Q("dve", I("reciprocal", out=gsm, in_=gsm), r=["gsm"], w=["gsm"])
            Q("dve", I("tensor_tensor", out=gates[b2].rearrange("p (h k) -> p h k", h=8), in0=gexv,
                          in1=gsm.unsqueeze(2).to_broadcast([128, 8, 16]), op=ALU.mult), r=["gex", "gsm"], w=[gr])
            if dbg and t == 0:
                Q("sp", I("dma_start", out=dbg_t["d_eidx"], in_=eidx[b2]), r=[er], w=["dbg3"], dma=True, key="dbg")
                Q("sp", I("dma_start", out=dbg_t["d_gates"], in_=gates[b2]), r=[gr], w=["dbg4"], dma=True, key="dbg")
                Q("sp", I("dma_start", out=dbg_t["d_m16"], in_=m16.rearrange("p g k -> p (g k)")), r=m_all, w=["dbg7"],
                     dma=True, key="dbg")
            return fops

        def pump(fops, n=None):
            if not fops:
                return
            k = len(fops) if n is None else min(n, len(fops))
            for a, kw in fops[:k]:
                P.op(*a, **kw)
            del fops[:k]

        def BE(t, nxt):
            b2 = t % 2
            x1r, h2r, er, gr = ("x1", b2), ("h2b", b2), ("eidx", b2), ("gates", b2)

            rate = (len(nxt) / (0.85 * 256)) if nxt else 0.0
            acc = [0.0]

            def gelu_part(g):
                P.op("act", I("activation", out=gl[b2][:, g * GRP:(g + 1) * GRP], in_=araw[b2][:, g * GRP:(g + 1) * GRP],
                              func=AF.Gelu), r=[("araw", b2, j) for j in range(g * GRP, (g + 1) * GRP)], w=[("gl", b2, g)])

            def diag_part(g):
                for j in range(g * GRP, (g + 1) * GRP):
                    s_, d_ = j % NSL, j % 4
                    P.op("dve", I("tensor_scalar", out=dgb[d_], in0=ident_b, scalar1=gl[b2][:, j:j + 1],
                                  scalar2=gates[b2][:, j:j + 1], op0=ALU.mult, op1=ALU.mult),
                         r=[("gl", b2, g), gr, "ident"], w=[("dgb", d_)])
                    for hf in range(2):
                        P.op("pe", I("matmul", ps[2 + hf], lhsT=dgb[d_], rhs=UV[s_][:, D + hf * 512:D + (hf + 1) * 512],
                                     start=(j == 0), stop=(j == 127)), r=[("dgb", d_), ("UV", s_)], w=[psr[2 + hf]])
                    acc[0] += rate
                    pump(nxt, int(acc[0]))
                    acc[0] -= int(acc[0])

            for g in range(NG):
                for j in range(g * GRP, (g + 1) * GRP):
                    s_, p_ = j % NSL, j % 2
                    P.op("pool", I("indirect_dma_start", out=UV[s_], out_offset=None, in_=uvb,
                                   in_offset=bass.IndirectOffsetOnAxis(ap=eidx[b2][:, j:j + 1], axis=0)),
                         r=[er], w=[("UV", s_)], dma=True)
                    P.op("dve", I("tensor_tensor", out=prod[p_], in0=UV[s_][:, 0:D], in1=h2b[b2], op=ALU.mult),
                         r=[("UV", s_), h2r], w=[("prod", p_)])
                    P.op("act", I("activation", out=prod[p_], in_=prod[p_], func=AF.Identity, accum_out=araw[b2][:, j:j + 1]),
                         r=[("prod", p_)], w=[("prod", p_), ("araw", b2, j)])
                    acc[0] += rate
                    pump(nxt, int(acc[0]))
                    acc[0] -= int(acc[0])
                if g >= 1:
                    gelu_part(g - 1)
                if g >= 2:
                    diag_part(g - 2)
            gelu_part(NG - 1)
            diag_part(NG - 2)
            diag_part(NG - 1)
            if dbg and t == 0:
                P.op("sp", I("dma_start", out=dbg_t["d_araw"], in_=araw[b2]), r=[("araw", b2, j) for j in range(128)],
                     w=["dbg6"], dma=True, key="dbg")
            pump(nxt)
            if t + 2 < ntc:
                fe_loads(t + 2)
            for hf in range(2):
                P.op("dve", I("tensor_tensor", out=tmpo[:, hf * 512:(hf + 1) * 512], in0=ps[2 + hf],
                              in1=G2[:, hf * 512:(hf + 1) * 512], op=ALU.mult), r=[psr[2 + hf]], w=[("tmpo", hf)])
            P.op("dve", I("tensor_tensor", out=x1[b2], in0=tmpo, in1=x1[b2], op=ALU.add),
                 r=[("tmpo", 0), ("tmpo", 1), x1r], w=[x1r])
            P.op("act", I("activation", out=tmpo, in_=x1[b2], func=AF.Square, accum_out=ssC[:, 4:5]),
                 r=[x1r], w=[("tmpo", 0), ("tmpo", 1), "ssC4"])
            P.op("act", I("activation", out=ssC[:, 5:6], in_=ssC[:, 4:5], func=AF.Sqrt, scale=1.0 / D, bias=EPS),
                 r=["ssC4"], w=["ssC5"])
            P.op("dve", I("reciprocal", out=ssC[:, 6:7], in_=ssC[:, 5:6]), r=["ssC5"], w=["ssC6"])
            P.op("dve", I("scalar_tensor_tensor", out=tmpo, in0=x1[b2], scalar=ssC[:, 6:7], in1=AFr,
                          op0=ALU.mult, op1=ALU.mult), r=[x1r, "ssC6"], w=[("tmpo", 0), ("tmpo", 1)])
            P.op("dve", I("tensor_tensor", out=tmpo, in0=tmpo, in1=BFr, op=ALU.add),
                 r=[("tmpo", 0), ("tmpo", 1)], w=[("tmpo", 0), ("tmpo", 1)])
            P.op("sp", I("dma_start", out=out[t * 128:(t + 1) * 128, :], in_=tmpo),
                 r=[("tmpo", 0), ("tmpo", 1)], w=[("out_d", t)], dma=True, key="outd")

        fe_loads(0)
        pump(FE(0))
        if ntc > 1:
            fe_loads(1)
        for t in range(ntc):
            nxt = FE(t + 1) if t + 1 < ntc else None
            BE(t, nxt)
        info = P.emit(final_wait_keys=["outd"])
        A.release()
    return nc, info


def make_in_maps(inputs, S=SEQ, n_cores=N_CORES):
    f = lambda a: np.ascontiguousarray(np.asarray(a, dtype=np.float32))
    x = f(inputs["x"])
    c = f(inputs["c"])
    gn = np.concatenate([f(inputs["gn_sb_g"])[0], f(inputs["gn_fox_g"])[0]], axis=0)
    gn = np.ascontiguousarray(gn.reshape(8, 128).T)
    shared = {
        "w_ada": f(inputs["w_ada"])[0],
        "b_ada": f(inputs["b_ada"])[0].reshape(1, -1),
        "norm_mix_g": f(inputs["norm_mix_g"])[0].reshape(1, -1),
        "w_in": f(inputs["w_in"])[0],
        "b_fgate": f(inputs["b_fgate"])[0].reshape(8, 1),
        "gn": gn,
        "w_out": f(inputs["w_out"])[0],
        "norm_ffn_g": f(inputs["norm_ffn_g"])[0].reshape(1, -1),
        "w_query": f(inputs["w_query"])[0],
        "sub_keys": f(inputs["sub_keys"])[0].reshape(16, 128, 128),
        "expert_uv": np.ascontiguousarray(np.concatenate([f(inputs["expert_u"])[0], f(inputs["expert_v"])[0]], axis=1)),
        "w_ada_final": f(inputs["w_ada_final"]),
        "b_ada_final": f(inputs["b_ada_final"]).reshape(1, -1),
        "norm_final_g": f(inputs["norm_final_g"]).reshape(1, -1),
    }
    maps = []
    for b in range(n_cores):
        m = dict(shared)
        m["x"] = np.ascontiguousarray(x[b, :S])
        m["cT"] = np.ascontiguousarray(c[b].reshape(8, 128).T)
        maps.append(m)
    return maps


def kernel(**inputs):
    nc, _ = build(SEQ)
    in_maps = make_in_maps(inputs, SEQ, N_CORES)
    res = run_bass_kernel_spmd(nc, in_maps, core_ids=list(range(N_CORES)))
    return np.stack([np.asarray(r["out"], dtype=np.float32) for r in res.results], axis=0)
```

```python
import contextlib
import math

import numpy as np
import concourse.bass as bass
import concourse.mybir as mybir
from concourse.bass_utils import run_bass_kernel_spmd

F32 = mybir.dt.float32
BF16 = mybir.dt.bfloat16
I32 = mybir.dt.int32
U32 = mybir.dt.uint32
AF = mybir.ActivationFunctionType
ALU = mybir.AluOpType
AX = mybir.AxisListType

D = 1024
EPS = 1e-6
N_CORES = 8
SEQ = 4096
NEG = -1.0e30


class _Op:
    __slots__ = ("eng", "fn", "deps", "dma", "semkey", "signal", "semval", "waits")

    def __init__(self, eng, fn, deps, dma, semkey):
        self.eng = eng
        self.fn = fn
        self.deps = deps
        self.dma = dma
        self.semkey = semkey
        self.signal = False
        self.semval = 0
        self.waits = ()


class Prog:
    ENGS = ("pe", "act", "dve", "pool", "sp")

    def __init__(self, nc):
        self.nc = nc
        self.ops = []
        self.last_w = {}
        self.readers = {}
        self.pending = {e: set() for e in self.ENGS}
        self.last_eng = {}
        self.last_dma = {}

    def op(self, eng, fn, r=(), w=(), dma=False, key=None):
        idx = len(self.ops)
        deps = set()
        for res in r:
            lw = self.last_w.get(res)
            if lw is not None:
                deps.add(lw)
        for res in w:
            lw = self.last_w.get(res)
            if lw is not None:
                deps.add(lw)
            deps.update(self.readers.get(res, ()))
        if self.pending[eng]:
            deps |= self.pending[eng]
            self.pending[eng] = set()
        for res in r:
            self.readers.setdefault(res, []).append(idx)
        for res in w:
            self.last_w[res] = idx
            self.readers[res] = []
        semkey = None
        if dma:
            semkey = key if key is not None else (w[0] if w else ("dma", idx))
            self.last_dma[semkey] = idx
        else:
            self.last_eng[eng] = idx
        self.ops.append(_Op(eng, fn, deps, dma, semkey))
        return idx

    def barrier(self):
        deps = set(self.last_eng.values()) | set(self.last_dma.values())
        for e in self.ENGS:
            self.pending[e] |= deps
        self.last_w.clear()
        self.readers.clear()

    def emit(self, final_wait_keys=()):
        nc = self.nc
        ops = self.ops
        cnt = {e: 0 for e in self.ENGS}
        for o in ops:
            best = {}
            keep = set()
            for d in o.deps:
                p = ops[d]
                if p.dma:
                    keep.add(d)
                else:
                    if p.eng == "pe" and o.eng == "pe" and not o.dma:
                        continue
                    if p.eng not in best or best[p.eng] < d:
                        best[p.eng] = d
            keep.update(best.values())
            o.deps = keep
            for d in keep:
                ops[d].signal = True
        dma_cnt = {}
        dma_keys = []
        for o in ops:
            waits = {}
            for d in o.deps:
                p = ops[d]
                if p.dma:
                    waits[("d", p.semkey)] = dma_cnt[p.semkey]
                else:
                    k = ("e", p.eng)
                    if waits.get(k, 0) < p.semval:
                        waits[k] = p.semval
            o.waits = waits
            if o.dma:
                if o.semkey not in dma_cnt:
                    dma_cnt[o.semkey] = 0
                    dma_keys.append(o.semkey)
                dma_cnt[o.semkey] += 16
                o.semval = dma_cnt[o.semkey]
                o.signal = True
            elif o.signal:
                cnt[o.eng] += 1
                o.semval = cnt[o.eng]
        with contextlib.ExitStack() as st:
            esem = {e: st.enter_context(nc.semaphore("s_" + e)) for e in self.ENGS}
            dsem = {}
            for k in dma_keys:
                dsem[k] = st.enter_context(nc.semaphore("d%d" % len(dsem)))
            block = st.enter_context(nc.Block())

            def run(engname, e):
                waited = {}
                for o in ops:
                    if o.eng != engname:
                        continue
                    for k, v in o.waits.items():
                        if waited.get(k, 0) >= v:
                            continue
                        s = dsem[k[1]] if k[0] == "d" else esem[k[1]]
                        e.wait_ge(s, v)
                        waited[k] = v
                    inst = o.fn(e)
                    if o.signal:
                        if o.dma:
                            inst.then_inc(dsem[o.semkey], 16)
                        else:
                            inst.then_inc(esem[o.eng], 1)
                if engname == "sp":
                    for k in final_wait_keys:
                        e.wait_ge(dsem[k], dma_cnt[k])

            @block.tensor
            def _(e):
                run("pe", e)

            @block.scalar
            def _(e):
                run("act", e)

            @block.vector
            def _(e):
                run("dve", e)

            @block.gpsimd
            def _(e):
                run("pool", e)

            @block.sync
            def _(e):
                run("sp", e)
        return len(dsem), dict(cnt)


def I(name, *args, **kwargs):
    return lambda e: getattr(e, name)(*args, **kwargs)


class Arena:
    def __init__(self, ap, nwords):
        self.ap = ap
        self.n = nwords
        self.off = 0
        self.marks = []

    def f32(self, n):
        assert self.off + n <= self.n, ("arena overflow", self.off, n, self.n)
        a = self.ap[:, self.off:self.off + n]
        self.off += n
        return a

    def b16(self, n):
        assert n % 2 == 0
        return self.f32(n // 2).bitcast(BF16)

    def i32(self, n):
        return self.f32(n).bitcast(I32)

    def u32(self, n):
        return self.f32(n).bitcast(U32)

    def mark(self):
        self.marks.append(self.off)

    def release(self):
        self.off = self.marks.pop()


def build(S=SEQ, dbg=False, n_tok_tiles_c=None):
    NT = S // 128
    NCH = S // 512
    nc = bass.Bass("TRN2", target_bir_lowering=False)

    def din(name, shape, dt=F32):
        return nc.dram_tensor(name, list(shape), dt, kind="ExternalInput").ap()

    x = din("x", [S, D])
    cT = din("cT", [128, 8])
    w_ada = din("w_ada", [D, 6 * D])
    b_ada = din("b_ada", [1, 6 * D])
    norm_mix_g = din("norm_mix_g", [1, D])
    w_in = din("w_in", [D, 3080])
    b_fgate = din("b_fgate", [8, 1])
    gn = din("gn", [128, 8])
    w_out = din("w_out", [D, D])
    norm_ffn_g = din("norm_ffn_g", [1, D])
    w_query = din("w_query", [D, 2048])
    sub_keys = din("sub_keys", [16, 128, 128])
    expert_uv = din("expert_uv", [16384, 2 * D])
    w_ada_final = din("w_ada_final", [D, 2 * D])
    b_ada_final = din("b_ada_final", [1, 2 * D])
    norm_final_g = din("norm_final_g", [1, D])
    out = nc.dram_tensor("out", [S, D], F32, kind="ExternalOutput").ap()

    def dscr(name, shape, dt=BF16):
        return nc.dram_tensor(name, list(shape), dt, kind=("ExternalOutput" if dbg else "Internal")).ap()

    dbg_t = {}
    if dbg:
        for nm, shp, dt in (("d_mod", [128, 8 * D], F32), ("d_x1", [128, D], F32), ("d_h2", [128, D], BF16),
                            ("d_eidx", [128, 128], I32), ("d_gates", [128, 128], F32), ("d_araw", [128, 128], F32),
                            ("d_sc", [128, 2048], F32), ("d_peer", [128, D], F32), ("d_m16", [128, 256], F32)):
            dbg_t[nm] = nc.dram_tensor(nm, shp, dt, kind="ExternalOutput").ap()

    qT_d = dscr("qT_d", [8, 128, S])
    kT_d = dscr("kT_d", [8, 128, S])
    v_d = dscr("v_d", [8, 128, NT, 128])
    aug_d = dscr("aug_d", [8, 2, 6, S])
    md_d = dscr("md_d", [8, 128, S])
    uvb = nc.dram_tensor("uvb", [16384, 2 * D], BF16, kind="Internal").ap()

    P = Prog(nc)
    ARENA_WORDS = 53000
    with contextlib.ExitStack() as st:
        arena_t = st.enter_context(nc.sbuf_tensor("arena", [128, ARENA_WORDS], F32))
        A = Arena(arena_t, ARENA_WORDS)
        psbig = st.enter_context(nc.psum_tensor("psbig", [128, 8 * 512], F32))
        ps = [psbig[:, i * 512:(i + 1) * 512] for i in range(8)]
        psr = ["ps%d" % i for i in range(8)]

        ones_f = A.f32(128)
        L_f = A.f32(128)
        bd_f = A.f32(128)
        mstrict_f = A.f32(128)
        ident_b = A.b16(128)
        ones_b = A.b16(128)
        mincl_b = A.b16(128)
        iota16 = A.f32(16)
        gn_sb = A.f32(8)
        nbfg = A.f32(1)
        MOD = A.f32(6 * D)
        MODF = A.f32(2 * D)
        B1, A1, G1 = MOD[:, 0:D], MOD[:, D:2 * D], MOD[:, 2 * D:3 * D]
        B2, A2, G2 = MOD[:, 3 * D:4 * D], MOD[:, 4 * D:5 * D], MOD[:, 5 * D:6 * D]
        BFr, AFr = MODF[:, 0:D], MODF[:, D:2 * D]
        WQ_OFF = ARENA_WORDS - 8192
        WIN_OFF = ARENA_WORDS - 12320
        w_in_b = arena_t[:, WIN_OFF:ARENA_WORDS].bitcast(BF16).rearrange("p (k n) -> p k n", k=8)
        wq_b = arena_t[:, WQ_OFF:ARENA_WORDS].bitcast(BF16).rearrange("p (k n) -> p k n", k=8)
        cv_off = A.off
        cvb = [A.b16(2 * 2 * D).rearrange("p (r n) -> p r n", r=2) for _ in range(2)]
        uv_v = expert_uv.rearrange("(p r) n -> p r n", p=128)
        uvb_v = uvb.rearrange("(p r) n -> p r n", p=128)
        conv_state = {"i": 0}

        def conv_step(n):
            for _ in range(n):
                i = conv_state["i"]
                if i > 0 and i <= 64:
                    j = i - 1
                    P.op("pool", I("dma_start", out=uvb_v[:, 2 * j:2 * j + 2, :], in_=cvb[j % 2]),
                         r=[("cvb", j % 2)], w=[("uvb", j)], dma=True, key=("cvs", j % 2))
                if i < 64:
                    P.op("pool", I("dma_start", out=cvb[i % 2], in_=uv_v[:, 2 * i:2 * i + 2, :]),
                         w=[("cvb", i % 2)], dma=True, key=("cvl", i % 2))
                conv_state["i"] = i + 1

        P.op("pool", I("memset", ones_f, 1.0), w=["ones_f"])
        P.op("pool", I("memset", ones_b, 1.0), w=["ones_b"])
        P.op("pool", I("affine_select", out=L_f, in_=ones_f, pattern=[[-1, 128]], compare_op=ALU.is_ge,
                                               fill=0.0, base=0, channel_multiplier=1), r=["ones_f"], w=["L_f"])
        P.op("pool", I("affine_select", out=mstrict_f, in_=ones_f, pattern=[[1, 128]], compare_op=ALU.is_gt,
                                               fill=0.0, base=0, channel_multiplier=-1), r=["ones_f"], w=["mstrict"])
        P.op("pool", I("affine_select", out=mincl_b, in_=ones_f, pattern=[[1, 128]], compare_op=ALU.is_ge,
                                               fill=0.0, base=0, channel_multiplier=-1), r=["ones_f"], w=["mincl"])
        P.op("pool", I("affine_select", out=ident_b, in_=ones_f, pattern=[[-1, 128]], compare_op=ALU.is_equal,
                                               fill=0.0, base=0, channel_multiplier=1), r=["ones_f"], w=["ident"])
        P.op("pool", I("memset", bd_f, 0.0), w=["bd"])
        P.op("pool", I("memset", bd_f[0:64, 0:64], 1.0), w=["bd"])
        P.op("pool", I("memset", bd_f[64:128, 64:128], 1.0), w=["bd"])
        for k in range(8):
            for (c0, c1) in ((0, 1024), (1024, 2048), (2048, 3080)):
                P.op("pool", I("dma_start", out=w_in_b[:, k, c0:c1],
                                                                       in_=w_in[k * 128:(k + 1) * 128, c0:c1]),
                     w=[("w_in", k, c0)], dma=True, key="w_in")
        A.mark()
        io_i = A.i32(16)
        P.op("pool", I("iota", io_i, pattern=[[1, 16]], base=0, channel_multiplier=0), w=["io_i"])
        P.op("dve", I("tensor_copy", out=iota16, in_=io_i), r=["io_i"], w=["iota16"])
        P.op("sp", I("dma_start", out=gn_sb, in_=gn), w=["gn"], dma=True)
        bfg = A.f32(1)
        P.op("sp", I("dma_start", out=bfg[0:8, :], in_=b_fgate), w=["bfg"], dma=True)
        P.op("dve", I("tensor_scalar", out=nbfg[0:8, :], in0=bfg[0:8, :], scalar1=-1.0, scalar2=None, op0=ALU.mult),
             r=["bfg"], w=["nbfg"])

        c_sb = A.f32(8)
        c_act = A.f32(8)
        cb = A.f32(8 * 128)
        cbv = cb.rearrange("p (k m) -> p k m", k=8)
        P.op("sp", I("dma_start", out=c_sb, in_=cT), w=["c_sb"], dma=True)
        P.op("act", I("activation", out=c_act, in_=c_sb, func=AF.Silu), r=["c_sb"], w=["c_act"])
        P.op("dve", I("tensor_copy", out=cbv, in_=c_act.unsqueeze(2).to_broadcast([128, 8, 128])),
             r=["c_act"], w=["cb"])
        wst = [A.f32(8 * 512), A.f32(8 * 512)]
        bst = [A.f32(512), A.f32(512)]
        gtmp = A.f32(D)
        blocks = [(w_ada, b_ada, MOD, i) for i in range(12)] + [(w_ada_final, b_ada_final, MODF, i) for i in range(4)]
        for bi, (wsrc, bsrc, dst, i) in enumerate(blocks):
            sb = bi % 2
            wv = wst[sb].rearrange("p (k n) -> p k n", k=8)
            P.op("sp", I("dma_start",
                out=wv, in_=wsrc.rearrange("(k p) n -> p k n", p=128)[:, :, i * 512:(i + 1) * 512]),
                w=[("wst", sb)], dma=True)
            P.op("act", I("dma_start",
                out=bst[sb], in_=bsrc[0:1, i * 512:(i + 1) * 512].partition_broadcast(128)),
                w=[("bst", sb)], dma=True)
            for k in range(8):
                P.op("pe", I("matmul", ps[sb], lhsT=cbv[:, k, :], rhs=wv[:, k, :],
                                                                 start=(k == 0), stop=(k == 7)),
                     r=["cb", ("wst", sb)], w=[psr[sb]])
            P.op("dve", I("tensor_tensor", out=dst[:, i * 512:(i + 1) * 512], in0=ps[sb],
                                                                      in1=bst[sb], op=ALU.add),
                 r=[psr[sb], ("bst", sb)], w=[("mod", bi)])
        for gsrc, row, deps in ((norm_mix_g, A1, (2, 3)), (norm_ffn_g, A2, (8, 9)), (norm_final_g, AFr, (14, 15))):
            P.op("sp", I("dma_start", out=gtmp, in_=gsrc.partition_broadcast(128)), w=["gtmp"], dma=True)
            P.op("dve", I("scalar_tensor_tensor", out=row, in0=row, scalar=1.0, in1=gtmp,
                                                                 op0=ALU.add, op1=ALU.mult),
                 r=["gtmp"] + [("mod", d) for d in deps], w=[("mod", d) for d in deps])
        if dbg:
            P.op("sp", I("dma_start", out=dbg_t["d_mod"][:, 0:6 * D], in_=MOD), r=[("mod", i) for i in range(16)], w=["dbg_mod"], dma=True)
            P.op("sp", I("dma_start", out=dbg_t["d_mod"][:, 6 * D:8 * D], in_=MODF), r=[("mod", i) for i in range(16)], w=["dbg_mod"], dma=True)
        P.barrier()
        A.release()

        A.mark()
        xt = [A.f32(D) for _ in range(8)]
        junkf = A.f32(D)
        tmpf = A.f32(D)
        hb = [A.b16(D), A.b16(D)]
        hT = [A.b16(8 * 512).rearrange("p (k t) -> p k t", k=8) for _ in range(2)]
        ssA = A.f32(4)
        qkst = [A.b16(512) for _ in range(4)]
        vst = [A.b16(D) for _ in range(2)]
        spf = A.f32(512)
        ef = A.f32(512)
        Gs = [A.f32(512), A.f32(512)]
        r1 = A.f32(512)
        r2 = A.f32(512)
        ones8 = A.f32(512)
        augst = [A.b16(2 * 6 * 512).rearrange("p (q r s) -> p q r s", q=2, r=6) for _ in range(2)]
        assert A.off <= WIN_OFF, ("phase A overlaps w_in_b", A.off, WIN_OFF)
        w_in_res = [("w_in", k, c0) for k in range(8) for c0 in (0, 1024, 2048)]
        P.op("pool", I("memset", ones8[0:8, :], 1.0), w=["ones8"])
        for b in range(2):
            P.op("pool", I("memset", augst[b][0:8, 0, 3:6, :], 1.0), w=[("augst", b)])
            P.op("pool", I("memset", augst[b][0:8, 1, 0:3, :], 1.0), w=[("augst", b)])
        psT_b = ps[7].bitcast(BF16)

        def qk_col(g):
            pair, isk = g % 8, g // 8
            if pair < 4:
                return (512 if isk else 0) + pair * 128
            return (2048 if isk else 1536) + (pair - 4) * 128

        def x_loads(ch):
            for tt in range(4):
                t = ch * 4 + tt
                P.op("sp", I("dma_start", out=xt[t % 8], in_=x[t * 128:(t + 1) * 128, :]), w=[("xt", t % 8)], dma=True)

        x_loads(0)
        for ch in range(NCH):
            hb_ = ch % 2
            if ch + 1 < NCH:
                x_loads(ch + 1)
            for tt in range(4):
                t = ch * 4 + tt
                xb_ = t % 8
                P.op("act", I("activation", out=junkf, in_=xt[xb_], func=AF.Square, accum_out=ssA[:, 0:1]),
                     r=[("xt", xb_)], w=["junkf", "ssA0"])
                P.op("act", I("activation", out=ssA[:, 1:2], in_=ssA[:, 0:1], func=AF.Sqrt, scale=1.0 / D, bias=EPS),
                     r=["ssA0"], w=["ssA1"])
                P.op("dve", I("reciprocal", out=ssA[:, 2:3], in_=ssA[:, 1:2]), r=["ssA1"], w=["ssA2"])
                P.op("dve", I("scalar_tensor_tensor", out=tmpf, in0=xt[xb_], scalar=ssA[:, 2:3], in1=A1,
                                                                     op0=ALU.mult, op1=ALU.mult),
                     r=[("xt", xb_), "ssA2"], w=["tmpf"])
                P.op("dve", I("tensor_tensor", out=hb[t % 2], in0=tmpf, in1=B1, op=ALU.add),
                     r=["tmpf"], w=[("hb", t % 2)])
                for k in range(8):
                    P.op("pe", I("transpose", out=psT_b[:, k * 128:(k + 1) * 128],
                                                                   in_=hb[t % 2][:, k * 128:(k + 1) * 128], identity=ident_b),
                         r=[("hb", t % 2), "ident"], w=[psr[7]])
                P.op("act", I("copy", out=hT[hb_][:, :, tt * 128:(tt + 1) * 128],
                                                             in_=psT_b.rearrange("p (k t) -> p k t", k=8)),
                     r=[psr[7]], w=[("hT", hb_, tt)])
            hres = [("hT", hb_, tt) for tt in range(4)]
            for g in range(16):
                pair, isk = g % 8, g // 8
                col0 = qk_col(g)
                bank = g % 2
                sb_ = g % 4
                for k in range(8):
                    P.op("pe", I("matmul",
                        ps[bank], lhsT=w_in_b[:, k, col0:col0 + 128], rhs=hT[hb_][:, k, :], start=(k == 0), stop=(k == 7)),
                        r=hres + w_in_res, w=[psr[bank]])
                if g % 2 == 0:
                    P.op("act", I("activation",
                        out=qkst[sb_], in_=ps[bank], func=AF.Copy, scale=(1.0 if isk else 0.125)),
                        r=[psr[bank]], w=[("qkst", sb_)])
                else:
                    P.op("dve", I("tensor_scalar",
                        out=qkst[sb_], in0=ps[bank], scalar1=(1.0 if isk else 0.125), scalar2=None, op0=ALU.mult),
                        r=[psr[bank]], w=[("qkst", sb_)])
                dst = (kT_d if isk else qT_d)[pair, :, ch * 512:(ch + 1) * 512]
                P.op("sp", I("dma_start", out=dst, in_=qkst[sb_]),
                     r=[("qkst", sb_)], w=[("qk_d", g, ch)], dma=True, key=("qkd", sb_))
            for tt in range(4):
                t = ch * 4 + tt
                vb_ = t % 2
                for vb in range(2):
                    c0 = 1024 if vb == 0 else 2560
                    bank = 2 + vb
                    for k in range(8):
                        P.op("pe", I("matmul",
                            ps[bank], lhsT=hT[hb_][:, k, tt * 128:(tt + 1) * 128], rhs=w_in_b[:, k, c0:c0 + 512],
                            start=(k == 0), stop=(k == 7)),
                            r=hres + w_in_res, w=[psr[bank]])
                    if vb == 0:
                        P.op("act", I("copy", out=vst[vb_][:, 0:512], in_=ps[bank]),
                             r=[psr[bank]], w=[("vst", vb_, 0)])
                    else:
                        P.op("dve", I("tensor_copy", out=vst[vb_][:, 512:1024], in_=ps[bank]),
                             r=[psr[bank]], w=[("vst", vb_, 1)])
                P.op("sp", I("dma_start",
                    out=v_d.rearrange("a p n c -> p a n c")[:, :, t, :],
                    in_=vst[vb_].rearrange("p (a c) -> p a c", a=8)),
                    r=[("vst", vb_, 0), ("vst", vb_, 1)], w=[("v_d", t)], dma=True, key=("vd", vb_))
            for k in range(8):
                P.op("pe", I("matmul", ps[4][0:8, :], lhsT=w_in_b[:, k, 3072:3080], rhs=hT[hb_][:, k, :],
                                                            start=(k == 0), stop=(k == 7)),
                     r=hres + w_in_res, w=[psr[4]])
            P.op("act", I("activation", out=ef[0:8, :], in_=ps[4][0:8, :], func=AF.Exp, scale=-1.0, bias=nbfg[0:8, :]),
                 r=[psr[4], "nbfg"], w=["ef"])
            P.op("act", I("activation", out=spf[0:8, :], in_=ef[0:8, :], func=AF.Ln, bias=1.0), r=["ef"], w=["spf"])
            gb = ch % 2
            if ch == 0:
                P.op("dve", I("tensor_tensor_scan", out=Gs[gb][0:8, :], data0=ones8[0:8, :], data1=spf[0:8, :],
                                                                 initial=0.0, op0=ALU.mult, op1=ALU.add),
                     r=["ones8", "spf"], w=[("Gs", gb)])
            else:
                P.op("dve", I("tensor_tensor_scan", out=Gs[gb][0:8, :], data0=ones8[0:8, :], data1=spf[0:8, :],
                                                                 initial=Gs[1 - gb][0:8, 511:512], op0=ALU.mult, op1=ALU.add),
                     r=["ones8", "spf", ("Gs", 1 - gb)], w=[("Gs", gb)])
            ab = augst[gb]
            ar = ("augst", gb)
            P.op("dve", I("tensor_copy", out=ab[0:8, 1, 3, :], in_=Gs[gb][0:8, :]), r=[("Gs", gb)], w=[ar])
            P.op("dve", I("tensor_tensor", out=r1[0:8, :], in0=Gs[gb][0:8, :], in1=ab[0:8, 1, 3, :],
                                                               op=ALU.subtract), r=[("Gs", gb), ar], w=["r1"])
            P.op("dve", I("tensor_copy", out=ab[0:8, 1, 4, :], in_=r1[0:8, :]), r=["r1"], w=[ar])
            P.op("dve", I("tensor_tensor", out=r2[0:8, :], in0=r1[0:8, :], in1=ab[0:8, 1, 4, :], op=ALU.subtract),
                 r=["r1", ar], w=["r2"])
            P.op("dve", I("tensor_copy", out=ab[0:8, 1, 5, :], in_=r2[0:8, :]), r=["r2"], w=[ar])
            P.op("dve", I("tensor_scalar", out=ab[0:8, 0, 0:3, :], in0=ab[0:8, 1, 3:6, :], scalar1=-1.0,
                                                         scalar2=None, op0=ALU.mult), r=[ar], w=[ar])
            P.op("sp", I("dma_start", out=aug_d[:, :, :, ch * 512:(ch + 1) * 512], in_=ab[0:8, :, :, :]),
                 r=[ar], w=[("aug_d", ch)], dma=True, key=("augd", gb))
        P.barrier()
        A.release()

        A.mark()
        qTp = [A.b16(S) for _ in range(2)]
        kTp = [A.b16(S) for _ in range(2)]
        Vp = [A.b16(NT * 128).rearrange("p (n c) -> p n c", c=128) for _ in range(2)]
        augt = [A.b16(2 * S).rearrange("p (q s) -> p q s", q=2) for _ in range(2)]
        eb_off = A.off
        ebuf = [[A.f32(512) for _ in range(2)] for _ in range(2)]
        spbuf = [[A.f32(512) for _ in range(2)] for _ in range(2)]
        Vx = [arena_t[:, eb_off + 2048 * b_:eb_off + 2048 * b_ + S // 2].bitcast(BF16).rearrange("p (n c) -> p n c", c=128)
              for b_ in range(2)]
        VX_RES = [[("ebuf", h_, k_) for h_ in range(2) for k_ in range(2)],
                  [("spbuf", h_, k_) for h_ in range(2) for k_ in range(2)]]
        rbuf = [A.f32(512) for _ in range(2)]
        abuf_pair = [A.b16(1024) for _ in range(3)]
        abuf = [[abuf_pair[k_][:, h_ * 512:(h_ + 1) * 512] for k_ in range(3)] for h_ in range(2)]
        Sacc = [A.f32(512) for _ in range(2)]
        on_sb = A.f32(512)
        sq_sb = A.f32(512)
        rt_sb = A.f32(512)
        mst = [A.b16(512) for _ in range(2)]
        ZB = (0, 1)
        IB = (2, 3)
        OB = (4, 5)
        LB = (6, 7)

        def pair_loads(pair):
            fox = pair >= 4
            sb = pair % 2
            if not fox:
                P.op("sp", I("dma_start", out=qTp[sb], in_=qT_d[pair]), w=[("qTp", sb, 0), ("qTp", sb, 1)], dma=True,
                     key=("qTp", sb))
                P.op("sp", I("dma_start", out=kTp[sb], in_=kT_d[pair]), w=[("kTp", sb, 0), ("kTp", sb, 1)], dma=True,
                     key=("kTp", sb))
            else:
                hA, hB = 2 * (pair - 4), 2 * (pair - 4) + 1
                for (dst, src_d, qk, nm) in ((qTp[sb], qT_d, 0, "qTp"), (kTp[sb], kT_d, 1, "kTp")):
                    P.op("sp", I("dma_start", out=dst[0:64, :], in_=src_d[pair, 0:64, :]), w=[(nm, sb, 0)], dma=True,
                         key=(nm, sb, "m"))
                    P.op("pool", I("memset", dst[64:128, :], 0.0), w=[(nm, sb, 1)])
                    P.op("sp", I("dma_start", out=dst[64:70, :], in_=aug_d[hA, qk]), w=[(nm, sb, 1)], dma=True,
                         key=(nm, sb, "a"))
                for (src_d, qk) in ((qT_d, 0), (kT_d, 1)):
                    P.op("sp", I("dma_start", out=augt[sb][0:64, qk, :], in_=src_d[pair, 64:128, :]),
                         w=[("augt", sb, qk, 0)], dma=True, key=("augt", sb, qk, "m"))
                    P.op("pool", I("memset", augt[sb][64:128, qk, :], 0.0), w=[("augt", sb, qk, 1)])
                    P.op("sp", I("dma_start", out=augt[sb][64:70, qk, :], in_=aug_d[hB, qk]),
                         w=[("augt", sb, qk, 1)], dma=True, key=("augt", sb, qk, "a"))
            P.op("sp", I("dma_start", out=Vp[sb], in_=v_d[pair]), w=[("Vp", sb)], dma=True)

        pending_epi = []

        def flush_epi(part=None):
            for epi in pending_epi:
                k = next(i for i, (a, kw) in enumerate(epi) if a[0] == "pe") if epi else 0
                if part in (0, None):
                    for a, kw in epi[:k]:
                        P.op(*a, **kw)
                    del epi[:k]
                if part in (1, None):
                    for a, kw in epi:
                        P.op(*a, **kw)
                    del epi[:]
            if part in (1, None):
                del pending_epi[:]

        pair_loads(0)
        for pair in range(8):
            fox = pair >= 4
            sb = pair % 2
            if pair + 1 < 8:
                pair_loads(pair + 1)
            if fox:
                P.op("pool", I("memset", Vx[sb][:, :, 0:64], 1.0), w=VX_RES[sb])
                P.op("pool", I("tensor_copy", out=Vx[sb][:, :, 64:128], in_=Vp[sb][:, :, 64:128]),
                     r=[("Vp", sb)], w=VX_RES[sb])
                P.op("pool", I("memset", Vp[sb][:, :, 64:128], 1.0), w=[("Vp", sb)])
            v_r = ("Vp", sb)
            for c in range(NCH):
                nk = 4 * (c + 1)
                order = list(range(nk)) if fox else list(range(nk - 1, -1, -1))
                gc = pair * NCH + c
                ob = OB[gc % 2]
                lb = LB[gc % 2]
                conv_step(-(-65 // (8 * NCH)))
                if pair == 7 and c == 0:
                    assert A.off <= WQ_OFF, ("phase B overlaps wq_b", A.off, WQ_OFF)
                    for k in range(8):
                        for h in range(2):
                            P.op("pool", I("dma_start", out=wq_b[:, k, h * 1024:(h + 1) * 1024],
                                           in_=w_query[k * 128:(k + 1) * 128, h * 1024:(h + 1) * 1024]),
                                 w=[("wq", k, h)], dma=True, key="wq")
                if not fox:
                    for hh in range(2):
                        P.op("pool", I("memset", Sacc[hh], 0.0), w=[("S", hh)])

                def rng(kb):
                    j = kb - 4 * c
                    q0 = 128 * j if j > 0 else 0
                    return j, q0

                def emit_qk(i):
                    kb = order[i]
                    j, q0 = rng(kb)
                    for hh in range(2):
                        pb = 64 * hh
                        zb = (ZB[hh], IB[hh])[i % 2] if fox else ZB[hh]
                        if not fox:
                            P.op("pe", I("matmul", ps[zb][:, q0:512], lhsT=kTp[sb][pb:pb + 64, kb * 128:(kb + 1) * 128],
                                         rhs=qTp[sb][pb:pb + 64, c * 512 + q0:(c + 1) * 512], start=True, stop=True),
                                 r=[("qTp", sb, 0), ("qTp", sb, 1), ("kTp", sb, 0), ("kTp", sb, 1)], w=[psr[zb]])
                        else:
                            qs = qTp[sb] if hh == 0 else augt[sb][:, 0, :]
                            ks = kTp[sb] if hh == 0 else augt[sb][:, 1, :]
                            rr = ([("qTp", sb, 0), ("qTp", sb, 1), ("kTp", sb, 0), ("kTp", sb, 1)] if hh == 0 else
                                  [("augt", sb, 0, 0), ("augt", sb, 0, 1), ("augt", sb, 1, 0), ("augt", sb, 1, 1)])
                            P.op("pe", I("matmul", ps[zb][:, q0:512], lhsT=ks[:, kb * 128:(kb + 1) * 128],
                                         rhs=qs[:, c * 512 + q0:(c + 1) * 512], start=True, stop=True),
                                 r=rr, w=[psr[zb]])

                def emit_exp(i):
                    kb = order[i]
                    j, q0 = rng(kb)
                    if fox:
                        zA = (ZB[0], IB[0])[i % 2]
                        P.op("act", I("activation",
                                      out=abuf_pair[i % 3].rearrange("p (h n) -> p h n", h=2)[:, :, q0:512],
                                      in_=psbig[:, zA * 512:(zA + 2) * 512].rearrange("p (h n) -> p h n", h=2)[:, :, q0:512],
                                      func=AF.Exp),
                             r=[psr[zA], psr[zA + 1]], w=[("abuf", 0, i % 3), ("abuf", 1, i % 3)])
                    for hh in range(2):
                        zb = (ZB[hh], IB[hh])[i % 2] if fox else ZB[hh]
                        if fox:
                            pbuf = abuf[hh][i % 3]
                            pr = ("abuf", hh, i % 3)
                            if j >= 0:
                                P.op("pool", I("affine_select", out=pbuf[:, q0:q0 + 128], in_=pbuf[:, q0:q0 + 128],
                                               pattern=[[1, 128]], compare_op=ALU.is_ge, fill=0.0, base=0,
                                               channel_multiplier=-1), r=[pr], w=[pr])
                        else:
                            eb = ebuf[hh][i % 2]
                            er = ("ebuf", hh, i % 2)
                            P.op("act", I("activation",
                                out=eb[:, q0:512], in_=ps[zb][:, q0:512], func=AF.Exp), r=[psr[zb]], w=[er])
                            if j >= 0:
                                P.op("dve", I("tensor_tensor",
                                    out=eb[:, q0:q0 + 128], in0=eb[:, q0:q0 + 128], in1=mstrict_f, op=ALU.mult),
                                    r=[er, "mstrict"], w=[er])

                def emit_ln(i):
                    kb = order[i]
                    j, q0 = rng(kb)
                    for hh in range(2):
                        eb, sp_ = ebuf[hh][i % 2], spbuf[hh][i % 2]
                        P.op("act", I("activation",
                            out=sp_[:, q0:512], in_=eb[:, q0:512], func=AF.Ln, bias=1.0),
                            r=[("ebuf", hh, i % 2)], w=[("spbuf", hh, i % 2)])

                def emit_cum(i):
                    kb = order[i]
                    j, q0 = rng(kb)
                    for hh in range(2):
                        sp_ = spbuf[hh][i % 2]
                        ib = IB[hh]
                        P.op("pe", I("matmul",
                            ps[ib][:, q0:512], lhsT=L_f, rhs=sp_[:, q0:512], start=True, stop=(i == 0)),
                            r=[("spbuf", hh, i % 2), "L_f"], w=[psr[ib]])
                        if i > 0:
                            P.op("pe", I("matmul",
                                ps[ib][:, q0:512], lhsT=ones_f, rhs=Sacc[hh][:, q0:512], start=False, stop=True),
                                r=[("S", hh), "ones_f"], w=[psr[ib]])
                        if i < nk - 1:
                            P.op("pool", I("tensor_tensor",
                                out=Sacc[hh][:, q0:512], in0=Sacc[hh][:, q0:512], in1=sp_[:, q0:512], op=ALU.add),
                                r=[("spbuf", hh, i % 2), ("S", hh)], w=[("S", hh)])

                def emit_neg(i):
                    kb = order[i]
                    j, q0 = rng(kb)
                    for hh in range(2):
                        ib = IB[hh]
                        P.op("act", I("activation",
                            out=rbuf[hh][:, q0:512], in_=ps[ib][:, q0:512], func=AF.Exp, scale=-1.0),
                            r=[psr[ib]], w=[("rbuf", hh)])
                        eb, ab_ = ebuf[hh][i % 2], abuf[hh][i % 3]
                        P.op("dve", I("tensor_tensor",
                            out=ab_[:, q0:512], in0=eb[:, q0:512], in1=rbuf[hh][:, q0:512], op=ALU.mult),
                            r=[("ebuf", hh, i % 2), ("rbuf", hh)], w=[("abuf", hh, i % 3)])

                def emit_av(i):
                    kb = order[i]
                    j, q0 = rng(kb)
                    for hh in range(2):
                        pb = 64 * hh
                        ab_ = abuf[hh][i % 3]
                        if fox:
                            bank = (ob, lb)[hh]
                            lhs = (Vp[sb], Vx[sb])[hh]
                            P.op("pe", I("matmul", ps[bank][:, q0:512], lhsT=lhs[:, kb, :], rhs=ab_[:, q0:512],
                                         start=(i == 0), stop=(i == nk - 1), skip_group_check=True),
                                 r=[("abuf", hh, i % 3), v_r] + (VX_RES[sb] if hh == 1 else []),
                                 w=[(psr[bank], 0), (psr[bank], 1)])
                        else:
                            P.op("pe", I("matmul", ps[ob][pb:pb + 64, q0:512], lhsT=Vp[sb][:, kb, pb:pb + 64],
                                         rhs=ab_[:, q0:512], start=(i == 0), stop=(i == nk - 1), skip_group_check=True),
                                 r=[("abuf", hh, i % 3), v_r], w=[(psr[ob], hh)])

                emit_qk(0)
                for i in range(nk):
                    emit_exp(i)
                    if i + 1 < nk:
                        emit_qk(i + 1)
                    if i == 0:
                        flush_epi(0)
                    if i == 2:
                        flush_epi(1)
                    if fox:
                        if i > 0:
                            emit_av(i - 1)
                    else:
                        emit_ln(i)
                        if i > 0:
                            emit_neg(i - 1)
                        emit_cum(i)
                        if i > 0:
                            emit_av(i - 1)
                if not fox:
                    emit_neg(nk - 1)
                emit_av(nk - 1)

                epi = []
                o_r = [(psr[ob], 0), (psr[ob], 1)]
                l_r = [(psr[lb], 0), (psr[lb], 1)]
                if fox:
                    epi.append((("act", I("activation", out=rt_sb[0:64, :], in_=ps[ob][64:128, :], func=AF.Ln)),
                                dict(r=o_r, w=["rt_sb"])))
                    epi.append((("act", I("activation", out=rt_sb[64:128, :], in_=ps[lb][0:64, :], func=AF.Ln)),
                                dict(r=l_r, w=["rt_sb"])))
                    epi.append((("act", I("activation", out=rt_sb, in_=rt_sb, func=AF.Exp, scale=-1.0)),
                                dict(r=["rt_sb"], w=["rt_sb"])))
                    epi.append((("dve", I("tensor_tensor", out=on_sb[0:64, :], in0=ps[ob][0:64, :], in1=rt_sb[0:64, :],
                                          op=ALU.mult)), dict(r=o_r + ["rt_sb"], w=["on_sb"])))
                    epi.append((("dve", I("tensor_tensor", out=on_sb[64:128, :], in0=ps[lb][64:128, :],
                                          in1=rt_sb[64:128, :], op=ALU.mult)), dict(r=l_r + ["rt_sb"], w=["on_sb"])))
                else:
                    epi.append((("act", I("copy", out=on_sb, in_=ps[ob])), dict(r=o_r, w=["on_sb"])))
                epi.append((("dve", I("tensor_tensor", out=sq_sb, in0=on_sb, in1=on_sb, op=ALU.mult)),
                            dict(r=["on_sb"], w=["sq_sb"])))
                epi.append((("pe", I("matmul", ps[lb], lhsT=bd_f, rhs=sq_sb, start=True, stop=True)),
                            dict(r=["sq_sb", "bd"], w=l_r)))
                epi.append((("act", I("activation", out=sq_sb, in_=ps[lb], func=AF.Ln, scale=1.0 / 64, bias=EPS)),
                            dict(r=l_r, w=["sq_sb"])))
                epi.append((("act", I("activation", out=rt_sb, in_=sq_sb, func=AF.Exp, scale=-0.5)),
                            dict(r=["sq_sb"], w=["rt_sb"])))
                mb = gc % 2
                epi.append((("dve", I("scalar_tensor_tensor", out=mst[mb], in0=on_sb, scalar=gn_sb[:, pair:pair + 1],
                                      in1=rt_sb, op0=ALU.mult, op1=ALU.mult)),
                            dict(r=["on_sb", "rt_sb", "gn"], w=[("mst", mb)])))
                epi.append((("sp", I("dma_start", out=md_d[pair, :, c * 512:(c + 1) * 512], in_=mst[mb])),
                            dict(r=[("mst", mb)], w=[("md_d", pair, c)], dma=True, key=("mdd", mb))))
                pending_epi.append(epi)
        flush_epi()
        conv_step(66)
        P.barrier()
        A.release()

        A.mark()
        A.off = cv_off
        w_out_b = A.b16(8 * D).rearrange("p (k n) -> p k n", k=8)
        skT = A.b16(16 * 128).rearrange("p (g n) -> p g n", g=16)
        NSL = 16
        GRP = 2
        NG = 128 // GRP
        UV = [A.b16(2 * D) for _ in range(NSL)]
        mdt = A.b16(8 * 128).rearrange("p (k t) -> p k t", k=8)
        xc = A.f32(D)
        x1 = [A.f32(D), MOD[:, D:2 * D]]
        tmpc = A.f32(D)
        tmpo = MOD[:, 0:D]
        h2b = [A.b16(D) for _ in range(2)]
        h2T = A.b16(8 * 128).rearrange("p (k t) -> p k t", k=8)
        sc_raw = A.f32(2048)
        sc = sc_raw.rearrange("p (g n) -> p g n", g=16)
        qb = sc_raw[:, 0:1024].bitcast(BF16)
        qpT = sc_raw[:, 1024:2048].bitcast(BF16).rearrange("p (g t) -> p g t", g=16)
        sc2s = [A.f32(128) for _ in range(2)]
        m16 = A.f32(256).rearrange("p (g k) -> p g k", g=16)
        i16 = A.u32(256).rearrange("p (g k) -> p g k", g=16)
        i16f = A.f32(256)
        cand = sc_raw.rearrange("p (h q) -> p h q", h=8)
        oh = A.f32(2048)
        cand2 = oh.rearrange("p (h q) -> p h q", h=8)
        bs = A.f32(128).rearrange("p (h k) -> p h k", h=8)
        bpos = A.u32(128)
        aidx = A.u32(128)
        bidx = A.u32(128)
        af_ = A.f32(128)
        bf_ = A.f32(128)
        i1s = A.f32(128)
        i2s = A.f32(128)
        eidf = A.f32(128)
        eidx = [A.i32(128) for _ in range(2)]
        gex = A.f32(128)
        gsm = A.f32(8)
        gates = [A.f32(128) for _ in range(2)]
        araw = [A.f32(128) for _ in range(2)]
        gl = [A.f32(128) for _ in range(2)]
        prod = [A.b16(D) for _ in range(2)]
        dgb = [A.b16(128) for _ in range(4)]
        ssC = A.f32(16)
        skst = sc_raw.rearrange("p (g c) -> p g c", g=16)
        assert A.off <= WQ_OFF, ("phase C overlaps wq_b", A.off, WQ_OFF)
        skb = oh[:, 0:1024].bitcast(BF16).rearrange("p (g c) -> p g c", g=16)

        for k in range(8):
            P.op("pool", I("dma_start", out=w_out_b[:, k, :], in_=w_out[k * 128:(k + 1) * 128, :]),
                 w=[("w_out", k)], dma=True, key="wc")
        wo_res = [("w_out", k) for k in range(8)]
        wq_res = []
        P.op("sp", I("dma_start", out=skst, in_=sub_keys.rearrange("g n c -> n g c")), w=["skst"], dma=True)
        P.op("dve", I("tensor_copy", out=skb, in_=skst), r=["skst"], w=["skb"])
        pT = [ps[4].bitcast(BF16), ps[5].bitcast(BF16)]
        for g in range(16):
            P.op("pe", I("transpose", out=pT[g // 8][:, (g % 8) * 128:(g % 8 + 1) * 128], in_=skb[:, g, :],
                                                  identity=ident_b), r=["skb", "ident"], w=[psr[4 + g // 8]])
        for hf in range(2):
            P.op("act", I("copy", out=skT[:, hf * 8:(hf + 1) * 8, :],
                                                in_=pT[hf].rearrange("p (g n) -> p g n", g=8)),
                 r=[psr[4 + hf]], w=[("skT", hf)])
        sk_res = [("skT", 0), ("skT", 1)]
        ntc = NT if n_tok_tiles_c is None else n_tok_tiles_c

        def fe_loads(t):
            P.op("sp", I("dma_start", out=xc, in_=x[t * 128:(t + 1) * 128, :]), w=["xc"], dma=True)
            P.op("sp", I("dma_start", out=mdt, in_=md_d.rearrange("a p s -> p a s")[:, :, t * 128:(t + 1) * 128]),
                 w=["mdt"], dma=True)

        def FE(t):
            b2 = t % 2
            x1r, h2r, er, gr = ("x1", b2), ("h2b", b2), ("eidx", b2), ("gates", b2)
            fops = []

            def Q(*a, **k):
                fops.append((a, k))
            for hf in range(2):
                for k in range(8):
                    Q("pe", I("matmul", ps[hf], lhsT=mdt[:, k, :], rhs=w_out_b[:, k, hf * 512:(hf + 1) * 512],
                                 start=(k == 0), stop=(k == 7)), r=["mdt"] + wo_res, w=[psr[hf]])
                Q("dve", I("tensor_tensor", out=tmpc[:, hf * 512:(hf + 1) * 512], in0=ps[hf],
                              in1=G1[:, hf * 512:(hf + 1) * 512], op=ALU.mult), r=[psr[hf]], w=[("tmpc", hf)])
            Q("dve", I("tensor_tensor", out=x1[b2], in0=tmpc, in1=xc, op=ALU.add),
                 r=[("tmpc", 0), ("tmpc", 1), "xc"], w=[x1r])
            Q("act", I("activation", out=tmpc, in_=x1[b2], func=AF.Square, accum_out=ssC[:, 0:1]),
                 r=[x1r], w=[("tmpc", 0), ("tmpc", 1), "ssC0"])
            Q("act", I("activation", out=ssC[:, 1:2], in_=ssC[:, 0:1], func=AF.Sqrt, scale=1.0 / D, bias=EPS),
                 r=["ssC0"], w=["ssC1"])
            Q("dve", I("reciprocal", out=ssC[:, 2:3], in_=ssC[:, 1:2]), r=["ssC1"], w=["ssC2"])
            Q("dve", I("scalar_tensor_tensor", out=tmpc, in0=x1[b2], scalar=ssC[:, 2:3], in1=A2,
                          op0=ALU.mult, op1=ALU.mult), r=[x1r, "ssC2"], w=[("tmpc", 0), ("tmpc", 1)])
            Q("dve", I("tensor_tensor", out=h2b[b2], in0=tmpc, in1=B2, op=ALU.add),
                 r=[("tmpc", 0), ("tmpc", 1)], w=[h2r])
            for k in range(8):
                Q("pe", I("transpose", out=pT[0][:, k * 128:(k + 1) * 128], in_=h2b[b2][:, k * 128:(k + 1) * 128],
                             identity=ident_b), r=[h2r, "ident"], w=[psr[4]])
            Q("act", I("copy", out=h2T, in_=pT[0].rearrange("p (k t) -> p k t", k=8)), r=[psr[4]], w=["h2T"])
            if dbg and t == 0:
                Q("sp", I("dma_start", out=dbg_t["d_x1"], in_=x1[b2]), r=[x1r], w=["dbg1"], dma=True, key="dbg")
                Q("sp", I("dma_start", out=dbg_t["d_h2"], in_=h2b[b2]), r=[h2r], w=["dbg2"], dma=True, key="dbg")
            for blk in range(4):
                bank = 4 + blk
                for k in range(8):
                    Q("pe", I("matmul", ps[bank], lhsT=h2T[:, k, :], rhs=wq_b[:, k, blk * 512:(blk + 1) * 512],
                                 start=(k == 0), stop=(k == 7)), r=["h2T"] + wq_res, w=[psr[bank]])
                if blk % 2 == 0:
                    Q("act", I("copy", out=qb[:, blk * 512:(blk + 1) * 512], in_=ps[bank]), r=[psr[bank]], w=[("qb", blk)])
                else:
                    Q("dve", I("tensor_copy", out=qb[:, blk * 512:(blk + 1) * 512], in_=ps[bank]),
                         r=[psr[bank]], w=[("qb", blk)])
            for g in range(16):
                Q("pe", I("transpose", out=pT[g // 8][:, (g % 8) * 128:(g % 8 + 1) * 128],
                             in_=qb[:, g * 128:(g + 1) * 128], identity=ident_b),
                     r=[("qb", g // 4), "ident"], w=[psr[4 + g // 8]])
            Q("act", I("copy", out=qpT[:, 0:8, :], in_=pT[0].rearrange("p (g n) -> p g n", g=8)),
                 r=[psr[4]], w=[("qpT", 0)])
            Q("dve", I("tensor_copy", out=qpT[:, 8:16, :], in_=pT[1].rearrange("p (g n) -> p g n", g=8)),
                 r=[psr[5]], w=[("qpT", 1)])
            for g in range(16):
                bank = 4 + g // 4
                Q("pe", I("matmul", ps[bank][:, (g % 4) * 128:(g % 4 + 1) * 128], lhsT=qpT[:, g, :], rhs=skT[:, g, :],
                             start=True, stop=True), r=[("qpT", g // 8)] + sk_res, w=[psr[bank]])
            for q4 in range(4):
                if q4 % 2 == 0:
                    Q("act", I("copy", out=sc[:, q4 * 4:(q4 + 1) * 4, :], in_=ps[4 + q4].rearrange("p (g n) -> p g n", g=4)),
                         r=[psr[4 + q4], ("qpT", 0), ("qpT", 1)] + [("qb", b_) for b_ in range(4)], w=[("sc", q4), "cand"])
                else:
                    Q("dve", I("tensor_copy", out=sc[:, q4 * 4:(q4 + 1) * 4, :],
                                  in_=ps[4 + q4].rearrange("p (g n) -> p g n", g=4)),
                         r=[psr[4 + q4], ("qpT", 0), ("qpT", 1)] + [("qb", b_) for b_ in range(4)], w=[("sc", q4), "cand"])
            for g in range(16):
                sr = ("sc", g // 4)
                s2 = sc2s[g % 2]
                s2r = ("sc2", g % 2)
                Q("dve", I("max", out=m16[:, g, 0:8], in_=sc[:, g, :]), r=[sr], w=[("m16a", g)])
                Q("dve", I("max_index", out=i16[:, g, 0:8], in_max=m16[:, g, 0:8], in_values=sc[:, g, :]),
                     r=[sr, ("m16a", g)], w=[("i16a", g)])
                Q("dve", I("match_replace", out=s2, in_to_replace=m16[:, g, 0:8], in_values=sc[:, g, :], imm_value=NEG),
                     r=[sr, ("m16a", g)], w=[s2r])
                Q("dve", I("max", out=m16[:, g, 8:16], in_=s2), r=[s2r], w=[("m16b", g)])
                Q("dve", I("max_index", out=i16[:, g, 8:16], in_max=m16[:, g, 8:16], in_values=s2),
                     r=[s2r, ("m16b", g)], w=[("i16b", g)])
            m_all = [("m16a", g) for g in range(16)] + [("m16b", g) for g in range(16)]
            i_all = [("i16a", g) for g in range(16)] + [("i16b", g) for g in range(16)]
            Q("dve", I("tensor_copy", out=i16f, in_=i16.rearrange("p g k -> p (g k)")), r=i_all, w=["i16f"])
            m16v = m16.rearrange("p (h two) k -> p h two k", two=2)
            candv = cand.rearrange("p h (a b) -> p h a b", a=16)
            Q("dve", I("tensor_tensor", out=candv, in0=m16v[:, :, 0, :].unsqueeze(3).to_broadcast([128, 8, 16, 16]),
                          in1=m16v[:, :, 1, :].unsqueeze(2).to_broadcast([128, 8, 16, 16]), op=ALU.add),
                 r=m_all, w=["cand"] + [("sc", q4) for q4 in range(4)])
            bposv = bpos.rearrange("p (h k) -> p h k", h=8)
            for h in range(8):
                Q("dve", I("max", out=bs[:, h, 0:8], in_=cand[:, h, :]), r=["cand"], w=[("bsa", h)])
                Q("dve", I("max_index", out=bposv[:, h, 0:8], in_max=bs[:, h, 0:8], in_values=cand[:, h, :]),
                     r=["cand", ("bsa", h)], w=[("bpa", h)])
                Q("dve", I("match_replace", out=cand2[:, h, :], in_to_replace=bs[:, h, 0:8], in_values=cand[:, h, :],
                              imm_value=NEG), r=["cand", ("bsa", h)], w=[("cand2", h), "oh"])
                Q("dve", I("max", out=bs[:, h, 8:16], in_=cand2[:, h, :]), r=[("cand2", h)], w=[("bsb", h)])
                Q("dve", I("max_index", out=bposv[:, h, 8:16], in_max=bs[:, h, 8:16], in_values=cand2[:, h, :]),
                     r=[("cand2", h), ("bsb", h)], w=[("bpb", h)])
            bs_all = [("bsa", h) for h in range(8)] + [("bsb", h) for h in range(8)]
            bp_all = [("bpa", h) for h in range(8)] + [("bpb", h) for h in range(8)]
            Q("dve", I("tensor_single_scalar", out=aidx, in_=bpos, scalar=4, op=ALU.logical_shift_right), r=bp_all, w=["aidx"])
            Q("dve", I("tensor_single_scalar", out=bidx, in_=bpos, scalar=15, op=ALU.bitwise_and), r=bp_all, w=["bidx"])
            Q("dve", I("tensor_copy", out=af_, in_=aidx), r=["aidx"], w=["af"])
            Q("dve", I("tensor_copy", out=bf_, in_=bidx), r=["bidx"], w=["bf"])
            i16fv = i16f.rearrange("p (h two k) -> p h two k", h=8, two=2)
            ohv = oh.rearrange("p (h k a) -> p h k a", h=8, k=16)
            io_b = iota16.unsqueeze(1).unsqueeze(1).to_broadcast([128, 8, 16, 16])
            for which, (posf, dst) in enumerate(((af_, i1s), (bf_, i2s))):
                pv = posf.rearrange("p (h k) -> p h k", h=8)
                Q("dve", I("tensor_tensor", out=ohv, in0=io_b, in1=pv.unsqueeze(3).to_broadcast([128, 8, 16, 16]),
                              op=ALU.is_equal), r=["iota16", "af", "bf"] + [("cand2", h) for h in range(8)], w=["oh"])
                Q("dve", I("tensor_tensor", out=ohv, in0=ohv,
                              in1=i16fv[:, :, which, :].unsqueeze(2).to_broadcast([128, 8, 16, 16]), op=ALU.mult),
                     r=["oh", "i16f"], w=["oh"])
                Q("dve", I("tensor_reduce", out=dst.rearrange("p (h k) -> p h k", h=8), in_=ohv, axis=AX.X, op=ALU.add),
                     r=["oh"], w=[("isel", which)])
            Q("dve", I("scalar_tensor_tensor", out=eidf, in0=i1s, scalar=128.0, in1=i2s, op0=ALU.mult, op1=ALU.add),
                 r=[("isel", 0), ("isel", 1)], w=["eidf"])
            Q("dve", I("tensor_copy", out=eidx[b2], in_=eidf), r=["eidf"], w=[er])
            gexv = gex.rearrange("p (h k) -> p h k", h=8)
            Q("dve", I("tensor_tensor", out=gexv, in0=bs, in1=bs[:, :, 0:1].to_broadcast([128, 8, 16]), op=ALU.subtract),
                 r=bs_all, w=["gex"])
            Q("act", I("activation", out=gex, in_=gex, func=AF.Exp), r=["gex"], w=["gex"])
            Q("dve", I("tensor_reduce", out=gsm, in_=gexv, axis=AX.X, op=ALU.add), r=["gex"], w=["gsm"])
            Q("dve", I("reciprocal", out=gsm, in_=gsm), r=["gsm"], w=["gsm"])
            Q("dve", I("tensor_tensor", out=gates[b2].rearrange("p (h k) -> p h k", h=8), in0=gexv,
                          in1=gsm.unsqueeze(2).to_broadcast([128, 8, 16]), op=ALU.mult), r=["gex", "gsm"], w=[gr])
            if dbg and t == 0:
                Q("sp", I("dma_start", out=dbg_t["d_eidx"], in_=eidx[b2]), r=[er], w=["dbg3"], dma=True, key="dbg")
                Q("sp", I("dma_start", out=dbg_t["d_gates"], in_=gates[b2]), r=[gr], w=["dbg4"], dma=True, key="dbg")
                Q("sp", I("dma_start", out=dbg_t["d_m16"], in_=m16.rearrange("p g k -> p (g k)")), r=m_all, w=["dbg7"],
                     dma=True, key="dbg")
            return fops

        def pump(fops, n=None):
            if not fops:
                return
            k = len(fops) if n is None else min(n, len(fops))
            for a, kw in fops[:k]:
                P.op(*a, **kw)
            del fops[:k]

        def BE(t, nxt):
            b2 = t % 2
            x1r, h2r, er, gr = ("x1", b2), ("h2b", b2), ("eidx", b2), ("gates", b2)

            rate = (len(nxt) / (0.85 * 256)) if nxt else 0.0
            acc = [0.0]

            def gelu_part(g):
                P.op("act", I("activation", out=gl[b2][:, g * GRP:(g + 1) * GRP], in_=araw[b2][:, g * GRP:(g + 1) * GRP],
                              func=AF.Gelu), r=[("araw", b2, j) for j in range(g * GRP, (g + 1) * GRP)], w=[("gl", b2, g)])

            def diag_part(g):
                for j in range(g * GRP, (g + 1) * GRP):
                    s_, d_ = j % NSL, j % 4
                    P.op("dve", I("tensor_scalar", out=dgb[d_], in0=ident_b, scalar1=gl[b2][:, j:j + 1],
                                  scalar2=gates[b2][:, j:j + 1], op0=ALU.mult, op1=ALU.mult),
                         r=[("gl", b2, g), gr, "ident"], w=[("dgb", d_)])
                    for hf in range(2):
                        P.op("pe", I("matmul", ps[2 + hf], lhsT=dgb[d_], rhs=UV[s_][:, D + hf * 512:D + (hf + 1) * 512],
                                     start=(j == 0), stop=(j == 127)), r=[("dgb", d_), ("UV", s_)], w=[psr[2 + hf]])
                    acc[0] += rate
                    pump(nxt, int(acc[0]))
                    acc[0] -= int(acc[0])

            for g in range(NG):
                for j in range(g * GRP, (g + 1) * GRP):
                    s_, p_ = j % NSL, j % 2
                    P.op("pool", I("indirect_dma_start", out=UV[s_], out_offset=None, in_=uvb,
                                   in_offset=bass.IndirectOffsetOnAxis(ap=eidx[b2][:, j:j + 1], axis=0)),
                         r=[er], w=[("UV", s_)], dma=True)
                    P.op("dve", I("tensor_tensor", out=prod[p_], in0=UV[s_][:, 0:D], in1=h2b[b2], op=ALU.mult),
                         r=[("UV", s_), h2r], w=[("prod", p_)])
                    P.op("act", I("activation", out=prod[p_], in_=prod[p_], func=AF.Identity, accum_out=araw[b2][:, j:j + 1]),
                         r=[("prod", p_)], w=[("prod", p_), ("araw", b2, j)])
                    acc[0] += rate
                    pump(nxt, int(acc[0]))
                    acc[0] -= int(acc[0])
                if g >= 1:
                    gelu_part(g - 1)
                if g >= 2:
                    diag_part(g - 2)
            gelu_part(NG - 1)
            diag_part(NG - 2)
            diag_part(NG - 1)
            if dbg and t == 0:
                P.op("sp", I("dma_start", out=dbg_t["d_araw"], in_=araw[b2]), r=[("araw", b2, j) for j in range(128)],
                     w=["dbg6"], dma=True, key="dbg")
            pump(nxt)
            if t + 2 < ntc:
                fe_loads(t + 2)
            for hf in range(2):
                P.op("dve", I("tensor_tensor", out=tmpo[:, hf * 512:(hf + 1) * 512], in0=ps[2 + hf],
                              in1=G2[:, hf * 512:(hf + 1) * 512], op=ALU.mult), r=[psr[2 + hf]], w=[("tmpo", hf)])
            P.op("dve", I("tensor_tensor", out=x1[b2], in0=tmpo, in1=x1[b2], op=ALU.add),
                 r=[("tmpo", 0), ("tmpo", 1), x1r], w=[x1r])
            P.op("act", I("activation", out=tmpo, in_=x1[b2], func=AF.Square, accum_out=ssC[:, 4:5]),
                 r=[x1r], w=[("tmpo", 0), ("tmpo", 1), "ssC4"])
            P.op("act", I("activation", out=ssC[:, 5:6], in_=ssC[:, 4:5], func=AF.Sqrt, scale=1.0 / D, bias=EPS),
                 r=["ssC4"], w=["ssC5"])
            P.op("dve", I("reciprocal", out=ssC[:, 6:7], in_=ssC[:, 5:6]), r=["ssC5"], w=["ssC6"])
            P.op("dve", I("scalar_tensor_tensor", out=tmpo, in0=x1[b2], scalar=ssC[:, 6:7], in1=AFr,
                          op0=ALU.mult, op1=ALU.mult), r=[x1r, "ssC6"], w=[("tmpo", 0), ("tmpo", 1)])
            P.op("dve", I("tensor_tensor", out=tmpo, in0=tmpo, in1=BFr, op=ALU.add),
                 r=[("tmpo", 0), ("tmpo", 1)], w=[("tmpo", 0), ("tmpo", 1)])
            P.op("sp", I("dma_start", out=out[t * 128:(t + 1) * 128, :], in_=tmpo),
                 r=[("tmpo", 0), ("tmpo", 1)], w=[("out_d", t)], dma=True, key="outd")

        fe_loads(0)
        pump(FE(0))
        if ntc > 1:
            fe_loads(1)
        for t in range(ntc):
            nxt = FE(t + 1) if t + 1 < ntc else None
            BE(t, nxt)
        info = P.emit(final_wait_keys=["outd"])
        A.release()
    return nc, info


def make_in_maps(inputs, S=SEQ, n_cores=N_CORES):
    f = lambda a: np.ascontiguousarray(np.asarray(a, dtype=np.float32))
    x = f(inputs["x"])
    c = f(inputs["c"])
    gn = np.concatenate([f(inputs["gn_sb_g"])[0], f(inputs["gn_fox_g"])[0]], axis=0)
    gn = np.ascontiguousarray(gn.reshape(8, 128).T)
    shared = {
        "w_ada": f(inputs["w_ada"])[0],
        "b_ada": f(inputs["b_ada"])[0].reshape(1, -1),
        "norm_mix_g": f(inputs["norm_mix_g"])[0].reshape(1, -1),
        "w_in": f(inputs["w_in"])[0],
        "b_fgate": f(inputs["b_fgate"])[0].reshape(8, 1),
        "gn": gn,
        "w_out": f(inputs["w_out"])[0],
        "norm_ffn_g": f(inputs["norm_ffn_g"])[0].reshape(1, -1),
        "w_query": f(inputs["w_query"])[0],
        "sub_keys": f(inputs["sub_keys"])[0].reshape(16, 128, 128),
        "expert_uv": np.ascontiguousarray(np.concatenate([f(inputs["expert_u"])[0], f(inputs["expert_v"])[0]], axis=1)),
        "w_ada_final": f(inputs["w_ada_final"]),
        "b_ada_final": f(inputs["b_ada_final"]).reshape(1, -1),
        "norm_final_g": f(inputs["norm_final_g"]).reshape(1, -1),
    }
    maps = []
    for b in range(n_cores):
        m = dict(shared)
        m["x"] = np.ascontiguousarray(x[b, :S])
        m["cT"] = np.ascontiguousarray(c[b].reshape(8, 128).T)
        maps.append(m)
    return maps


def kernel(**inputs):
    nc, _ = build(SEQ)
    in_maps = make_in_maps(inputs, SEQ, N_CORES)
    res = run_bass_kernel_spmd(nc, in_maps, core_ids=list(range(N_CORES)))
    return np.stack([np.asarray(r["out"], dtype=np.float32) for r in res.results], axis=0)
```

```python
import contextlib
import math

import numpy as np
import concourse.bass as bass
import concourse.mybir as mybir
from concourse.bass_utils import run_bass_kernel_spmd

F32 = mybir.dt.float32
BF16 = mybir.dt.bfloat16
I32 = mybir.dt.int32
U32 = mybir.dt.uint32
AF = mybir.ActivationFunctionType
ALU = mybir.AluOpType
AX = mybir.AxisListType

D = 1024
EPS = 1e-6
N_CORES = 8
SEQ = 4096
NEG = -1.0e30


class _Op:
    __slots__ = ("eng", "fn", "deps", "dma", "semkey", "signal", "semval", "waits")

    def __init__(self, eng, fn, deps, dma, semkey):
        self.eng = eng
        self.fn = fn
        self.deps = deps
        self.dma = dma
        self.semkey = semkey
        self.signal = False
        self.semval = 0
        self.waits = ()


class Prog:
    ENGS = ("pe", "act", "dve", "pool", "sp")

    def __init__(self, nc):
        self.nc = nc
        self.ops = []
        self.last_w = {}
        self.readers = {}
        self.pending = {e: set() for e in self.ENGS}
        self.last_eng = {}
        self.last_dma = {}

    def op(self, eng, fn, r=(), w=(), dma=False, key=None):
        idx = len(self.ops)
        deps = set()
        for res in r:
            lw = self.last_w.get(res)
            if lw is not None:
                deps.add(lw)
        for res in w:
            lw = self.last_w.get(res)
            if lw is not None:
                deps.add(lw)
            deps.update(self.readers.get(res, ()))
        if self.pending[eng]:
            deps |= self.pending[eng]
            self.pending[eng] = set()
        for res in r:
            self.readers.setdefault(res, []).append(idx)
        for res in w:
            self.last_w[res] = idx
            self.readers[res] = []
        semkey = None
        if dma:
            semkey = key if key is not None else (w[0] if w else ("dma", idx))
            self.last_dma[semkey] = idx
        else:
            self.last_eng[eng] = idx
        self.ops.append(_Op(eng, fn, deps, dma, semkey))
        return idx

    def barrier(self):
        deps = set(self.last_eng.values()) | set(self.last_dma.values())
        for e in self.ENGS:
            self.pending[e] |= deps
        self.last_w.clear()
        self.readers.clear()

    def emit(self, final_wait_keys=()):
        nc = self.nc
        ops = self.ops
        cnt = {e: 0 for e in self.ENGS}
        for o in ops:
            best = {}
            keep = set()
            for d in o.deps:
                p = ops[d]
                if p.dma:
                    keep.add(d)
                else:
                    if p.eng == "pe" and o.eng == "pe" and not o.dma:
                        continue
                    if p.eng not in best or best[p.eng] < d:
                        best[p.eng] = d
            keep.update(best.values())
            o.deps = keep
            for d in keep:
                ops[d].signal = True
        dma_cnt = {}
        dma_keys = []
        for o in ops:
            waits = {}
            for d in o.deps:
                p = ops[d]
                if p.dma:
                    waits[("d", p.semkey)] = dma_cnt[p.semkey]
                else:
                    k = ("e", p.eng)
                    if waits.get(k, 0) < p.semval:
                        waits[k] = p.semval
            o.waits = waits
            if o.dma:
                if o.semkey not in dma_cnt:
                    dma_cnt[o.semkey] = 0
                    dma_keys.append(o.semkey)
                dma_cnt[o.semkey] += 16
                o.semval = dma_cnt[o.semkey]
                o.signal = True
            elif o.signal:
                cnt[o.eng] += 1
                o.semval = cnt[o.eng]
        with contextlib.ExitStack() as st:
            esem = {e: st.enter_context(nc.semaphore("s_" + e)) for e in self.ENGS}
            dsem = {}
            for k in dma_keys:
                dsem[k] = st.enter_context(nc.semaphore("d%d" % len(dsem)))
            block = st.enter_context(nc.Block())

            def run(engname, e):
                waited = {}
                for o in ops:
                    if o.eng != engname:
                        continue
                    for k, v in o.waits.items():
                        if waited.get(k, 0) >= v:
                            continue
                        s = dsem[k[1]] if k[0] == "d" else esem[k[1]]
                        e.wait_ge(s, v)
                        waited[k] = v
                    inst = o.fn(e)
                    if o.signal:
                        if o.dma:
                            inst.then_inc(dsem[o.semkey], 16)
                        else:
                            inst.then_inc(esem[o.eng], 1)
                if engname == "sp":
                    for k in final_wait_keys:
                        e.wait_ge(dsem[k], dma_cnt[k])

            @block.tensor
            def _(e):
                run("pe", e)

            @block.scalar
            def _(e):
                run("act", e)

            @block.vector
            def _(e):
                run("dve", e)

            @block.gpsimd
            def _(e):
                run("pool", e)

            @block.sync
            def _(e):
                run("sp", e)
        return len(dsem), dict(cnt)


def I(name, *args, **kwargs):
    return lambda e: getattr(e, name)(*args, **kwargs)


class Arena:
    def __init__(self, ap, nwords):
        self.ap = ap
        self.n = nwords
        self.off = 0
        self.marks = []

    def f32(self, n):
        assert self.off + n <= self.n, ("arena overflow", self.off, n, self.n)
        a = self.ap[:, self.off:self.off + n]
        self.off += n
        return a

    def b16(self, n):
        assert n % 2 == 0
        return self.f32(n // 2).bitcast(BF16)

    def i32(self, n):
        return self.f32(n).bitcast(I32)

    def u32(self, n):
        return self.f32(n).bitcast(U32)

    def mark(self):
        self.marks.append(self.off)

    def release(self):
        self.off = self.marks.pop()


def build(S=SEQ, dbg=False, n_tok_tiles_c=None):
    NT = S // 128
    NCH = S // 512
    nc = bass.Bass("TRN2", target_bir_lowering=False)

    def din(name, shape, dt=F32):
        return nc.dram_tensor(name, list(shape), dt, kind="ExternalInput").ap()

    x = din("x", [S, D])
    cT = din("cT", [128, 8])
    w_ada = din("w_ada", [D, 6 * D])
    b_ada = din("b_ada", [1, 6 * D])
    norm_mix_g = din("norm_mix_g", [1, D])
    w_in = din("w_in", [D, 3080])
    b_fgate = din("b_fgate", [8, 1])
    gn = din("gn", [128, 8])
    w_out = din("w_out", [D, D])
    norm_ffn_g = din("norm_ffn_g", [1, D])
    w_query = din("w_query", [D, 2048])
    sub_keys = din("sub_keys", [16, 128, 128])
    expert_uv = din("expert_uv", [16384, 2 * D])
    w_ada_final = din("w_ada_final", [D, 2 * D])
    b_ada_final = din("b_ada_final", [1, 2 * D])
    norm_final_g = din("norm_final_g", [1, D])
    out = nc.dram_tensor("out", [S, D], F32, kind="ExternalOutput").ap()

    def dscr(name, shape, dt=BF16):
        return nc.dram_tensor(name, list(shape), dt, kind=("ExternalOutput" if dbg else "Internal")).ap()

    dbg_t = {}
    if dbg:
        for nm, shp, dt in (("d_mod", [128, 8 * D], F32), ("d_x1", [128, D], F32), ("d_h2", [128, D], BF16),
                            ("d_eidx", [128, 128], I32), ("d_gates", [128, 128], F32), ("d_araw", [128, 128], F32),
                            ("d_sc", [128, 2048], F32), ("d_peer", [128, D], F32), ("d_m16", [128, 256], F32)):
            dbg_t[nm] = nc.dram_tensor(nm, shp, dt, kind="ExternalOutput").ap()

    qT_d = dscr("qT_d", [8, 128, S])
    kT_d = dscr("kT_d", [8, 128, S])
    v_d = dscr("v_d", [8, 128, NT, 128])
    aug_d = dscr("aug_d", [8, 2, 6, S])
    md_d = dscr("md_d", [8, 128, S])
    uvb = nc.dram_tensor("uvb", [16384, 2 * D], BF16, kind="Internal").ap()

    P = Prog(nc)
    ARENA_WORDS = 53000
    with contextlib.ExitStack() as st:
        arena_t = st.enter_context(nc.sbuf_tensor("arena", [128, ARENA_WORDS], F32))
        A = Arena(arena_t, ARENA_WORDS)
        psbig = st.enter_context(nc.psum_tensor("psbig", [128, 8 * 512], F32))
        ps = [psbig[:, i * 512:(i + 1) * 512] for i in range(8)]
        psr = ["ps%d" % i for i in range(8)]

        ones_f = A.f32(128)
        L_f = A.f32(128)
        bd_f = A.f32(128)
        mstrict_f = A.f32(128)
        ident_b = A.b16(128)
        ones_b = A.b16(128)
        mincl_b = A.b16(128)
        iota16 = A.f32(16)
        gn_sb = A.f32(8)
        nbfg = A.f32(1)
        MOD = A.f32(6 * D)
        MODF = A.f32(2 * D)
        B1, A1, G1 = MOD[:, 0:D], MOD[:, D:2 * D], MOD[:, 2 * D:3 * D]
        B2, A2, G2 = MOD[:, 3 * D:4 * D], MOD[:, 4 * D:5 * D], MOD[:, 5 * D:6 * D]
        BFr, AFr = MODF[:, 0:D], MODF[:, D:2 * D]
        WQ_OFF = ARENA_WORDS - 8192
        wq_b = arena_t[:, WQ_OFF:ARENA_WORDS].bitcast(BF16).rearrange("p (k n) -> p k n", k=8)
        cv_off = A.off
        cvb = [A.b16(2 * 2 * D).rearrange("p (r n) -> p r n", r=2) for _ in range(2)]
        uv_v = expert_uv.rearrange("(p r) n -> p r n", p=128)
        uvb_v = uvb.rearrange("(p r) n -> p r n", p=128)
        conv_state = {"i": 0}

        def conv_step(n):
            for _ in range(n):
                i = conv_state["i"]
                if i > 0 and i <= 64:
                    j = i - 1
                    P.op("pool", I("dma_start", out=uvb_v[:, 2 * j:2 * j + 2, :], in_=cvb[j % 2]),
                         r=[("cvb", j % 2)], w=[("uvb", j)], dma=True, key=("cvs", j % 2))
                if i < 64:
                    P.op("pool", I("dma_start", out=cvb[i % 2], in_=uv_v[:, 2 * i:2 * i + 2, :]),
                         w=[("cvb", i % 2)], dma=True, key=("cvl", i % 2))
                conv_state["i"] = i + 1

        P.op("pool", I("memset", ones_f, 1.0), w=["ones_f"])
        P.op("pool", I("memset", ones_b, 1.0), w=["ones_b"])
        P.op("pool", I("affine_select", out=L_f, in_=ones_f, pattern=[[-1, 128]], compare_op=ALU.is_ge,
                                               fill=0.0, base=0, channel_multiplier=1), r=["ones_f"], w=["L_f"])
        P.op("pool", I("affine_select", out=mstrict_f, in_=ones_f, pattern=[[1, 128]], compare_op=ALU.is_gt,
                                               fill=0.0, base=0, channel_multiplier=-1), r=["ones_f"], w=["mstrict"])
        P.op("pool", I("affine_select", out=mincl_b, in_=ones_f, pattern=[[1, 128]], compare_op=ALU.is_ge,
                                               fill=0.0, base=0, channel_multiplier=-1), r=["ones_f"], w=["mincl"])
        P.op("pool", I("affine_select", out=ident_b, in_=ones_f, pattern=[[-1, 128]], compare_op=ALU.is_equal,
                                               fill=0.0, base=0, channel_multiplier=1), r=["ones_f"], w=["ident"])
        P.op("pool", I("memset", bd_f, 0.0), w=["bd"])
        P.op("pool", I("memset", bd_f[0:64, 0:64], 1.0), w=["bd"])
        P.op("pool", I("memset", bd_f[64:128, 64:128], 1.0), w=["bd"])
        A.mark()
        io_i = A.i32(16)
        P.op("pool", I("iota", io_i, pattern=[[1, 16]], base=0, channel_multiplier=0), w=["io_i"])
        P.op("dve", I("tensor_copy", out=iota16, in_=io_i), r=["io_i"], w=["iota16"])
        P.op("sp", I("dma_start", out=gn_sb, in_=gn), w=["gn"], dma=True)
        bfg = A.f32(1)
        P.op("sp", I("dma_start", out=bfg[0:8, :], in_=b_fgate), w=["bfg"], dma=True)
        P.op("dve", I("tensor_scalar", out=nbfg[0:8, :], in0=bfg[0:8, :], scalar1=-1.0, scalar2=None, op0=ALU.mult),
             r=["bfg"], w=["nbfg"])

        c_sb = A.f32(8)
        c_act = A.f32(8)
        cb = A.f32(8 * 128)
        cbv = cb.rearrange("p (k m) -> p k m", k=8)
        P.op("sp", I("dma_start", out=c_sb, in_=cT), w=["c_sb"], dma=True)
        P.op("act", I("activation", out=c_act, in_=c_sb, func=AF.Silu), r=["c_sb"], w=["c_act"])
        P.op("dve", I("tensor_copy", out=cbv, in_=c_act.unsqueeze(2).to_broadcast([128, 8, 128])),
             r=["c_act"], w=["cb"])
        wst = [A.f32(8 * 512), A.f32(8 * 512)]
        bst = [A.f32(512), A.f32(512)]
        gtmp = A.f32(D)
        blocks = [(w_ada, b_ada, MOD, i) for i in range(12)] + [(w_ada_final, b_ada_final, MODF, i) for i in range(4)]
        for bi, (wsrc, bsrc, dst, i) in enumerate(blocks):
            sb = bi % 2
            wv = wst[sb].rearrange("p (k n) -> p k n", k=8)
            P.op("sp", I("dma_start",
                out=wv, in_=wsrc.rearrange("(k p) n -> p k n", p=128)[:, :, i * 512:(i + 1) * 512]),
                w=[("wst", sb)], dma=True)
            P.op("act", I("dma_start",
                out=bst[sb], in_=bsrc[0:1, i * 512:(i + 1) * 512].partition_broadcast(128)),
                w=[("bst", sb)], dma=True)
            for k in range(8):
                P.op("pe", I("matmul", ps[sb], lhsT=cbv[:, k, :], rhs=wv[:, k, :],
                                                                 start=(k == 0), stop=(k == 7)),
                     r=["cb", ("wst", sb)], w=[psr[sb]])
            P.op("dve", I("tensor_tensor", out=dst[:, i * 512:(i + 1) * 512], in0=ps[sb],
                                                                      in1=bst[sb], op=ALU.add),
                 r=[psr[sb], ("bst", sb)], w=[("mod", bi)])
        for gsrc, row, deps in ((norm_mix_g, A1, (2, 3)), (norm_ffn_g, A2, (8, 9)), (norm_final_g, AFr, (14, 15))):
            P.op("sp", I("dma_start", out=gtmp, in_=gsrc.partition_broadcast(128)), w=["gtmp"], dma=True)
            P.op("dve", I("scalar_tensor_tensor", out=row, in0=row, scalar=1.0, in1=gtmp,
                                                                 op0=ALU.add, op1=ALU.mult),
                 r=["gtmp"] + [("mod", d) for d in deps], w=[("mod", d) for d in deps])
        if dbg:
            P.op("sp", I("dma_start", out=dbg_t["d_mod"][:, 0:6 * D], in_=MOD), r=[("mod", i) for i in range(16)], w=["dbg_mod"], dma=True)
            P.op("sp", I("dma_start", out=dbg_t["d_mod"][:, 6 * D:8 * D], in_=MODF), r=[("mod", i) for i in range(16)], w=["dbg_mod"], dma=True)
        P.barrier()
        A.release()

        A.mark()
        w_in_b = A.b16(8 * 3080).rearrange("p (k n) -> p k n", k=8)
        xt = [A.f32(D) for _ in range(8)]
        junkf = A.f32(D)
        tmpf = A.f32(D)
        hb = [A.b16(D), A.b16(D)]
        hT = [A.b16(8 * 512).rearrange("p (k t) -> p k t", k=8) for _ in range(2)]
        ssA = A.f32(4)
        qkst = [A.b16(512) for _ in range(4)]
        vst = [A.b16(D) for _ in range(2)]
        spf = A.f32(512)
        ef = A.f32(512)
        Gs = [A.f32(512), A.f32(512)]
        r1 = A.f32(512)
        r2 = A.f32(512)
        ones8 = A.f32(512)
        augst = [A.b16(2 * 6 * 512).rearrange("p (q r s) -> p q r s", q=2, r=6) for _ in range(2)]
        for k in range(8):
            for (c0, c1) in ((0, 1024), (1024, 2048), (2048, 3080)):
                P.op("pool", I("dma_start", out=w_in_b[:, k, c0:c1],
                                                                       in_=w_in[k * 128:(k + 1) * 128, c0:c1]),
                     w=[("w_in", k, c0)], dma=True, key="w_in")
        w_in_res = [("w_in", k, c0) for k in range(8) for c0 in (0, 1024, 2048)]
        P.op("pool", I("memset", ones8[0:8, :], 1.0), w=["ones8"])
        for b in range(2):
            P.op("pool", I("memset", augst[b][0:8, 0, 3:6, :], 1.0), w=[("augst", b)])
            P.op("pool", I("memset", augst[b][0:8, 1, 0:3, :], 1.0), w=[("augst", b)])
        psT_b = ps[7].bitcast(BF16)

        def qk_col(g):
            pair, isk = g % 8, g // 8
            if pair < 4:
                return (512 if isk else 0) + pair * 128
            return (2048 if isk else 1536) + (pair - 4) * 128

        def x_loads(ch):
            for tt in range(4):
                t = ch * 4 + tt
                P.op("sp", I("dma_start", out=xt[t % 8], in_=x[t * 128:(t + 1) * 128, :]), w=[("xt", t % 8)], dma=True)

        x_loads(0)
        for ch in range(NCH):
            hb_ = ch % 2
            if ch + 1 < NCH:
                x_loads(ch + 1)
            for tt in range(4):
                t = ch * 4 + tt
                xb_ = t % 8
                P.op("act", I("activation", out=junkf, in_=xt[xb_], func=AF.Square, accum_out=ssA[:, 0:1]),
                     r=[("xt", xb_)], w=["junkf", "ssA0"])
                P.op("act", I("activation", out=ssA[:, 1:2], in_=ssA[:, 0:1], func=AF.Sqrt, scale=1.0 / D, bias=EPS),
                     r=["ssA0"], w=["ssA1"])
                P.op("dve", I("reciprocal", out=ssA[:, 2:3], in_=ssA[:, 1:2]), r=["ssA1"], w=["ssA2"])
                P.op("dve", I("scalar_tensor_tensor", out=tmpf, in0=xt[xb_], scalar=ssA[:, 2:3], in1=A1,
                                                                     op0=ALU.mult, op1=ALU.mult),
                     r=[("xt", xb_), "ssA2"], w=["tmpf"])
                P.op("dve", I("tensor_tensor", out=hb[t % 2], in0=tmpf, in1=B1, op=ALU.add),
                     r=["tmpf"], w=[("hb", t % 2)])
                for k in range(8):
                    P.op("pe", I("transpose", out=psT_b[:, k * 128:(k + 1) * 128],
                                                                   in_=hb[t % 2][:, k * 128:(k + 1) * 128], identity=ident_b),
                         r=[("hb", t % 2), "ident"], w=[psr[7]])
                P.op("act", I("copy", out=hT[hb_][:, :, tt * 128:(tt + 1) * 128],
                                                             in_=psT_b.rearrange("p (k t) -> p k t", k=8)),
                     r=[psr[7]], w=[("hT", hb_, tt)])
            hres = [("hT", hb_, tt) for tt in range(4)]
            for g in range(16):
                pair, isk = g % 8, g // 8
                col0 = qk_col(g)
                bank = g % 2
                sb_ = g % 4
                for k in range(8):
                    P.op("pe", I("matmul",
                        ps[bank], lhsT=w_in_b[:, k, col0:col0 + 128], rhs=hT[hb_][:, k, :], start=(k == 0), stop=(k == 7)),
                        r=hres + w_in_res, w=[psr[bank]])
                if g % 2 == 0:
                    P.op("act", I("activation",
                        out=qkst[sb_], in_=ps[bank], func=AF.Copy, scale=(1.0 if isk else 0.125)),
                        r=[psr[bank]], w=[("qkst", sb_)])
                else:
                    P.op("dve", I("tensor_scalar",
                        out=qkst[sb_], in0=ps[bank], scalar1=(1.0 if isk else 0.125), scalar2=None, op0=ALU.mult),
                        r=[psr[bank]], w=[("qkst", sb_)])
                dst = (kT_d if isk else qT_d)[pair, :, ch * 512:(ch + 1) * 512]
                P.op("sp", I("dma_start", out=dst, in_=qkst[sb_]),
                     r=[("qkst", sb_)], w=[("qk_d", g, ch)], dma=True, key=("qkd", sb_))
            for tt in range(4):
                t = ch * 4 + tt
                vb_ = t % 2
                for vb in range(2):
                    c0 = 1024 if vb == 0 else 2560
                    bank = 2 + vb
                    for k in range(8):
                        P.op("pe", I("matmul",
                            ps[bank], lhsT=hT[hb_][:, k, tt * 128:(tt + 1) * 128], rhs=w_in_b[:, k, c0:c0 + 512],
                            start=(k == 0), stop=(k == 7)),
                            r=hres + w_in_res, w=[psr[bank]])
                    if vb == 0:
                        P.op("act", I("copy", out=vst[vb_][:, 0:512], in_=ps[bank]),
                             r=[psr[bank]], w=[("vst", vb_, 0)])
                    else:
                        P.op("dve", I("tensor_copy", out=vst[vb_][:, 512:1024], in_=ps[bank]),
                             r=[psr[bank]], w=[("vst", vb_, 1)])
                P.op("sp", I("dma_start",
                    out=v_d.rearrange("a p n c -> p a n c")[:, :, t, :],
                    in_=vst[vb_].rearrange("p (a c) -> p a c", a=8)),
                    r=[("vst", vb_, 0), ("vst", vb_, 1)], w=[("v_d", t)], dma=True, key=("vd", vb_))
            for k in range(8):
                P.op("pe", I("matmul", ps[4][0:8, :], lhsT=w_in_b[:, k, 3072:3080], rhs=hT[hb_][:, k, :],
                                                            start=(k == 0), stop=(k == 7)),
                     r=hres + w_in_res, w=[psr[4]])
            P.op("act", I("activation", out=ef[0:8, :], in_=ps[4][0:8, :], func=AF.Exp, scale=-1.0, bias=nbfg[0:8, :]),
                 r=[psr[4], "nbfg"], w=["ef"])
            P.op("act", I("activation", out=spf[0:8, :], in_=ef[0:8, :], func=AF.Ln, bias=1.0), r=["ef"], w=["spf"])
            gb = ch % 2
            if ch == 0:
                P.op("dve", I("tensor_tensor_scan", out=Gs[gb][0:8, :], data0=ones8[0:8, :], data1=spf[0:8, :],
                                                                 initial=0.0, op0=ALU.mult, op1=ALU.add),
                     r=["ones8", "spf"], w=[("Gs", gb)])
            else:
                P.op("dve", I("tensor_tensor_scan", out=Gs[gb][0:8, :], data0=ones8[0:8, :], data1=spf[0:8, :],
                                                                 initial=Gs[1 - gb][0:8, 511:512], op0=ALU.mult, op1=ALU.add),
                     r=["ones8", "spf", ("Gs", 1 - gb)], w=[("Gs", gb)])
            ab = augst[gb]
            ar = ("augst", gb)
            P.op("dve", I("tensor_copy", out=ab[0:8, 1, 3, :], in_=Gs[gb][0:8, :]), r=[("Gs", gb)], w=[ar])
            P.op("dve", I("tensor_tensor", out=r1[0:8, :], in0=Gs[gb][0:8, :], in1=ab[0:8, 1, 3, :],
                                                               op=ALU.subtract), r=[("Gs", gb), ar], w=["r1"])
            P.op("dve", I("tensor_copy", out=ab[0:8, 1, 4, :], in_=r1[0:8, :]), r=["r1"], w=[ar])
            P.op("dve", I("tensor_tensor", out=r2[0:8, :], in0=r1[0:8, :], in1=ab[0:8, 1, 4, :], op=ALU.subtract),
                 r=["r1", ar], w=["r2"])
            P.op("dve", I("tensor_copy", out=ab[0:8, 1, 5, :], in_=r2[0:8, :]), r=["r2"], w=[ar])
            P.op("dve", I("tensor_scalar", out=ab[0:8, 0, 0:3, :], in0=ab[0:8, 1, 3:6, :], scalar1=-1.0,
                                                         scalar2=None, op0=ALU.mult), r=[ar], w=[ar])
            P.op("sp", I("dma_start", out=aug_d[:, :, :, ch * 512:(ch + 1) * 512], in_=ab[0:8, :, :, :]),
                 r=[ar], w=[("aug_d", ch)], dma=True, key=("augd", gb))
        P.barrier()
        A.release()

        A.mark()
        qTp = [A.b16(S) for _ in range(2)]
        kTp = [A.b16(S) for _ in range(2)]
        Vp = [A.b16(NT * 128).rearrange("p (n c) -> p n c", c=128) for _ in range(2)]
        augt = [A.b16(2 * S).rearrange("p (q s) -> p q s", q=2) for _ in range(2)]
        eb_off = A.off
        ebuf = [[A.f32(512) for _ in range(2)] for _ in range(2)]
        spbuf = [[A.f32(512) for _ in range(2)] for _ in range(2)]
        Vx = [arena_t[:, eb_off + 2048 * b_:eb_off + 2048 * b_ + S // 2].bitcast(BF16).rearrange("p (n c) -> p n c", c=128)
              for b_ in range(2)]
        VX_RES = [[("ebuf", h_, k_) for h_ in range(2) for k_ in range(2)],
                  [("spbuf", h_, k_) for h_ in range(2) for k_ in range(2)]]
        rbuf = [A.f32(512) for _ in range(2)]
        abuf_pair = [A.b16(1024) for _ in range(3)]
        abuf = [[abuf_pair[k_][:, h_ * 512:(h_ + 1) * 512] for k_ in range(3)] for h_ in range(2)]
        Sacc = [A.f32(512) for _ in range(2)]
        on_sb = A.f32(512)
        sq_sb = A.f32(512)
        rt_sb = A.f32(512)
        mst = [A.b16(512) for _ in range(2)]
        ZB = (0, 1)
        IB = (2, 3)
        OB = (4, 5)
        LB = (6, 7)

        def pair_loads(pair):
            fox = pair >= 4
            sb = pair % 2
            if not fox:
                P.op("sp", I("dma_start", out=qTp[sb], in_=qT_d[pair]), w=[("qTp", sb, 0), ("qTp", sb, 1)], dma=True,
                     key=("qTp", sb))
                P.op("sp", I("dma_start", out=kTp[sb], in_=kT_d[pair]), w=[("kTp", sb, 0), ("kTp", sb, 1)], dma=True,
                     key=("kTp", sb))
            else:
                hA, hB = 2 * (pair - 4), 2 * (pair - 4) + 1
                for (dst, src_d, qk, nm) in ((qTp[sb], qT_d, 0, "qTp"), (kTp[sb], kT_d, 1, "kTp")):
                    P.op("sp", I("dma_start", out=dst[0:64, :], in_=src_d[pair, 0:64, :]), w=[(nm, sb, 0)], dma=True,
                         key=(nm, sb, "m"))
                    P.op("pool", I("memset", dst[64:128, :], 0.0), w=[(nm, sb, 1)])
                    P.op("sp", I("dma_start", out=dst[64:70, :], in_=aug_d[hA, qk]), w=[(nm, sb, 1)], dma=True,
                         key=(nm, sb, "a"))
                for (src_d, qk) in ((qT_d, 0), (kT_d, 1)):
                    P.op("sp", I("dma_start", out=augt[sb][0:64, qk, :], in_=src_d[pair, 64:128, :]),
                         w=[("augt", sb, qk, 0)], dma=True, key=("augt", sb, qk, "m"))
                    P.op("pool", I("memset", augt[sb][64:128, qk, :], 0.0), w=[("augt", sb, qk, 1)])
                    P.op("sp", I("dma_start", out=augt[sb][64:70, qk, :], in_=aug_d[hB, qk]),
                         w=[("augt", sb, qk, 1)], dma=True, key=("augt", sb, qk, "a"))
            P.op("sp", I("dma_start", out=Vp[sb], in_=v_d[pair]), w=[("Vp", sb)], dma=True)

        pending_epi = []

        def flush_epi(part=None):
            for epi in pending_epi:
                k = next(i for i, (a, kw) in enumerate(epi) if a[0] == "pe") if epi else 0
                if part in (0, None):
                    for a, kw in epi[:k]:
                        P.op(*a, **kw)
                    del epi[:k]
                if part in (1, None):
                    for a, kw in epi:
                        P.op(*a, **kw)
                    del epi[:]
            if part in (1, None):
                del pending_epi[:]

        pair_loads(0)
        for pair in range(8):
            fox = pair >= 4
            sb = pair % 2
            if pair + 1 < 8:
                pair_loads(pair + 1)
            if fox:
                P.op("pool", I("memset", Vx[sb][:, :, 0:64], 1.0), w=VX_RES[sb])
                P.op("pool", I("tensor_copy", out=Vx[sb][:, :, 64:128], in_=Vp[sb][:, :, 64:128]),
                     r=[("Vp", sb)], w=VX_RES[sb])
                P.op("pool", I("memset", Vp[sb][:, :, 64:128], 1.0), w=[("Vp", sb)])
            v_r = ("Vp", sb)
            for c in range(NCH):
                nk = 4 * (c + 1)
                order = list(range(nk)) if fox else list(range(nk - 1, -1, -1))
                gc = pair * NCH + c
                ob = OB[gc % 2]
                lb = LB[gc % 2]
                conv_step(-(-65 // (8 * NCH)))
                if pair == 7 and c == 0:
                    assert A.off <= WQ_OFF, ("phase B overlaps wq_b", A.off, WQ_OFF)
                    for k in range(8):
                        for h in range(2):
                            P.op("pool", I("dma_start", out=wq_b[:, k, h * 1024:(h + 1) * 1024],
                                           in_=w_query[k * 128:(k + 1) * 128, h * 1024:(h + 1) * 1024]),
                                 w=[("wq", k, h)], dma=True, key="wq")
                if not fox:
                    for hh in range(2):
                        P.op("pool", I("memset", Sacc[hh], 0.0), w=[("S", hh)])

                def rng(kb):
                    j = kb - 4 * c
                    q0 = 128 * j if j > 0 else 0
                    return j, q0

                def emit_qk(i):
                    kb = order[i]
                    j, q0 = rng(kb)
                    for hh in range(2):
                        pb = 64 * hh
                        zb = (ZB[hh], IB[hh])[i % 2] if fox else ZB[hh]
                        if not fox:
                            P.op("pe", I("matmul", ps[zb][:, q0:512], lhsT=kTp[sb][pb:pb + 64, kb * 128:(kb + 1) * 128],
                                         rhs=qTp[sb][pb:pb + 64, c * 512 + q0:(c + 1) * 512], start=True, stop=True),
                                 r=[("qTp", sb, 0), ("qTp", sb, 1), ("kTp", sb, 0), ("kTp", sb, 1)], w=[psr[zb]])
                        else:
                            qs = qTp[sb] if hh == 0 else augt[sb][:, 0, :]
                            ks = kTp[sb] if hh == 0 else augt[sb][:, 1, :]
                            rr = ([("qTp", sb, 0), ("qTp", sb, 1), ("kTp", sb, 0), ("kTp", sb, 1)] if hh == 0 else
                                  [("augt", sb, 0, 0), ("augt", sb, 0, 1), ("augt", sb, 1, 0), ("augt", sb, 1, 1)])
                            P.op("pe", I("matmul", ps[zb][:, q0:512], lhsT=ks[:, kb * 128:(kb + 1) * 128],
                                         rhs=qs[:, c * 512 + q0:(c + 1) * 512], start=True, stop=True),
                                 r=rr, w=[psr[zb]])

                def emit_exp(i):
                    kb = order[i]
                    j, q0 = rng(kb)
                    if fox:
                        zA = (ZB[0], IB[0])[i % 2]
                        P.op("act", I("activation",
                                      out=abuf_pair[i % 3].rearrange("p (h n) -> p h n", h=2)[:, :, q0:512],
                                      in_=psbig[:, zA * 512:(zA + 2) * 512].rearrange("p (h n) -> p h n", h=2)[:, :, q0:512],
                                      func=AF.Exp),
                             r=[psr[zA], psr[zA + 1]], w=[("abuf", 0, i % 3), ("abuf", 1, i % 3)])
                    for hh in range(2):
                        zb = (ZB[hh], IB[hh])[i % 2] if fox else ZB[hh]
                        if fox:
                            pbuf = abuf[hh][i % 3]
                            pr = ("abuf", hh, i % 3)
                            if j >= 0:
                                P.op("pool", I("affine_select", out=pbuf[:, q0:q0 + 128], in_=pbuf[:, q0:q0 + 128],
                                               pattern=[[1, 128]], compare_op=ALU.is_ge, fill=0.0, base=0,
                                               channel_multiplier=-1), r=[pr], w=[pr])
                        else:
                            eb = ebuf[hh][i % 2]
                            er = ("ebuf", hh, i % 2)
                            P.op("act", I("activation",
                                out=eb[:, q0:512], in_=ps[zb][:, q0:512], func=AF.Exp), r=[psr[zb]], w=[er])
                            if j >= 0:
                                P.op("dve", I("tensor_tensor",
                                    out=eb[:, q0:q0 + 128], in0=eb[:, q0:q0 + 128], in1=mstrict_f, op=ALU.mult),
                                    r=[er, "mstrict"], w=[er])

                def emit_ln(i):
                    kb = order[i]
                    j, q0 = rng(kb)
                    for hh in range(2):
                        eb, sp_ = ebuf[hh][i % 2], spbuf[hh][i % 2]
                        P.op("act", I("activation",
                            out=sp_[:, q0:512], in_=eb[:, q0:512], func=AF.Ln, bias=1.0),
                            r=[("ebuf", hh, i % 2)], w=[("spbuf", hh, i % 2)])

                def emit_cum(i):
                    kb = order[i]
                    j, q0 = rng(kb)
                    for hh in range(2):
                        sp_ = spbuf[hh][i % 2]
                        ib = IB[hh]
                        P.op("pe", I("matmul",
                            ps[ib][:, q0:512], lhsT=L_f, rhs=sp_[:, q0:512], start=True, stop=(i == 0)),
                            r=[("spbuf", hh, i % 2), "L_f"], w=[psr[ib]])
                        if i > 0:
                            P.op("pe", I("matmul",
                                ps[ib][:, q0:512], lhsT=ones_f, rhs=Sacc[hh][:, q0:512], start=False, stop=True),
                                r=[("S", hh), "ones_f"], w=[psr[ib]])
                        if i < nk - 1:
                            P.op("pool", I("tensor_tensor",
                                out=Sacc[hh][:, q0:512], in0=Sacc[hh][:, q0:512], in1=sp_[:, q0:512], op=ALU.add),
                                r=[("spbuf", hh, i % 2), ("S", hh)], w=[("S", hh)])

                def emit_neg(i):
                    kb = order[i]
                    j, q0 = rng(kb)
                    for hh in range(2):
                        ib = IB[hh]
                        P.op("act", I("activation",
                            out=rbuf[hh][:, q0:512], in_=ps[ib][:, q0:512], func=AF.Exp, scale=-1.0),
                            r=[psr[ib]], w=[("rbuf", hh)])
                        eb, ab_ = ebuf[hh][i % 2], abuf[hh][i % 3]
                        P.op("dve", I("tensor_tensor",
                            out=ab_[:, q0:512], in0=eb[:, q0:512], in1=rbuf[hh][:, q0:512], op=ALU.mult),
                            r=[("ebuf", hh, i % 2), ("rbuf", hh)], w=[("abuf", hh, i % 3)])

                def emit_av(i):
                    kb = order[i]
                    j, q0 = rng(kb)
                    for hh in range(2):
                        pb = 64 * hh
                        ab_ = abuf[hh][i % 3]
                        if fox:
                            bank = (ob, lb)[hh]
                            lhs = (Vp[sb], Vx[sb])[hh]
                            P.op("pe", I("matmul", ps[bank][:, q0:512], lhsT=lhs[:, kb, :], rhs=ab_[:, q0:512],
                                         start=(i == 0), stop=(i == nk - 1), skip_group_check=True),
                                 r=[("abuf", hh, i % 3), v_r] + (VX_RES[sb] if hh == 1 else []),
                                 w=[(psr[bank], 0), (psr[bank], 1)])
                        else:
                            P.op("pe", I("matmul", ps[ob][pb:pb + 64, q0:512], lhsT=Vp[sb][:, kb, pb:pb + 64],
                                         rhs=ab_[:, q0:512], start=(i == 0), stop=(i == nk - 1), skip_group_check=True),
                                 r=[("abuf", hh, i % 3), v_r], w=[(psr[ob], hh)])

                emit_qk(0)
                for i in range(nk):
                    emit_exp(i)
                    if i + 1 < nk:
                        emit_qk(i + 1)
                    if i == 0:
                        flush_epi(0)
                    if i == 2:
                        flush_epi(1)
                    if fox:
                        if i > 0:
                            emit_av(i - 1)
                    else:
                        emit_ln(i)
                        if i > 0:
                            emit_neg(i - 1)
                        emit_cum(i)
                        if i > 0:
                            emit_av(i - 1)
                if not fox:
                    emit_neg(nk - 1)
                emit_av(nk - 1)

                epi = []
                o_r = [(psr[ob], 0), (psr[ob], 1)]
                l_r = [(psr[lb], 0), (psr[lb], 1)]
                if fox:
                    epi.append((("act", I("activation", out=rt_sb[0:64, :], in_=ps[ob][64:128, :], func=AF.Ln)),
                                dict(r=o_r, w=["rt_sb"])))
                    epi.append((("act", I("activation", out=rt_sb[64:128, :], in_=ps[lb][0:64, :], func=AF.Ln)),
                                dict(r=l_r, w=["rt_sb"])))
                    epi.append((("act", I("activation", out=rt_sb, in_=rt_sb, func=AF.Exp, scale=-1.0)),
                                dict(r=["rt_sb"], w=["rt_sb"])))
                    epi.append((("dve", I("tensor_tensor", out=on_sb[0:64, :], in0=ps[ob][0:64, :], in1=rt_sb[0:64, :],
                                          op=ALU.mult)), dict(r=o_r + ["rt_sb"], w=["on_sb"])))
                    epi.append((("dve", I("tensor_tensor", out=on_sb[64:128, :], in0=ps[lb][64:128, :],
                                          in1=rt_sb[64:128, :], op=ALU.mult)), dict(r=l_r + ["rt_sb"], w=["on_sb"])))
                else:
                    epi.append((("act", I("copy", out=on_sb, in_=ps[ob])), dict(r=o_r, w=["on_sb"])))
                epi.append((("dve", I("tensor_tensor", out=sq_sb, in0=on_sb, in1=on_sb, op=ALU.mult)),
                            dict(r=["on_sb"], w=["sq_sb"])))
                epi.append((("pe", I("matmul", ps[lb], lhsT=bd_f, rhs=sq_sb, start=True, stop=True)),
                            dict(r=["sq_sb", "bd"], w=l_r)))
                epi.append((("act", I("activation", out=sq_sb, in_=ps[lb], func=AF.Ln, scale=1.0 / 64, bias=EPS)),
                            dict(r=l_r, w=["sq_sb"])))
                epi.append((("act", I("activation", out=rt_sb, in_=sq_sb, func=AF.Exp, scale=-0.5)),
                            dict(r=["sq_sb"], w=["rt_sb"])))
                mb = gc % 2
                epi.append((("dve", I("scalar_tensor_tensor", out=mst[mb], in0=on_sb, scalar=gn_sb[:, pair:pair + 1],
                                      in1=rt_sb, op0=ALU.mult, op1=ALU.mult)),
                            dict(r=["on_sb", "rt_sb", "gn"], w=[("mst", mb)])))
                epi.append((("sp", I("dma_start", out=md_d[pair, :, c * 512:(c + 1) * 512], in_=mst[mb])),
                            dict(r=[("mst", mb)], w=[("md_d", pair, c)], dma=True, key=("mdd", mb))))
                pending_epi.append(epi)
        flush_epi()
        conv_step(66)
        P.barrier()
        A.release()

        A.mark()
        A.off = cv_off
        w_out_b = A.b16(8 * D).rearrange("p (k n) -> p k n", k=8)
        skT = A.b16(16 * 128).rearrange("p (g n) -> p g n", g=16)
        NSL = 16
        GRP = 2
        NG = 128 // GRP
        UV = [A.b16(2 * D) for _ in range(NSL)]
        mdt = A.b16(8 * 128).rearrange("p (k t) -> p k t", k=8)
        xc = A.f32(D)
        x1 = [A.f32(D), MOD[:, D:2 * D]]
        tmpc = A.f32(D)
        tmpo = MOD[:, 0:D]
        h2b = [A.b16(D) for _ in range(2)]
        h2T = A.b16(8 * 128).rearrange("p (k t) -> p k t", k=8)
        sc_raw = A.f32(2048)
        sc = sc_raw.rearrange("p (g n) -> p g n", g=16)
        qb = sc_raw[:, 0:1024].bitcast(BF16)
        qpT = sc_raw[:, 1024:2048].bitcast(BF16).rearrange("p (g t) -> p g t", g=16)
        sc2s = [A.f32(128) for _ in range(2)]
        m16 = A.f32(256).rearrange("p (g k) -> p g k", g=16)
        i16 = A.u32(256).rearrange("p (g k) -> p g k", g=16)
        i16f = A.f32(256)
        cand = sc_raw.rearrange("p (h q) -> p h q", h=8)
        oh = A.f32(2048)
        cand2 = oh.rearrange("p (h q) -> p h q", h=8)
        bs = A.f32(128).rearrange("p (h k) -> p h k", h=8)
        bpos = A.u32(128)
        aidx = A.u32(128)
        bidx = A.u32(128)
        af_ = A.f32(128)
        bf_ = A.f32(128)
        i1s = A.f32(128)
        i2s = A.f32(128)
        eidf = A.f32(128)
        eidx = [A.i32(128) for _ in range(2)]
        gex = A.f32(128)
        gsm = A.f32(8)
        gates = [A.f32(128) for _ in range(2)]
        araw = [A.f32(128) for _ in range(2)]
        gl = [A.f32(128) for _ in range(2)]
        prod = [A.b16(D) for _ in range(2)]
        dgb = [A.b16(128) for _ in range(4)]
        ssC = A.f32(16)
        skst = sc_raw.rearrange("p (g c) -> p g c", g=16)
        assert A.off <= WQ_OFF, ("phase C overlaps wq_b", A.off, WQ_OFF)
        skb = oh[:, 0:1024].bitcast(BF16).rearrange("p (g c) -> p g c", g=16)

        for k in range(8):
            P.op("pool", I("dma_start", out=w_out_b[:, k, :], in_=w_out[k * 128:(k + 1) * 128, :]),
                 w=[("w_out", k)], dma=True, key="wc")
        wo_res = [("w_out", k) for k in range(8)]
        wq_res = []
        P.op("sp", I("dma_start", out=skst, in_=sub_keys.rearrange("g n c -> n g c")), w=["skst"], dma=True)
        P.op("dve", I("tensor_copy", out=skb, in_=skst), r=["skst"], w=["skb"])
        pT = [ps[4].bitcast(BF16), ps[5].bitcast(BF16)]
        for g in range(16):
            P.op("pe", I("transpose", out=pT[g // 8][:, (g % 8) * 128:(g % 8 + 1) * 128], in_=skb[:, g, :],
                                                  identity=ident_b), r=["skb", "ident"], w=[psr[4 + g // 8]])
        for hf in range(2):
            P.op("act", I("copy", out=skT[:, hf * 8:(hf + 1) * 8, :],
                                                in_=pT[hf].rearrange("p (g n) -> p g n", g=8)),
                 r=[psr[4 + hf]], w=[("skT", hf)])
        sk_res = [("skT", 0), ("skT", 1)]
        ntc = NT if n_tok_tiles_c is None else n_tok_tiles_c

        def fe_loads(t):
            P.op("sp", I("dma_start", out=xc, in_=x[t * 128:(t + 1) * 128, :]), w=["xc"], dma=True)
            P.op("sp", I("dma_start", out=mdt, in_=md_d.rearrange("a p s -> p a s")[:, :, t * 128:(t + 1) * 128]),
                 w=["mdt"], dma=True)

        def FE(t):
            b2 = t % 2
            x1r, h2r, er, gr = ("x1", b2), ("h2b", b2), ("eidx", b2), ("gates", b2)
            fops = []

            def Q(*a, **k):
                fops.append((a, k))
            for hf in range(2):
                for k in range(8):
                    Q("pe", I("matmul", ps[hf], lhsT=mdt[:, k, :], rhs=w_out_b[:, k, hf * 512:(hf + 1) * 512],
                                 start=(k == 0), stop=(k == 7)), r=["mdt"] + wo_res, w=[psr[hf]])
                Q("dve", I("tensor_tensor", out=tmpc[:, hf * 512:(hf + 1) * 512], in0=ps[hf],
                              in1=G1[:, hf * 512:(hf + 1) * 512], op=ALU.mult), r=[psr[hf]], w=[("tmpc", hf)])
            Q("dve", I("tensor_tensor", out=x1[b2], in0=tmpc, in1=xc, op=ALU.add),
                 r=[("tmpc", 0), ("tmpc", 1), "xc"], w=[x1r])
            Q("act", I("activation", out=tmpc, in_=x1[b2], func=AF.Square, accum_out=ssC[:, 0:1]),
                 r=[x1r], w=[("tmpc", 0), ("tmpc", 1), "ssC0"])
            Q("act", I("activation", out=ssC[:, 1:2], in_=ssC[:, 0:1], func=AF.Sqrt, scale=1.0 / D, bias=EPS),
                 r=["ssC0"], w=["ssC1"])
            Q("dve", I("reciprocal", out=ssC[:, 2:3], in_=ssC[:, 1:2]), r=["ssC1"], w=["ssC2"])
            Q("dve", I("scalar_tensor_tensor", out=tmpc, in0=x1[b2], scalar=ssC[:, 2:3], in1=A2,
                          op0=ALU.mult, op1=ALU.mult), r=[x1r, "ssC2"], w=[("tmpc", 0), ("tmpc", 1)])
            Q("dve", I("tensor_tensor", out=h2b[b2], in0=tmpc, in1=B2, op=ALU.add),
                 r=[("tmpc", 0), ("tmpc", 1)], w=[h2r])
            for k in range(8):
                Q("pe", I("transpose", out=pT[0][:, k * 128:(k + 1) * 128], in_=h2b[b2][:, k * 128:(k + 1) * 128],
                             identity=ident_b), r=[h2r, "ident"], w=[psr[4]])
            Q("act", I("copy", out=h2T, in_=pT[0].rearrange("p (k t) -> p k t", k=8)), r=[psr[4]], w=["h2T"])
            if dbg and t == 0:
                Q("sp", I("dma_start", out=dbg_t["d_x1"], in_=x1[b2]), r=[x1r], w=["dbg1"], dma=True, key="dbg")
                Q("sp", I("dma_start", out=dbg_t["d_h2"], in_=h2b[b2]), r=[h2r], w=["dbg2"], dma=True, key="dbg")
            for blk in range(4):
                bank = 4 + blk
                for k in range(8):
                    Q("pe", I("matmul", ps[bank], lhsT=h2T[:, k, :], rhs=wq_b[:, k, blk * 512:(blk + 1) * 512],
                                 start=(k == 0), stop=(k == 7)), r=["h2T"] + wq_res, w=[psr[bank]])
                if blk % 2 == 0:
                    Q("act", I("copy", out=qb[:, blk * 512:(blk + 1) * 512], in_=ps[bank]), r=[psr[bank]], w=[("qb", blk)])
                else:
                    Q("dve", I("tensor_copy", out=qb[:, blk * 512:(blk + 1) * 512], in_=ps[bank]),
                         r=[psr[bank]], w=[("qb", blk)])
            for g in range(16):
                Q("pe", I("transpose", out=pT[g // 8][:, (g % 8) * 128:(g % 8 + 1) * 128],
                             in_=qb[:, g * 128:(g + 1) * 128], identity=ident_b),
                     r=[("qb", g // 4), "ident"], w=[psr[4 + g // 8]])
            Q("act", I("copy", out=qpT[:, 0:8, :], in_=pT[0].rearrange("p (g n) -> p g n", g=8)),
                 r=[psr[4]], w=[("qpT", 0)])
            Q("dve", I("tensor_copy", out=qpT[:, 8:16, :], in_=pT[1].rearrange("p (g n) -> p g n", g=8)),
                 r=[psr[5]], w=[("qpT", 1)])
            for g in range(16):
                bank = 4 + g // 4
                Q("pe", I("matmul", ps[bank][:, (g % 4) * 128:(g % 4 + 1) * 128], lhsT=qpT[:, g, :], rhs=skT[:, g, :],
                             start=True, stop=True), r=[("qpT", g // 8)] + sk_res, w=[psr[bank]])
            for q4 in range(4):
                if q4 % 2 == 0:
                    Q("act", I("copy", out=sc[:, q4 * 4:(q4 + 1) * 4, :], in_=ps[4 + q4].rearrange("p (g n) -> p g n", g=4)),
                         r=[psr[4 + q4], ("qpT", 0), ("qpT", 1)] + [("qb", b_) for b_ in range(4)], w=[("sc", q4), "cand"])
                else:
                    Q("dve", I("tensor_copy", out=sc[:, q4 * 4:(q4 + 1) * 4, :],
                                  in_=ps[4 + q4].rearrange("p (g n) -> p g n", g=4)),
                         r=[psr[4 + q4], ("qpT", 0), ("qpT", 1)] + [("qb", b_) for b_ in range(4)], w=[("sc", q4), "cand"])
            for g in range(16):
                sr = ("sc", g // 4)
                s2 = sc2s[g % 2]
                s2r = ("sc2", g % 2)
                Q("dve", I("max", out=m16[:, g, 0:8], in_=sc[:, g, :]), r=[sr], w=[("m16a", g)])
                Q("dve", I("max_index", out=i16[:, g, 0:8], in_max=m16[:, g, 0:8], in_values=sc[:, g, :]),
                     r=[sr, ("m16a", g)], w=[("i16a", g)])
                Q("dve", I("match_replace", out=s2, in_to_replace=m16[:, g, 0:8], in_values=sc[:, g, :], imm_value=NEG),
                     r=[sr, ("m16a", g)], w=[s2r])
                Q("dve", I("max", out=m16[:, g, 8:16], in_=s2), r=[s2r], w=[("m16b", g)])
                Q("dve", I("max_index", out=i16[:, g, 8:16], in_max=m16[:, g, 8:16], in_values=s2),
                     r=[s2r, ("m16b", g)], w=[("i16b", g)])
            m_all = [("m16a", g) for g in range(16)] + [("m16b", g) for g in range(16)]
            i_all = [("i16a", g) for g in range(16)] + [("i16b", g) for g in range(16)]
            Q("dve", I("tensor_copy", out=i16f, in_=i16.rearrange("p g k -> p (g k)")), r=i_all, w=["i16f"])
            m16v = m16.rearrange("p (h two) k -> p h two k", two=2)
            candv = cand.rearrange("p h (a b) -> p h a b", a=16)
            Q("dve", I("tensor_tensor", out=candv, in0=m16v[:, :, 0, :].unsqueeze(3).to_broadcast([128, 8, 16, 16]),
                          in1=m16v[:, :, 1, :].unsqueeze(2).to_broadcast([128, 8, 16, 16]), op=ALU.add),
                 r=m_all, w=["cand"] + [("sc", q4) for q4 in range(4)])
            bposv = bpos.rearrange("p (h k) -> p h k", h=8)
            for h in range(8):
                Q("dve", I("max", out=bs[:, h, 0:8], in_=cand[:, h, :]), r=["cand"], w=[("bsa", h)])
                Q("dve", I("max_index", out=bposv[:, h, 0:8], in_max=bs[:, h, 0:8], in_values=cand[:, h, :]),
                     r=["cand", ("bsa", h)], w=[("bpa", h)])
                Q("dve", I("match_replace", out=cand2[:, h, :], in_to_replace=bs[:, h, 0:8], in_values=cand[:, h, :],
                              imm_value=NEG), r=["cand", ("bsa", h)], w=[("cand2", h), "oh"])
                Q("dve", I("max", out=bs[:, h, 8:16], in_=cand2[:, h, :]), r=[("cand2", h)], w=[("bsb", h)])
                Q("dve", I("max_index", out=bposv[:, h, 8:16], in_max=bs[:, h, 8:16], in_values=cand2[:, h, :]),
                     r=[("cand2", h), ("bsb", h)], w=[("bpb", h)])
            bs_all = [("bsa", h) for h in range(8)] + [("bsb", h) for h in range(8)]
            bp_all = [("bpa", h) for h in range(8)] + [("bpb", h) for h in range(8)]
            Q("dve", I("tensor_single_scalar", out=aidx, in_=bpos, scalar=4, op=ALU.logical_shift_right), r=bp_all, w=["aidx"])
            Q("dve", I("tensor_single_scalar", out=bidx, in_=bpos, scalar=15, op=ALU.bitwise_and), r=bp_all, w=["bidx"])
            Q("dve", I("tensor_copy", out=af_, in_=aidx), r=["aidx"], w=["af"])
            Q("dve", I("tensor_copy", out=bf_, in_=bidx), r=["bidx"], w=["bf"])
            i16fv = i16f.rearrange("p (h two k) -> p h two k", h=8, two=2)
            ohv = oh.rearrange("p (h k a) -> p h k a", h=8, k=16)
            io_b = iota16.unsqueeze(1).unsqueeze(1).to_broadcast([128, 8, 16, 16])
            for which, (posf, dst) in enumerate(((af_, i1s), (bf_, i2s))):
                pv = posf.rearrange("p (h k) -> p h k", h=8)
                Q("dve", I("tensor_tensor", out=ohv, in0=io_b, in1=pv.unsqueeze(3).to_broadcast([128, 8, 16, 16]),
                              op=ALU.is_equal), r=["iota16", "af", "bf"] + [("cand2", h) for h in range(8)], w=["oh"])
                Q("dve", I("tensor_tensor", out=ohv, in0=ohv,
                              in1=i16fv[:, :, which, :].unsqueeze(2).to_broadcast([128, 8, 16, 16]), op=ALU.mult),
                     r=["oh", "i16f"], w=["oh"])
                Q("dve", I("tensor_reduce", out=dst.rearrange("p (h k) -> p h k", h=8), in_=ohv, axis=AX.X, op=ALU.add),
                     r=["oh"], w=[("isel", which)])
            Q("dve", I("scalar_tensor_tensor", out=eidf, in0=i1s, scalar=128.0, in1=i2s, op0=ALU.mult, op1=ALU.add),
                 r=[("isel", 0), ("isel", 1)], w=["eidf"])
            Q("dve", I("tensor_copy", out=eidx[b2], in_=eidf), r=["eidf"], w=[er])
            gexv = gex.rearrange("p (h k) -> p h k", h=8)
            Q("dve", I("tensor_tensor", out=gexv, in0=bs, in1=bs[:, :, 0:1].to_broadcast([128, 8, 16]), op=ALU.subtract),
                 r=bs_all, w=["gex"])
            Q("act", I("activation", out=gex, in_=gex, func=AF.Exp), r=["gex"], w=["gex"])
            Q("dve", I("tensor_reduce", out=gsm, in_=gexv, axis=AX.X, op=ALU.add), r=["gex"], w=["gsm"])
            Q("dve", I("reciprocal", out=gsm, in_=gsm), r=["gsm"], w=["gsm"])
            Q("dve", I("tensor_tensor", out=gates[b2].rearrange("p (h k) -> p h k", h=8), in0=gexv,
                          in1=gsm.unsqueeze(2).to_broadcast([128, 8, 16]), op=ALU.mult), r=["gex", "gsm"], w=[gr])
            if dbg and t == 0:
                Q("sp", I("dma_start", out=dbg_t["d_eidx"], in_=eidx[b2]), r=[er], w=["dbg3"], dma=True, key="dbg")
                Q("sp", I("dma_start", out=dbg_t["d_gates"], in_=gates[b2]), r=[gr], w=["dbg4"], dma=True, key="dbg")
                Q("sp", I("dma_start", out=dbg_t["d_m16"], in_=m16.rearrange("p g k -> p (g k)")), r=m_all, w=["dbg7"],
                     dma=True, key="dbg")
            return fops

        def pump(fops, n=None):
            if not fops:
                return
            k = len(fops) if n is None else min(n, len(fops))
            for a, kw in fops[:k]:
                P.op(*a, **kw)
            del fops[:k]

        state = {"fe": None, "rate": 0.0, "acc": 0.0}
        fe_lists = {}

        def pump_step():
            state["acc"] += state["rate"]
            k = int(state["acc"])
            if k:
                pump(state["fe"], k)
                state["acc"] -= k

        def be_head(t, g):
            b2 = t % 2
            for j in range(g * GRP, (g + 1) * GRP):
                s_, p_ = j % NSL, j % 2
                P.op("pool", I("indirect_dma_start", out=UV[s_], out_offset=None, in_=uvb,
                               in_offset=bass.IndirectOffsetOnAxis(ap=eidx[b2][:, j:j + 1], axis=0)),
                     r=[("eidx", b2)], w=[("UV", s_)], dma=True)
                P.op("dve", I("tensor_tensor", out=prod[p_], in0=UV[s_][:, 0:D], in1=h2b[b2], op=ALU.mult),
                     r=[("UV", s_), ("h2b", b2)], w=[("prod", p_)])
                P.op("act", I("activation", out=prod[p_], in_=prod[p_], func=AF.Identity, accum_out=araw[b2][:, j:j + 1]),
                     r=[("prod", p_)], w=[("prod", p_), ("araw", b2, j)])
                pump_step()

        def be_gelu(t, g):
            b2 = t % 2
            P.op("act", I("activation", out=gl[b2][:, g * GRP:(g + 1) * GRP], in_=araw[b2][:, g * GRP:(g + 1) * GRP],
                          func=AF.Gelu), r=[("araw", b2, j) for j in range(g * GRP, (g + 1) * GRP)], w=[("gl", b2, g)])

        def be_diag(t, g):
            b2 = t % 2
            for j in range(g * GRP, (g + 1) * GRP):
                s_, d_ = j % NSL, j % 4
                P.op("dve", I("tensor_scalar", out=dgb[d_], in0=ident_b, scalar1=gl[b2][:, j:j + 1],
                              scalar2=gates[b2][:, j:j + 1], op0=ALU.mult, op1=ALU.mult),
                     r=[("gl", b2, g), ("gates", b2), "ident"], w=[("dgb", d_)])
                for hf in range(2):
                    P.op("pe", I("matmul", ps[2 + hf], lhsT=dgb[d_], rhs=UV[s_][:, D + hf * 512:D + (hf + 1) * 512],
                                 start=(j == 0), stop=(j == 127)), r=[("dgb", d_), ("UV", s_)], w=[psr[2 + hf]])
                pump_step()

        def be_tail(t):
            b2 = t % 2
            x1r = ("x1", b2)
            for hf in range(2):
                P.op("dve", I("tensor_tensor", out=tmpo[:, hf * 512:(hf + 1) * 512], in0=ps[2 + hf],
                              in1=G2[:, hf * 512:(hf + 1) * 512], op=ALU.mult), r=[psr[2 + hf]], w=[("tmpo", hf)])
            P.op("dve", I("tensor_tensor", out=x1[b2], in0=tmpo, in1=x1[b2], op=ALU.add),
                 r=[("tmpo", 0), ("tmpo", 1), x1r], w=[x1r])
            P.op("act", I("activation", out=tmpo, in_=x1[b2], func=AF.Square, accum_out=ssC[:, 4:5]),
                 r=[x1r], w=[("tmpo", 0), ("tmpo", 1), "ssC4"])
            P.op("act", I("activation", out=ssC[:, 5:6], in_=ssC[:, 4:5], func=AF.Sqrt, scale=1.0 / D, bias=EPS),
                 r=["ssC4"], w=["ssC5"])
            P.op("dve", I("reciprocal", out=ssC[:, 6:7], in_=ssC[:, 5:6]), r=["ssC5"], w=["ssC6"])
            P.op("dve", I("scalar_tensor_tensor", out=tmpo, in0=x1[b2], scalar=ssC[:, 6:7], in1=AFr,
                          op0=ALU.mult, op1=ALU.mult), r=[x1r, "ssC6"], w=[("tmpo", 0), ("tmpo", 1)])
            P.op("dve", I("tensor_tensor", out=tmpo, in0=tmpo, in1=BFr, op=ALU.add),
                 r=[("tmpo", 0), ("tmpo", 1)], w=[("tmpo", 0), ("tmpo", 1)])
            P.op("sp", I("dma_start", out=out[t * 128:(t + 1) * 128, :], in_=tmpo),
                 r=[("tmpo", 0), ("tmpo", 1)], w=[("out_d", t)], dma=True, key="outd")

        def start_fe(t):
            if t < ntc:
                fe_lists[t] = FE(t)
                state.update(fe=fe_lists[t], rate=len(fe_lists[t]) / (0.85 * 256), acc=0.0)
            else:
                state.update(fe=None, rate=0.0, acc=0.0)

        fe_loads(0)
        pump(FE(0))
        if ntc > 1:
            fe_loads(1)
        start_fe(1)
        total = ntc * NG
        for n in range(total + 2):
            if n < total:
                t, g = divmod(n, NG)
                if g == 0 and t >= 1:
                    pump(fe_lists[t])
                be_head(t, g)
            if 1 <= n <= total:
                be_gelu(*divmod(n - 1, NG))
            if n >= 2:
                tt, gg = divmod(n - 2, NG)
                be_diag(tt, gg)
                if gg == NG - 1:
                    if tt + 1 < ntc:
                        pump(fe_lists[tt + 1])
                    if tt + 2 < ntc:
                        fe_loads(tt + 2)
                    be_tail(tt)
                    start_fe(tt + 2)
        info = P.emit(final_wait_keys=["outd"])
        A.release()
    return nc, info


def make_in_maps(inputs, S=SEQ, n_cores=N_CORES):
    f = lambda a: np.ascontiguousarray(np.asarray(a, dtype=np.float32))
    x = f(inputs["x"])
    c = f(inputs["c"])
    gn = np.concatenate([f(inputs["gn_sb_g"])[0], f(inputs["gn_fox_g"])[0]], axis=0)
    gn = np.ascontiguousarray(gn.reshape(8, 128).T)
    shared = {
        "w_ada": f(inputs["w_ada"])[0],
        "b_ada": f(inputs["b_ada"])[0].reshape(1, -1),
        "norm_mix_g": f(inputs["norm_mix_g"])[0].reshape(1, -1),
        "w_in": f(inputs["w_in"])[0],
        "b_fgate": f(inputs["b_fgate"])[0].reshape(8, 1),
        "gn": gn,
        "w_out": f(inputs["w_out"])[0],
        "norm_ffn_g": f(inputs["norm_ffn_g"])[0].reshape(1, -1),
        "w_query": f(inputs["w_query"])[0],
        "sub_keys": f(inputs["sub_keys"])[0].reshape(16, 128, 128),
        "expert_uv": np.ascontiguousarray(np.concatenate([f(inputs["expert_u"])[0], f(inputs["expert_v"])[0]], axis=1)),
        "w_ada_final": f(inputs["w_ada_final"]),
        "b_ada_final": f(inputs["b_ada_final"]).reshape(1, -1),
        "norm_final_g": f(inputs["norm_final_g"]).reshape(1, -1),
    }
    maps = []
    for b in range(n_cores):
        m = dict(shared)
        m["x"] = np.ascontiguousarray(x[b, :S])
        m["cT"] = np.ascontiguousarray(c[b].reshape(8, 128).T)
        maps.append(m)
    return maps


def kernel(**inputs):
    nc, _ = build(SEQ)
    in_maps = make_in_maps(inputs, SEQ, N_CORES)
    res = run_bass_kernel_spmd(nc, in_maps, core_ids=list(range(N_CORES)))
    return np.stack([np.asarray(r["out"], dtype=np.float32) for r in res.results], axis=0)
```
